# Optimizing a Trainium2 kernel written in Bass

```python
import math
import jax, jax.numpy as jnp
from jax import lax
import numpy as np

D_MODEL = 1024
BATCH = 8
SEQ = 4096
DEPTH = 4

CHUNK = 64
N_BRANCH = 4
BRANCH_WIDTH = D_MODEL // 2
CONV_WIDTH = 4
NORM_EPS = 1e-6

RW_HEAD = 64
RW_HEADS = BRANCH_WIDTH // RW_HEAD
RW_LORA = 64
RW_GN_EPS = 64e-5

RET_HEADS = 8
RET_V_HEAD = BRANCH_WIDTH // RET_HEADS
RET_QK_HEAD = RET_V_HEAD // 2
RET_QK_WIDTH = RET_HEADS * RET_QK_HEAD
ROPE_BASE = 10000.0

SSD_HEAD = 64
SSD_HEADS = BRANCH_WIDTH // SSD_HEAD
SSD_GROUPS = 2
SSD_STATE = 128
SSD_CONV_DIM = BRANCH_WIDTH + 2 * SSD_GROUPS * SSD_STATE

GDN_HEAD = 128
GDN_HEADS = BRANCH_WIDTH // GDN_HEAD
GDN_CONV_DIM = 3 * BRANCH_WIDTH

IN_WIDTHS = (
    BRANCH_WIDTH, BRANCH_WIDTH, BRANCH_WIDTH, RW_LORA, RW_LORA, BRANCH_WIDTH,
    RET_QK_WIDTH, RET_QK_WIDTH, BRANCH_WIDTH, BRANCH_WIDTH,
    SSD_CONV_DIM, BRANCH_WIDTH, SSD_HEADS,
    GDN_CONV_DIM, BRANCH_WIDTH, GDN_HEADS, GDN_HEADS,
    N_BRANCH * D_MODEL,
)
N_IN = sum(IN_WIDTHS)

kernel_name = 'hybrid_rwkv7_retnet_ssd_gdn_stream_encoder'


def _split(p, widths):
    idx = np.cumsum(widths)[:-1].tolist()
    return jnp.split(p, idx, axis=-1)


def _rms(x, eps=NORM_EPS):
    xf = x.astype(jnp.float32)
    return (xf * lax.rsqrt(jnp.mean(xf * xf, axis=-1, keepdims=True) + eps)).astype(x.dtype)


def _rmsnorm(x, w):
    return _rms(x) * w


def _l2norm(x, eps=1e-6):
    xf = x.astype(jnp.float32)
    return (xf * lax.rsqrt(jnp.sum(xf * xf, axis=-1, keepdims=True) + eps)).astype(x.dtype)


def _head_group_norm(y, w, b, eps):
    yf = y.astype(jnp.float32)
    mu = jnp.mean(yf, axis=-1, keepdims=True)
    var = jnp.mean(jnp.square(yf - mu), axis=-1, keepdims=True)
    yn = (yf - mu) * lax.rsqrt(var + eps)
    bsz, s, h, n = y.shape
    return yn.reshape(bsz, s, h * n).astype(y.dtype) * w + b


def _shift_mix(t, mu):
    prev = jnp.pad(t, ((0, 0), (1, 0), (0, 0)))[:, :-1]
    return t + (prev - t) * mu


def _causal_conv(x, w):
    k, c = w.shape
    return lax.conv_general_dilated(
        x, w[:, None, :].astype(x.dtype), window_strides=(1,), padding=((k - 1, 0),),
        dimension_numbers=('NWC', 'WIO', 'NWC'), feature_group_count=c)


def _rotary(t):
    s = t.shape[1]
    half = t.shape[-1] // 2
    angle = 1.0 / (ROPE_BASE ** jnp.linspace(0.0, 1.0, half, dtype=jnp.float32))
    theta = jnp.arange(s, dtype=jnp.float32)[:, None] * angle[None, :]
    cos = jnp.cos(theta)[None, :, None, :]
    sin = jnp.sin(theta)[None, :, None, :]
    t2 = t.astype(jnp.float32).reshape(*t.shape[:-1], half, 2)
    x1, x2 = t2[..., 0], t2[..., 1]
    out = jnp.stack([x1 * cos - x2 * sin, x1 * sin + x2 * cos], axis=-1)
    return out.reshape(t.shape).astype(t.dtype)


def _chunked_decay_attention(q, k, v, log_a):
    b, s, h, dk = q.shape
    dv = v.shape[-1]
    n = s // CHUNK
    qc = q.reshape(b, n, CHUNK, h, dk)
    kc = k.reshape(b, n, CHUNK, h, dk)
    vc = v.reshape(b, n, CHUNK, h, dv)
    g = jnp.cumsum(log_a.astype(jnp.float32).reshape(b, n, CHUNK, h), axis=2)
    g_last = g[:, :, -1:, :]
    causal = jnp.tril(jnp.ones((CHUNK, CHUNK), dtype=bool))
    gt = jnp.swapaxes(g, 2, 3)
    decay = jnp.exp(jnp.where(causal, gt[..., :, None] - gt[..., None, :], -jnp.inf))
    scores = jnp.einsum('bnihk,bnjhk->bnhij', qc, kc) * decay
    y_intra = jnp.einsum('bnhij,bnjhv->bnihv', scores, vc)
    k_tail = kc * jnp.exp(g_last - g)[..., None]
    chunk_state = jnp.einsum('bnjhk,bnjhv->nbhkv', k_tail, vc)
    chunk_decay = jnp.transpose(jnp.exp(g_last[:, :, 0, :]), (1, 0, 2))

    def step(state, inp):
        st, dc = inp
        return state * dc[..., None, None] + st, state

    init = jnp.zeros((b, h, dk, dv), jnp.float32)
    _, prev = lax.scan(step, init, (chunk_state, chunk_decay))
    y_inter = jnp.einsum('bnihk,nbhkv->bnihv', qc * jnp.exp(g)[..., None], prev)
    return (y_intra + y_inter).reshape(b, s, h, dv).astype(v.dtype)


def _chunked_gated_delta(q, k, v, log_a, beta):
    b, s, h, dk = q.shape
    dv = v.shape[-1]
    n = s // CHUNK
    f32 = jnp.float32

    def to_chunks(t):
        return t.reshape(b, n, CHUNK, h, t.shape[-1]).transpose(1, 0, 3, 2, 4).astype(f32)

    qc, kc, vc = to_chunks(q), to_chunks(k), to_chunks(v)
    bc = to_chunks(beta[..., None])
    g = jnp.cumsum(to_chunks(log_a[..., None])[..., 0], axis=-1)
    eye = jnp.eye(CHUNK, dtype=f32)
    incl = jnp.tril(jnp.ones((CHUNK, CHUNK), dtype=bool))
    strict = jnp.tril(jnp.ones((CHUNK, CHUNK), dtype=bool), -1)
    decay = jnp.exp(jnp.where(incl, g[..., :, None] - g[..., None, :], -jnp.inf))
    kb = kc * bc
    m = jnp.where(strict, jnp.einsum('nbhik,nbhjk->nbhij', kb, kc) * decay, 0.0)
    t_inv = lax.linalg.triangular_solve(eye + m, jnp.broadcast_to(eye, m.shape),
                                        left_side=True, lower=True, unit_diagonal=True)
    u = t_inv @ (vc * bc)
    w = t_inv @ (kb * jnp.exp(g)[..., None])
    attn = jnp.einsum('nbhik,nbhjk->nbhij', qc, kc) * decay
    q_dec = qc * jnp.exp(g)[..., None]
    k_tail = kc * jnp.exp(g[..., -1:] - g)[..., None]
    chunk_decay = jnp.exp(g[..., -1])

    def step(state, inp):
        u_i, w_i, attn_i, qd_i, kt_i, dc_i = inp
        v_new = u_i - w_i @ state
        o = qd_i @ state + attn_i @ v_new
        state = state * dc_i[..., None, None] + jnp.swapaxes(kt_i, -1, -2) @ v_new
        return state, o

    init = jnp.zeros((b, h, dk, dv), f32)
    _, o = lax.scan(step, init, (u, w, attn, q_dec, k_tail, chunk_decay))
    return o.transpose(1, 0, 3, 2, 4).reshape(b, s, h, dv).astype(v.dtype)


def _rwkv7_scan(r, w, k, v, a, bvec):
    bsz, s, h, n = r.shape

    def step(state, inp):
        r_t, w_t, k_t, v_t, a_t, b_t = inp
        sa = jnp.einsum('bhvk,bhk->bhv', state, a_t)
        state = (state * w_t[:, :, None, :] + sa[..., None] * b_t[:, :, None, :]
                 + v_t[..., None] * k_t[:, :, None, :])
        return state, jnp.einsum('bhvk,bhk->bhv', state, r_t)

    xs = tuple(jnp.moveaxis(t, 1, 0) for t in (r, w, k, v, a, bvec))
    init = jnp.zeros((bsz, h, n, n), jnp.float32)
    _, y = lax.scan(step, init, xs)
    return jnp.moveaxis(y, 0, 1)


def _rwkv7_branch(r, k, v, w_lo, a_lo, z, mu_rkv, mu_wa, w_up, w0, a_up, a0, k_k, k_a, r_k, ln_w, ln_b):
    b, s, _ = r.shape
    heads = lambda t: t.reshape(b, s, RW_HEADS, RW_HEAD)
    r = _shift_mix(r, mu_rkv[0])
    k = _shift_mix(k, mu_rkv[1])
    v = _shift_mix(v, mu_rkv[2])
    w_lo = _shift_mix(w_lo, mu_wa[0])
    a_lo = _shift_mix(a_lo, mu_wa[1])
    w_log = -jax.nn.softplus(-(w0 + jnp.tanh(w_lo) @ w_up).astype(jnp.float32)) - 0.5
    decay = jnp.exp(-jnp.exp(w_log))
    iclr = jax.nn.sigmoid(a0 + a_lo @ a_up)
    kk = _l2norm(heads(k * k_k))
    k = k * (1.0 + (iclr - 1.0) * k_a)
    rh, kh, vh = heads(r), heads(k), heads(v)
    y = _rwkv7_scan(rh, heads(decay), kh, vh, -kk, kk * heads(iclr))
    y = _head_group_norm(y, ln_w, ln_b, RW_GN_EPS)
    bonus = (jnp.sum(rh * kh * r_k, axis=-1, keepdims=True) * vh).reshape(b, s, BRANCH_WIDTH)
    return (y + bonus) * jax.nn.silu(z)


def _retention_branch(q, k, v, z, norm_w):
    b, s, _ = q.shape
    qh = _rotary(q.reshape(b, s, RET_HEADS, RET_QK_HEAD))
    kh = _rotary(k.reshape(b, s, RET_HEADS, RET_QK_HEAD)) * (RET_QK_HEAD ** -0.5)
    vh = v.reshape(b, s, RET_HEADS, RET_V_HEAD)
    log_gamma = jnp.log(1.0 - jnp.exp2(-5.0 - jnp.arange(RET_HEADS, dtype=jnp.float32)))
    log_a = jnp.broadcast_to(log_gamma, (b, s, RET_HEADS))
    y = _chunked_decay_attention(qh, kh, vh, log_a)
    y = _rms(y).reshape(b, s, BRANCH_WIDTH) * norm_w
    return y * jax.nn.silu(z)


def _ssd_branch(xbc, z, dt, conv_w, conv_b, dt_bias, a_log, d_skip, norm_w):
    b, s, _ = xbc.shape
    xbc = jax.nn.silu(_causal_conv(xbc, conv_w) + conv_b)
    xs, bm, cm = _split(xbc, (BRANCH_WIDTH, SSD_GROUPS * SSD_STATE, SSD_GROUPS * SSD_STATE))
    rep = SSD_HEADS // SSD_GROUPS
    xh = xs.reshape(b, s, SSD_HEADS, SSD_HEAD)
    bh = jnp.repeat(bm.reshape(b, s, SSD_GROUPS, SSD_STATE), rep, axis=2)
    ch = jnp.repeat(cm.reshape(b, s, SSD_GROUPS, SSD_STATE), rep, axis=2)
    dt = jax.nn.softplus(dt.astype(jnp.float32) + dt_bias)
    a = -jnp.exp(a_log.astype(jnp.float32))
    y = _chunked_decay_attention(ch, bh, xh * dt[..., None], dt * a)
    y = y + xh * d_skip[:, None]
    y = y.reshape(b, s, BRANCH_WIDTH) * jax.nn.silu(z)
    y = _rms(y.reshape(b, s, SSD_GROUPS, BRANCH_WIDTH // SSD_GROUPS)).reshape(b, s, BRANCH_WIDTH)
    return y * norm_w


def _gdn_branch(qkv, z, beta_raw, alpha_raw, conv_w, dt_bias, a_log, norm_w):
    b, s, _ = qkv.shape
    qkv = jax.nn.silu(_causal_conv(qkv, conv_w))
    q, k, v = _split(qkv, (BRANCH_WIDTH, BRANCH_WIDTH, BRANCH_WIDTH))
    heads = lambda t: t.reshape(b, s, GDN_HEADS, GDN_HEAD)
    q = _l2norm(heads(q)) * (GDN_HEAD ** -0.5)
    k = _l2norm(heads(k))
    beta = jax.nn.sigmoid(beta_raw)
    log_a = -jnp.exp(a_log.astype(jnp.float32)) * jax.nn.softplus(alpha_raw.astype(jnp.float32) + dt_bias)
    y = _chunked_gated_delta(q, k, heads(v), log_a, beta)
    y = (_rms(y) * norm_w).reshape(b, s, BRANCH_WIDTH)
    return y * jax.nn.silu(z)


def _layer(x, norm_w, w_in, mu_rkv, mu_wa, w_up, w0, a_up, a0, k_k, k_a, r_k, ln_w, ln_b,
           ret_norm_w, ssd_conv_w, ssd_conv_b, ssd_dt_bias, ssd_a_log, ssd_d, ssd_norm_w,
           gdn_conv_w, gdn_dt_bias, gdn_a_log, gdn_norm_w, w_branch, w_out):
    b, s, _ = x.shape
    h = _rmsnorm(x, norm_w)
    p = h @ w_in
    (rw_r, rw_k, rw_v, rw_wlo, rw_alo, rw_z,
     rt_q, rt_k, rt_v, rt_z,
     sd_xbc, sd_z, sd_dt,
     gd_qkv, gd_z, gd_b, gd_a,
     gate_logits) = _split(p, IN_WIDTHS)
    u_a = _rwkv7_branch(rw_r, rw_k, rw_v, rw_wlo, rw_alo, rw_z, mu_rkv, mu_wa,
                        w_up, w0, a_up, a0, k_k, k_a, r_k, ln_w, ln_b)
    u_b = _retention_branch(rt_q, rt_k, rt_v, rt_z, ret_norm_w)
    u_c = _ssd_branch(sd_xbc, sd_z, sd_dt, ssd_conv_w, ssd_conv_b, ssd_dt_bias, ssd_a_log, ssd_d, ssd_norm_w)
    u_d = _gdn_branch(gd_qkv, gd_z, gd_b, gd_a, gdn_conv_w, gdn_dt_bias, gdn_a_log, gdn_norm_w)
    gates = jax.nn.sigmoid(gate_logits.reshape(b, s, N_BRANCH, D_MODEL))
    merged = gates[:, :, 0] * (u_a @ w_branch[0])
    merged = merged + gates[:, :, 1] * (u_b @ w_branch[1])
    merged = merged + gates[:, :, 2] * (u_c @ w_branch[2])
    merged = merged + gates[:, :, 3] * (u_d @ w_branch[3])
    return x + merged @ w_out


def setup_inputs(seed: int = 0) -> dict:
    key = jax.random.key(seed)
    ks = iter(jax.random.split(key, 32))
    f32 = jnp.float32
    L, W, D = DEPTH, BRANCH_WIDTH, D_MODEL

    def normal(shape, scale):
        return jax.random.normal(next(ks), shape, f32) * scale

    def uniform(shape, lo, hi):
        return jax.random.uniform(next(ks), shape, f32, lo, hi)

    def dt_bias(shape):
        dt = jnp.exp(uniform(shape, math.log(1e-3), math.log(1e-1)))
        return dt + jnp.log(-jnp.expm1(-dt))

    return {
        'x': normal((BATCH, SEQ, D), 1.0),
        'norm_w': 1.0 + normal((L, D), 0.02),
        'w_in': normal((L, D, N_IN), D ** -0.5),
        'rwkv_mu_rkv': uniform((L, 3, W), 0.0, 1.0),
        'rwkv_mu_wa': uniform((L, 2, RW_LORA), 0.0, 1.0),
        'rwkv_w_up': normal((L, RW_LORA, W), 0.5 * RW_LORA ** -0.5),
        'rwkv_w0': uniform((L, W), -6.0, 1.0),
        'rwkv_a_up': normal((L, RW_LORA, W), 0.5 * RW_LORA ** -0.5),
        'rwkv_a0': normal((L, W), 0.1),
        'rwkv_k_k': 0.85 + normal((L, W), 0.02),
        'rwkv_k_a': 1.0 + normal((L, W), 0.02),
        'rwkv_r_k': normal((L, RW_HEADS, RW_HEAD), 0.1),
        'rwkv_ln_w': 1.0 + normal((L, W), 0.02),
        'rwkv_ln_b': normal((L, W), 0.02),
        'ret_norm_w': 1.0 + normal((L, W), 0.02),
        'ssd_conv_w': normal((L, CONV_WIDTH, SSD_CONV_DIM), CONV_WIDTH ** -0.5),
        'ssd_conv_b': normal((L, SSD_CONV_DIM), 0.02),
        'ssd_dt_bias': dt_bias((L, SSD_HEADS)),
        'ssd_A_log': jnp.log(uniform((L, SSD_HEADS), 1.0, 16.0)),
        'ssd_D': 1.0 + normal((L, SSD_HEADS), 0.02),
        'ssd_norm_w': 1.0 + normal((L, W), 0.02),
        'gdn_conv_w': normal((L, CONV_WIDTH, GDN_CONV_DIM), CONV_WIDTH ** -0.5),
        'gdn_dt_bias': dt_bias((L, GDN_HEADS)),
        'gdn_A_log': jnp.log(uniform((L, GDN_HEADS), 1.0, 16.0)),
        'gdn_norm_w': 1.0 + normal((L, GDN_HEAD), 0.02),
        'w_branch': normal((L, N_BRANCH, W, D), W ** -0.5),
        'w_out': normal((L, D, D), D ** -0.5),
        'final_norm_w': 1.0 + normal((D,), 0.02),
    }


def reference(x, norm_w, w_in, rwkv_mu_rkv, rwkv_mu_wa, rwkv_w_up, rwkv_w0, rwkv_a_up, rwkv_a0,
              rwkv_k_k, rwkv_k_a, rwkv_r_k, rwkv_ln_w, rwkv_ln_b, ret_norm_w, ssd_conv_w, ssd_conv_b,
              ssd_dt_bias, ssd_A_log, ssd_D, ssd_norm_w, gdn_conv_w, gdn_dt_bias, gdn_A_log, gdn_norm_w,
              w_branch, w_out, final_norm_w):
    for i in range(DEPTH):
        x = _layer(x, norm_w[i], w_in[i], rwkv_mu_rkv[i], rwkv_mu_wa[i], rwkv_w_up[i], rwkv_w0[i],
                   rwkv_a_up[i], rwkv_a0[i], rwkv_k_k[i], rwkv_k_a[i], rwkv_r_k[i], rwkv_ln_w[i],
                   rwkv_ln_b[i], ret_norm_w[i], ssd_conv_w[i], ssd_conv_b[i], ssd_dt_bias[i],
                   ssd_A_log[i], ssd_D[i], ssd_norm_w[i], gdn_conv_w[i], gdn_dt_bias[i],
                   gdn_A_log[i], gdn_norm_w[i], w_branch[i], w_out[i])
    return _rmsnorm(x, final_norm_w)
```

```python
import contextlib
import numpy as np
import concourse.bass as bass
import concourse.mybir as mybir
from concourse.bass_utils import run_bass_kernel_spmd

F32 = mybir.dt.float32
BF16 = mybir.dt.bfloat16
ALU = mybir.AluOpType
AF = mybir.ActivationFunctionType
AX = mybir.AxisListType

D = 1024
NIN = 11408
DEPTH = 4
SEQ = 4096
BATCH = 8
C = 128
O_RW, O_RET, O_SSD, O_GDN, O_GATE = 0, 2176, 3712, 5256, 7312


class Op:
    __slots__ = ("eng", "fn", "deps", "needed", "dma_sem", "token", "group", "is_dma")


class Group:
    def __init__(self):
        self.n = 0
        self.token = None


class Sched:
    ROT = 3500
    DROT = 3488

    def __init__(self, nc, stack, same_sync=("act", "dve", "pool")):
        self.nc = nc
        self.stack = stack
        self.ops = []
        self.w = {}
        self.r = {}
        self.same_sync = set(same_sync)
        self.nsem = 0

    def new_sem(self, name):
        self.nsem += 1
        return self.stack.enter_context(self.nc.semaphore(f"{name}_{self.nsem}"))

    @staticmethod
    def key(a):
        if isinstance(a, (tuple, str)):
            return a
        return a.name

    def add(self, eng, fn, ins=(), outs=(), dma_sem=None, group=None):
        op = Op()
        op.eng = eng
        op.fn = fn
        op.needed = False
        op.dma_sem = dma_sem
        op.is_dma = dma_sem is not None
        op.group = group
        op.token = None
        if group is not None:
            group.n += 1
        ikeys = [self.key(a) for a in ins if a is not None]
        okeys = [self.key(a) for a in outs if a is not None]
        ikeys.append("EPOCH")
        deps = []
        for k in ikeys:
            deps += self.w.get(k, [])
        for k in okeys:
            deps += self.w.get(k, [])
            rd = self.r.get(k)
            if rd:
                deps += list(rd[0].values())
                deps += rd[1]
        fdeps = []
        seen = set()
        for d in deps:
            if id(d) in seen or d is op:
                continue
            seen.add(id(d))
            if group is not None and d.group is group:
                continue
            if (not d.is_dma) and d.eng == eng and eng not in self.same_sync:
                continue
            d.needed = True
            fdeps.append(d)
        op.deps = fdeps
        for k in ikeys:
            rd = self.r.setdefault(k, [{}, []])
            if op.is_dma:
                rd[1].append(op)
            else:
                rd[0][eng] = op
        for k in okeys:
            cur = self.w.get(k, [])
            if group is not None and cur and all(c.group is group for c in cur):
                cur.append(op)
                self.w[k] = cur
            else:
                self.w[k] = [op]
            self.r[k] = [{}, []]
        self.ops.append(op)
        return op

    def barrier(self):
        self.add("dve", lambda e: e.engine_nop(), outs=["EPOCH"])

    def emit(self):
        nc = self.nc
        cnt, cursem, dcount, waited, dsem = {}, {}, {}, {}, {}
        per_eng = {e: [] for e in ("pe", "act", "dve", "pool", "sp")}
        nwaits = 0
        for op in self.ops:
            waits = {}
            for d in op.deps:
                sem, val = d.token
                k = id(sem)
                if k not in waits or waits[k][1] < val:
                    waits[k] = (sem, val)
            wl = []
            for k, (sem, val) in waits.items():
                wk = (op.eng, k)
                if waited.get(wk, 0) >= val:
                    continue
                waited[wk] = val
                wl.append((sem, val))
            nwaits += len(wl)
            inc = None
            if op.is_dma:
                fam = id(op.dma_sem)
                g = op.group
                need = 16 * (g.n if (g is not None and g.token is None) else 1)
                if fam not in dsem:
                    dsem[fam] = op.dma_sem
                    dcount[fam] = 0
                if (g is None or g.token is None) and dcount[fam] + need > self.DROT:
                    dsem[fam] = self.new_sem("dr")
                    dcount[fam] = 0
                sem = dsem[fam]
                if g is not None:
                    if g.token is None:
                        g.token = (sem, dcount[fam] + 16 * g.n)
                    sem = g.token[0]
                    dcount[fam] += 16
                    op.token = g.token
                else:
                    dcount[fam] += 16
                    op.token = (sem, dcount[fam])
                inc = (sem, 16)
            elif op.needed:
                e = op.eng
                if e not in cursem or cnt[e] >= self.ROT:
                    cursem[e] = self.new_sem("e" + e)
                    cnt[e] = 0
                cnt[e] += 1
                op.token = (cursem[e], cnt[e])
                inc = (cursem[e], 1)
            per_eng[op.eng].append((wl, op.fn, inc))
            op.deps = None
        self.stats = dict(n_ops=len(self.ops), n_waits=nwaits, n_sems=self.nsem,
                          per_eng={e: len(v) for e, v in per_eng.items()})

        def run(e, lst):
            for wl, fn, inc in lst:
                for sem, val in wl:
                    e.wait_ge(sem, val)
                ins = fn(e)
                if inc is not None:
                    ins.then_inc(inc[0], inc[1])

        with nc.Block() as block:
            @block.tensor
            def _(e):
                run(e, per_eng["pe"])

            @block.scalar
            def _(e):
                run(e, per_eng["act"])

            @block.vector
            def _(e):
                run(e, per_eng["dve"])

            @block.gpsimd
            def _(e):
                run(e, per_eng["pool"])

            @block.sync
            def _(e):
                run(e, per_eng["sp"])


CST = {}


def _cst_layout():
    off = 0
    for name, n in [("ident", 128), ("maskA", 512), ("iu", 128), ("niu", 128), ("negt", 128),
                    ("negs", 128), ("sl", 128), ("tmat", 132), ("ones", 128), ("bones", 128),
                    ("sel2", 8), ("swapp", 128), ("dm2", 1152), ("qdec", 384), ("ktbl", 8),
                    ("tblc", 768), ("mask01", 256)]:
        CST[name] = (off, n)
        off += n
    return off


NCST = _cst_layout()


def make_consts(S):
    c = np.zeros((128, NCST), np.float32)

    def put(name, arr):
        o, n = CST[name]
        c[:, o:o + arr.shape[1]] = arr

    i = np.arange(128)
    su = (i[:, None] < i[None, :]).astype(np.float32)
    iu = (i[:, None] <= i[None, :]).astype(np.float32)
    put("ident", np.eye(128, dtype=np.float32))
    put("maskA", np.concatenate([su, iu, su, iu], axis=1))
    put("iu", iu)
    put("niu", -iu)
    put("negt", np.where(i[:, None] <= i[None, :], 0.0, -1e30).astype(np.float32))
    put("negs", np.where(i[None, :] < i[:, None], 0.0, -1e30).astype(np.float32))
    put("sl", su.T.copy())
    tm = np.zeros((128, 132), np.float32)
    m = 63
    tm[:, :128] = iu - (i[:, None] <= m).astype(np.float32)
    tm[:, 128] = -(i <= m).astype(np.float32)
    put("tmat", tm)
    put("ones", np.ones((128, 128), np.float32))
    bo = np.zeros((128, 128), np.float32)
    bo[:64, :64] = 1
    bo[64:, 64:] = 1
    put("bones", bo)
    s2 = np.zeros((128, 8), np.float32)
    s2[:64, 0] = 1
    s2[64:, 1] = 1
    put("sel2", s2)
    sp = np.zeros((128, 128), np.float32)
    sp[i, i ^ 1] = 1
    put("swapp", sp)
    gam = 1.0 - np.exp2(-5.0 - np.arange(8, dtype=np.float64))
    lg = np.log(gam)
    scale = 32 ** -0.5
    dm2 = np.zeros((128, 3, 3, 128), np.float64)
    for h in range(8):
        r, bq = h % 3, h // 3
        dm2[:, r, bq, :] = np.where(i[:, None] <= i[None, :], np.exp(lg[h] * (i[None, :] - i[:, None])), 0.0) * scale
    put("dm2", dm2.reshape(128, 1152).astype(np.float32))
    qd = np.ones((128, 3, 128), np.float64)
    for bq in range(3):
        for p in range(96):
            h = bq * 3 + p // 32
            if h < 8:
                qd[p, bq, :] = np.exp(lg[h] * (i + 1))
    put("qdec", qd.reshape(128, 384).astype(np.float32))
    kt = np.zeros((128, 8), np.float64)
    for h in range(8):
        kt[:, h] = np.exp(lg[h] * (127 - i)) * scale
    put("ktbl", kt.astype(np.float32))
    tc_ = np.zeros((128, 3, 256), np.float64)
    m01 = np.zeros((128, 256), np.float32)
    for hh in range(4):
        m01[hh * 32:(hh + 1) * 32, hh * 64:(hh + 1) * 64] = 1
    for bq in range(3):
        for hh in range(3):
            h = bq * 3 + hh
            if h < 8:
                tc_[hh * 32:(hh + 1) * 32, bq, hh * 64:(hh + 1) * 64] = np.exp(lg[h] * 128)
    put("tblc", tc_.reshape(128, 768).astype(np.float32))
    put("mask01", m01)
    half = 16
    angle = 1.0 / (10000.0 ** np.linspace(0.0, 1.0, half, dtype=np.float32)).astype(np.float32)
    theta = np.arange(S, dtype=np.float32)[:, None] * angle[None, :]
    cos = np.cos(theta).astype(np.float32)
    sin = np.sin(theta).astype(np.float32)
    rope = np.zeros((128, 2, S), np.float32)
    for p in range(128):
        ii = (p % 32) // 2
        rope[p, 0, :] = cos[:, ii]
        rope[p, 1, :] = (-sin[:, ii]) if (p % 2 == 0) else sin[:, ii]
    return c, rope


PARAM_NAMES = ["norm_w", "w_in", "rwkv_mu_rkv", "rwkv_mu_wa", "rwkv_w_up", "rwkv_w0", "rwkv_a_up", "rwkv_a0",
               "rwkv_k_k", "rwkv_k_a", "rwkv_r_k", "rwkv_ln_w", "rwkv_ln_b", "ret_norm_w", "ssd_conv_w",
               "ssd_conv_b", "ssd_dt_bias", "ssd_A_log", "ssd_D", "ssd_norm_w", "gdn_conv_w", "gdn_dt_bias",
               "gdn_A_log", "gdn_norm_w", "w_branch", "w_out", "final_norm_w"]
PARAM_SHAPES = {
    "norm_w": [4, 1024], "w_in": [4, 1024, NIN], "rwkv_mu_rkv": [4, 3, 512], "rwkv_mu_wa": [4, 2, 64],
    "rwkv_w_up": [4, 64, 512], "rwkv_w0": [4, 512], "rwkv_a_up": [4, 64, 512], "rwkv_a0": [4, 512],
    "rwkv_k_k": [4, 512], "rwkv_k_a": [4, 512], "rwkv_r_k": [4, 8, 64], "rwkv_ln_w": [4, 512],
    "rwkv_ln_b": [4, 512], "ret_norm_w": [4, 512], "ssd_conv_w": [4, 4, 1024], "ssd_conv_b": [4, 1024],
    "ssd_dt_bias": [4, 8], "ssd_A_log": [4, 8], "ssd_D": [4, 8], "ssd_norm_w": [4, 512],
    "gdn_conv_w": [4, 4, 1536], "gdn_dt_bias": [4, 4], "gdn_A_log": [4, 4], "gdn_norm_w": [4, 128],
    "w_branch": [4, 4, 512, 1024], "w_out": [4, 1024, 1024], "final_norm_w": [1024],
}

RP = {}


def _rp_layout():
    off = 0
    for name, n in [("w0", 512), ("lnw", 512), ("lnb", 512), ("retnw", 512), ("ssdnw", 512), ("gdnnw", 128),
                    ("ssdD", 8), ("ssddtb", 8), ("ssdA", 8), ("gdndtb", 4), ("gdnA", 4)]:
        RP[name] = (off, n)
        off += n
    return off


NRP = _rp_layout()
PP = {"mu": 0, "kk": 13, "ka": 17, "rk": 21, "a0": 25, "scw": 29, "scb": 61, "gcw": 69}
NPP = 128


def build(S, layers, NT, final_norm, mixers=("rwkv", "ret", "ssd", "gdn"), dbg=False, same_sync=True,
          nslot=3, n_param_layers=DEPTH, lmap=None):
    lmap = lmap or {l: l for l in range(DEPTH)}
    NCH = NT // C
    NMT = S // NT
    nc = bass.Bass("TRN2", target_bir_lowering=False)
    dr = {}
    dr["x"] = nc.dram_tensor("x", [S, D], F32, kind="ExternalInput").ap()
    for n in PARAM_NAMES:
        shp = list(PARAM_SHAPES[n])
        if n != "final_norm_w":
            shp[0] = n_param_layers
        dr[n] = nc.dram_tensor(n, shp, F32, kind="ExternalInput").ap()
    dr["cst"] = nc.dram_tensor("cst", [128, NCST], F32, kind="ExternalInput").ap()
    dr["rope"] = nc.dram_tensor("rope", [128, 2, S], F32, kind="ExternalInput").ap()
    out = nc.dram_tensor("out", [S, D], F32, kind="ExternalOutput").ap()
    scr = [nc.dram_tensor(f"scr{i}", [S, D], F32, kind="Internal").ap() for i in range(2)] if len(layers) > 1 else []
    if dbg:
        dbg_u = nc.dram_tensor("dbg_u", [16 * 128, S], BF16, kind="ExternalOutput").ap()

    with contextlib.ExitStack() as st:
        S_ = Sched(nc, st, same_sync=("act", "dve", "pool") if same_sync else ())
        add = S_.add
        scopes = [st]
        tcount = [0]
        tcache = {}

        def T(name, shape, dt=F32):
            ck = (id(scopes[-1]), name)
            if ck in tcache:
                return tcache[ck]
            tcount[0] += 1
            t_ = scopes[-1].enter_context(nc.sbuf_tensor(f"s{tcount[0]}_{name}", shape, dt))
            tcache[ck] = t_
            return t_

        pbs = [st.enter_context(nc.psum_tensor(f"pb{i}", [128, 512], F32)) for i in range(7)]
        pbt = st.enter_context(nc.psum_tensor("pbt", [128, 1024], BF16))
        pstate = [0]

        def PS():
            p = pbs[pstate[0] % 7]
            pstate[0] += 1
            return p

        def mm(out_, lhsT, rhs, start=True, stop=True):
            add("pe", lambda e: e.matmul(out_, lhsT, rhs, start=start, stop=stop), ins=[lhsT, rhs], outs=[out_])

        def tp(out_, in_, ident):
            add("pe", lambda e: e.transpose(out_, in_, ident), ins=[in_, ident], outs=[out_])

        def A(out_, in_, func, bias=None, scale=None, accum=None):
            kw = {}
            ins = [in_]
            if bias is not None:
                kw["bias"] = bias
                if not isinstance(bias, float):
                    ins.append(bias)
            if scale is not None:
                kw["scale"] = scale
                if not isinstance(scale, float):
                    ins.append(scale)
            outs = [out_]
            if accum is not None:
                kw["accum_out"] = accum
                outs.append(accum)
            add("act", lambda e: e.activation(out_, in_, func, **kw), ins=ins, outs=outs)

        def Acp(out_, in_):
            add("act", lambda e: e.copy(out_, in_), ins=[in_], outs=[out_])

        def Vcp(out_, in_):
            add("dve", lambda e: e.tensor_copy(out_, in_), ins=[in_], outs=[out_])

        def Vtt(out_, a, b, op):
            add("dve", lambda e: e.tensor_tensor(out_, a, b, op), ins=[a, b], outs=[out_])

        def Vts(out_, a, s1, op0, s2=None, op1=None):
            ins = [a] + [s for s in (s1, s2) if s is not None and not isinstance(s, float)]
            if op1 is None:
                add("dve", lambda e: e.tensor_scalar(out_, a, s1, None, op0), ins=ins, outs=[out_])
            else:
                add("dve", lambda e: e.tensor_scalar(out_, a, s1, s2, op0, op1), ins=ins, outs=[out_])

        def Vstt(out_, in0, scalar, in1, op0, op1):
            ins = [in0, in1] + ([] if isinstance(scalar, float) else [scalar])
            add("dve", lambda e: e.scalar_tensor_tensor(out_, in0, scalar, in1, op0, op1), ins=ins, outs=[out_])

        def Vred(out_, in_):
            add("dve", lambda e: e.reduce_sum(out_, in_, AX.X), ins=[in_], outs=[out_])

        def Vrec(out_, in_):
            add("dve", lambda e: e.reciprocal(out_, in_), ins=[in_], outs=[out_])

        def Vset(out_, val):
            add("dve", lambda e: e.memset(out_, val), outs=[out_])

        def rsqrt_(out_, in_, mult, eps):
            Vts(out_, in_, mult, ALU.mult, eps, ALU.add)
            Vrec(out_, out_)
            A(out_, out_, AF.Sqrt)

        cpflip = [0]

        def CP(out_, in_):
            cpflip[0] ^= 1
            (Acp if cpflip[0] else Vcp)(out_, in_)

        def dma(eng, out_, in_, sem, ins=(), outs=(), group=None, slow=False):
            if slow:
                add(eng, lambda e: e.dma_start(out=out_, in_=in_, allow_slow_non_contiguous=True), ins=ins, outs=outs, dma_sem=sem, group=group)
            else:
                add(eng, lambda e: e.dma_start(out=out_, in_=in_), ins=ins, outs=outs, dma_sem=sem, group=group)

        cst = T("cst", [128, NCST])
        sem_c = S_.new_sem("cst")
        g0 = Group()
        dma("sp", cst[:, 0:NCST // 2], dr["cst"][:, 0:NCST // 2], sem_c, outs=[cst], group=g0)
        dma("sp", cst[:, NCST // 2:NCST], dr["cst"][:, NCST // 2:NCST], sem_c, outs=[cst], group=g0)

        def K(name, a=0, b=None):
            o, n = CST[name]
            if b is None:
                b = n
            return cst[:, o + a:o + b]

        ident = K("ident")
        identb = T("identb", [128, 128], BF16)
        Vcp(identb[:], ident)
        finw = T("finw", [128, D])
        sem_f = S_.new_sem("finw")
        if final_norm:
            dma("sp", finw[:], dr["final_norm_w"].partition_broadcast(128), sem_f, outs=[finw])

        xt = T("xt", [128, NCH, D])
        hT = T("hT", [128, 8, NT], BF16)
        u_all = T("u_all", [128, 16, NT], BF16)
        normw = T("normw", [128, D])
        pp = T("pp", [128, NPP])
        rp = T("rp", [128, NRP])
        wau = T("wau", [128, 512])
        ropet = T("ropet", [128, 2, NT])
        slots = [T(f"wslot{i}", [128, 4096], BF16) for i in range(nslot)]
        slot_sem = [S_.new_sem(f"ws{i}") for i in range(nslot)]
        sem_x = S_.new_sem("x")
        sem_o = S_.new_sem("o")
        sem_p = S_.new_sem("p")
        sem_r = S_.new_sem("rope")
        sem_d = S_.new_sem("dbg")
        carry_rw = T("carry_rw", [128, 16])
        carry_sd = T("carry_sd", [128, 8, 3])
        carry_gd = T("carry_gd", [128, 12, 3])
        st_rw = [T(f"st_rw{b}", [128, 128]) for b in range(4)]
        st_ret = [T(f"st_ret{b}", [128, 256]) for b in range(3)]
        st_sd = [T(f"st_sd{g}", [128, 256]) for g in range(2)]
        st_gd = [T(f"st_gd{h}", [128, 128]) for h in range(4)]

        if len(mixers) < 4:
            Vset(u_all[:], 0.0)

        def RPv(name, a=0, b=None):
            o, n = RP[name]
            if b is None:
                b = n
            return rp[:, o + a:o + b]

        jobs = []
        for l in layers:
            for mt in range(NMT):
                if "rwkv" in mixers:
                    jobs += [("in", l, O_RW + 0, 512), ("in", l, O_RW + 512, 512), ("in", l, O_RW + 1024, 512),
                             ("in", l, O_RW + 1536, 128), ("in", l, O_RW + 1664, 512)]
                if "ret" in mixers:
                    jobs += [("in", l, O_RET, 512), ("in", l, O_RET + 512, 512), ("in", l, O_RET + 1024, 512)]
                if "ssd" in mixers:
                    jobs += [("in", l, O_SSD, 512), ("in", l, O_SSD + 512, 512), ("in", l, O_SSD + 1024, 512),
                             ("in", l, O_SSD + 1536, 8)]
                if "gdn" in mixers:
                    jobs += [("in", l, O_GDN, 512), ("in", l, O_GDN + 512, 512), ("in", l, O_GDN + 1024, 512),
                             ("in", l, O_GDN + 1536, 512), ("in", l, O_GDN + 2048, 8)]
                for br in range(4):
                    jobs += [("br", l, br, 0), ("in", l, O_GATE + br * 1024, 512), ("in", l, O_GATE + br * 1024 + 512, 512)]
                jobs += [("out", l, 0, 512), ("out", l, 512, 512)]
        jstate = {"issued": 0, "used": 0}
        stg = [T(f"wstg{i}", [128, 2048]) for i in range(2)]
        stg_sem = [S_.new_sem(f"wstg{i}") for i in range(2)]

        def job_src(j, hh):
            kind, l, a, n = jobs[j]
            l = lmap[l]
            if kind == "in":
                src = dr["w_in"][l][:, a:a + n].rearrange("(k p) c -> p k c", p=128)
                return src[:, hh * 4:(hh + 1) * 4, :], 4, n
            if kind == "br":
                src = dr["w_branch"][l][a].rearrange("(k p) c -> p k c", p=128)
                return src[:, hh * 2:(hh + 1) * 2, :], 2, 1024
            src = dr["w_out"][l][:, a:a + n].rearrange("(k p) c -> p k c", p=128)
            return src[:, hh * 4:(hh + 1) * 4, :], 4, n

        def issue_dma(j):
            for hh in range(2):
                src, nk, n = job_src(j, hh)
                sv = stg[hh][:, 0:nk * n].rearrange("p (k c) -> p k c", k=nk)
                dma("sp", sv, src, stg_sem[hh], outs=[stg[hh]])

        def issue_cast(j):
            sl = slots[j % nslot]
            for hh in range(2):
                src, nk, n = job_src(j, hh)
                w_ = nk * n
                (Acp if hh == 0 else Vcp)(sl[:, hh * w_:(hh + 1) * w_], stg[hh][:, 0:w_])

        def W(desc, live=0):
            j = jstate["used"]
            assert jobs[j] == desc, (jobs[j], desc)
            assert live < nslot - 0
            if j == 0:
                issue_dma(0)
            issue_cast(j)
            if j + 1 < len(jobs):
                issue_dma(j + 1)
            jstate["used"] += 1
            kind, l, a, n = desc
            sl = slots[j % nslot]
            if kind == "br":
                return sl[:, 0:4096].rearrange("p (k c) -> p k c", k=4)
            return sl[:, 0:8 * n].rearrange("p (k c) -> p k c", k=8)

        def proj_fm(Wv, col0, nrows, ps, t0=0, nt=None):
            nt = NT if nt is None else nt
            for kc in range(8):
                mm(ps[:nrows, :nt], Wv[:, kc, col0:col0 + nrows], hT[:, kc, t0:t0 + nt], start=(kc == 0), stop=(kc == 7))

        def proj_tm(Wv, col0, ncols, c, ps):
            for kc in range(8):
                mm(ps[:, :ncols], hT[:, kc, c * C:(c + 1) * C], Wv[:, kc, col0:col0 + ncols], start=(kc == 0), stop=(kc == 7))

        def u_store(u_tm, blk0):
            raise NotImplementedError

        def tm_to_fm_bf16(src_tm, blk0, c):
            ps = PS()
            for b in range(4):
                tp(ps[:, b * 128:(b + 1) * 128], src_tm[:, b * 128:(b + 1) * 128], ident)
            CP(u_all[:, blk0:blk0 + 4, c * C:(c + 1) * C], ps[:, 0:512].rearrange("p (b t) -> p b t", b=4))

        def head_rms(y_ap3, nh, hd, eps, tagscope, sq=None):
            if sq is None:
                sq = T(f"sq_{tagscope}", [128, nh * hd])
            Vtt(sq[:].rearrange("p (h d) -> p h d", h=nh), y_ap3, y_ap3, ALU.mult)
            ss = T(f"ss_{tagscope}", [128, nh])
            Vred(ss[:], sq[:].rearrange("p (h d) -> p h d", h=nh))
            rsqrt_(ss[:], ss[:], 1.0 / hd, eps)
            return ss

        def load_params(l):
            l = lmap[l]
            g = Group()
            dma("sp", normw[:], dr["norm_w"][l].partition_broadcast(128), sem_p, outs=[normw], group=g)
            for name, src in [("w0", dr["rwkv_w0"][l]), ("lnw", dr["rwkv_ln_w"][l]), ("lnb", dr["rwkv_ln_b"][l]),
                              ("retnw", dr["ret_norm_w"][l]), ("ssdnw", dr["ssd_norm_w"][l]),
                              ("gdnnw", dr["gdn_norm_w"][l]), ("ssdD", dr["ssd_D"][l]),
                              ("ssddtb", dr["ssd_dt_bias"][l]), ("ssdA", dr["ssd_A_log"][l]),
                              ("gdndtb", dr["gdn_dt_bias"][l]), ("gdnA", dr["gdn_A_log"][l])]:
                dma("sp", RPv(name), src.partition_broadcast(128), sem_p, outs=[rp], group=g)
            dma("sp", wau[0:64, :], dr["rwkv_w_up"][l], sem_p, outs=[wau], group=g)
            dma("sp", wau[64:128, :], dr["rwkv_a_up"][l], sem_p, outs=[wau], group=g)

            def ppl(col, src, nb):
                dma("sp", pp[:, col:col + nb], src.rearrange("(b p) -> p b", p=128), sem_p, outs=[pp], group=g, slow=True)
            for j in range(3):
                ppl(PP["mu"] + 4 * j, dr["rwkv_mu_rkv"][l][j], 4)
            ppl(PP["mu"] + 12, dr["rwkv_mu_wa"][l].rearrange("a b -> (a b)"), 1)
            ppl(PP["kk"], dr["rwkv_k_k"][l], 4)
            ppl(PP["ka"], dr["rwkv_k_a"][l], 4)
            ppl(PP["rk"], dr["rwkv_r_k"][l].rearrange("a b -> (a b)"), 4)
            ppl(PP["a0"], dr["rwkv_a0"][l], 4)
            for j in range(4):
                ppl(PP["scw"] + 8 * j, dr["ssd_conv_w"][l][j], 8)
            ppl(PP["scb"], dr["ssd_conv_b"][l], 8)
            for j in range(4):
                ppl(PP["gcw"] + 12 * j, dr["gdn_conv_w"][l][j], 12)
            A(RPv("ssdA"), RPv("ssdA"), AF.Exp)
            Vts(RPv("ssdA"), RPv("ssdA"), -1.0, ALU.mult)
            A(RPv("gdnA"), RPv("gdnA"), AF.Exp)
            Vts(RPv("gdnA"), RPv("gdnA"), -1.0, ALU.mult)
            for t_ in [carry_rw, carry_sd, carry_gd] + st_rw + st_ret + st_sd + st_gd:
                Vset(t_[:], 0.0)

        def conv_block(ps, carry, bi, wcol, nblk, bias, out_ap, rbuf, acc):
            Acp(rbuf[:, 0:3], carry[:, bi, :])
            Acp(rbuf[:, 3:3 + NT], ps[:, 0:NT])
            Vts(acc[:], rbuf[:, 0:NT], pp[:, wcol + bi:wcol + bi + 1], ALU.mult)
            for j in range(1, 4):
                c0 = wcol + nblk * j + bi
                Vstt(acc[:], rbuf[:, j:j + NT], pp[:, c0:c0 + 1], acc[:], ALU.mult, ALU.add)
            Acp(carry[:, bi, :], rbuf[:, NT:NT + 3])
            if bias is None:
                A(out_ap, acc[:], AF.Silu)
            else:
                A(out_ap, acc[:], AF.Silu, bias=bias)

        def mixer_ret(l, mt):
            S_.barrier()
            HB = [(0, 3), (3, 3), (6, 2)]
            with contextlib.ExitStack() as sc:
                scopes.append(sc)
                Wqk = W(("in", l, O_RET, 512))
                qk_raw = T("rt_qkraw", [128, NT])
                qk = T("rt_qk", [128, 6, NT])
                t1 = T("rt_t1", [128, NT])
                for b in range(6):
                    h0, nh = HB[b % 3]
                    nr = 32 * nh
                    col0 = (0 if b < 3 else 256) + 32 * h0
                    ps = PS()
                    proj_fm(Wqk, col0, nr, ps)
                    Acp(qk_raw[0:nr, :], ps[0:nr, 0:NT])
                    ps2 = PS()
                    mm(ps2[0:nr, 0:NT], K("swapp")[0:nr, 0:nr], qk_raw[0:nr, :])
                    Vtt(t1[0:nr, :], qk_raw[0:nr, :], ropet[0:nr, 0, :], ALU.mult)
                    Vtt(qk[0:nr, b, :], ps2[0:nr, 0:NT], ropet[0:nr, 1, :], ALU.mult)
                    Vtt(qk[0:nr, b, :], qk[0:nr, b, :], t1[0:nr, :], ALU.add)
                Wv = W(("in", l, O_RET + 512, 512))
                v_tm = [T(f"rt_v{c}", [128, 512]) for c in range(NCH)]
                for c in range(NCH):
                    ps = PS()
                    proj_tm(Wv, 0, 512, c, ps)
                    Acp(v_tm[c][:], ps[:, 0:512])
                Wz = W(("in", l, O_RET + 1024, 512))
                zs = [T(f"rt_z{c}", [128, 512]) for c in range(NCH)]
                for c in range(NCH):
                    ps = PS()
                    proj_tm(Wz, 0, 512, c, ps)
                    A(zs[c][:], ps[:, 0:512], AF.Silu)
                ktail = T("rt_ktail", [128, 256])
                P = T("rt_P", [128, 3, 384])
                qd = T("rt_qd", [128, 3, 128])
                y = T("rt_y", [128, 512])
                tmp = T("rt_tmp", [128, 256])
                for c in range(NCH):
                    cs = slice(c * C, (c + 1) * C)
                    ps = PS()
                    for bq in range(3):
                        h0, nh = HB[bq]
                        nr = 32 * nh
                        tp(ps[:, 32 * h0:32 * h0 + nr], qk[0:nr, 3 + bq, cs], ident[0:nr, 0:nr])
                    Vtt(ktail[:].rearrange("p (h d) -> p h d", h=8), ps[:, 0:256].rearrange("p (h d) -> p h d", h=8),
                        K("ktbl")[:, 0:8].unsqueeze(2).to_broadcast([128, 8, 32]), ALU.mult)
                    pr = [PS() for _ in range(3)]
                    for r in range(3):
                        for bq in range(3):
                            if bq * 3 + r >= 8:
                                continue
                            mm(pr[r][:, bq * 128:(bq + 1) * 128], qk[32 * r:32 * r + 32, 3 + bq, cs], qk[32 * r:32 * r + 32, bq, cs])
                    for r in range(3):
                        w_ = 384 if r < 2 else 256
                        Vtt(P[:, r, 0:w_], pr[r][:, 0:w_], K("dm2")[:, r * 384:r * 384 + w_], ALU.mult)
                    for bq in range(3):
                        nr = 32 * HB[bq][1]
                        Vtt(qd[0:nr, bq, :], qk[0:nr, bq, cs], K("qdec")[0:nr, bq * 128:(bq + 1) * 128], ALU.mult)
                    py = PS()
                    for bq in range(3):
                        h0, nh = HB[bq]
                        nr = 32 * nh
                        mm(py[:, 64 * h0:64 * (h0 + nh)], qd[0:nr, bq, :], st_ret[bq][0:nr, 0:64 * nh], start=True, stop=False)
                        for r in range(nh):
                            h = h0 + r
                            mm(py[:, h * 64:(h + 1) * 64], P[:, r, bq * 128:(bq + 1) * 128], v_tm[c][:, h * 64:(h + 1) * 64],
                               start=False, stop=(r == nh - 1))
                    Acp(y[:], py[:, 0:512])
                    y3 = y[:].rearrange("p (h d) -> p h d", h=8)
                    rstd = head_rms(y3, 8, 64, 1e-6, "rt")
                    Vtt(y3, y3, rstd[:, 0:8].unsqueeze(2).to_broadcast([128, 8, 64]), ALU.mult)
                    Vtt(y[:], y[:], RPv("retnw"), ALU.mult)
                    Vtt(y[:], y[:], zs[c][:], ALU.mult)
                    tm_to_fm_bf16(y, 4, c)
                    for bq in range(3):
                        h0, nh = HB[bq]
                        nr, nv = 32 * nh, 64 * nh
                        ps = PS()
                        mm(ps[0:nr, 0:nv], ktail[:, 32 * h0:32 * h0 + nr], v_tm[c][:, 64 * h0:64 * h0 + nv])
                        Vtt(tmp[0:nr, 0:nv], ps[0:nr, 0:nv], K("mask01")[0:nr, 0:nv], ALU.mult)
                        Vtt(st_ret[bq][0:nr, 0:nv], st_ret[bq][0:nr, 0:nv], K("tblc")[0:nr, bq * 256:bq * 256 + nv], ALU.mult)
                        Vtt(st_ret[bq][0:nr, 0:nv], st_ret[bq][0:nr, 0:nv], tmp[0:nr, 0:nv], ALU.add)
                scopes.pop()

        def decay_prep(ps_raw, nh, dtb, Aneg, tag):
            dt = T(f"dp_dt_{tag}", [128, nh])
            Vtt(dt[:], ps_raw, dtb, ALU.add)
            A(dt[:], dt[:], AF.Exp)
            A(dt[:], dt[:], AF.Ln, bias=1.0)
            la = T(f"dp_la_{tag}", [128, nh])
            Vtt(la[:], dt[:], Aneg, ALU.mult)
            ps = PS()
            mm(ps[:, 0:nh], K("iu"), la[:])
            g = T(f"dp_g_{tag}", [128, nh])
            Acp(g[:], ps[:, 0:nh])
            ng = T(f"dp_ng_{tag}", [128, nh])
            Vts(ng[:], g[:], -1.0, ALU.mult)
            eg = T(f"dp_eg_{tag}", [128, nh])
            A(eg[:], g[:], AF.Exp)
            return dict(dt=dt, la=la, g=g, ng=ng, eg=eg)

        def mixer_ssd(l, mt):
            S_.barrier()
            with contextlib.ExitStack() as sc:
                scopes.append(sc)
                xbc = T("sd_xbc", [128, 8, NT])
                rbuf = T("sd_rbuf", [128, NT + 3])
                acc = T("sd_acc", [128, NT])
                for half in range(2):
                    Wx = W(("in", l, O_SSD + 512 * half, 512))
                    for b4 in range(4):
                        bi = half * 4 + b4
                        ps = PS()
                        proj_fm(Wx, b4 * 128, 128, ps)
                        conv_block(ps, carry_sd, bi, PP["scw"], 8, pp[:, PP["scb"] + bi:PP["scb"] + bi + 1], xbc[:, bi, :], rbuf, acc)
                Wz = W(("in", l, O_SSD + 1024, 512))
                zs = [T(f"sd_z{c}", [128, 512]) for c in range(NCH)]
                for c in range(NCH):
                    ps = PS()
                    proj_tm(Wz, 0, 512, c, ps)
                    A(zs[c][:], ps[:, 0:512], AF.Silu)
                Wdt = W(("in", l, O_SSD + 1536, 8))
                x_tm = T("sd_x", [128, 512])
                b_tm = T("sd_b", [128, 256])
                xdt = T("sd_xdt", [128, 512])
                xdt2 = T("sd_xdt2", [128, 512])
                sc_ = T("sd_sc", [128, 256])
                LAb = [T(f"sd_lab{i}", [128, 128]) for i in range(2)]
                DT = [T(f"sd_dt{i}", [128, 128]) for i in range(2)]
                P = T("sd_P", [128, 8, 128])
                yi = T("sd_yi", [128, 512])
                y = T("sd_y", [128, 512])
                egl = T("sd_egl", [128, 8])
                for c in range(NCH):
                    cs = slice(c * C, (c + 1) * C)
                    ps = PS()
                    proj_tm(Wdt, 0, 8, c, ps)
                    dp = decay_prep(ps[:, 0:8], 8, RPv("ssddtb"), RPv("ssdA"), "sd")
                    ps = PS()
                    mm(ps[:, 0:8], K("ones"), dp["la"][:])
                    A(egl[:], ps[:, 0:8], AF.Exp)
                    ps = PS()
                    for b in range(4):
                        tp(ps[:, b * 128:(b + 1) * 128], xbc[:, b, cs], ident)
                    Acp(x_tm[:], ps[:, 0:512])
                    ps = PS()
                    for g in range(2):
                        tp(ps[:, g * 128:(g + 1) * 128], xbc[:, 4 + g, cs], ident)
                    Vcp(b_tm[:], ps[:, 0:256])
                    Vtt(xdt[:].rearrange("p (h d) -> p h d", h=8), x_tm[:].rearrange("p (h d) -> p h d", h=8),
                        dp["dt"][:, 0:8].unsqueeze(2).to_broadcast([128, 8, 64]), ALU.mult)
                    ps = PS()
                    for g in range(2):
                        mm(ps[:, g * 128:(g + 1) * 128], xbc[:, 4 + g, cs], xbc[:, 6 + g, cs])
                    Acp(sc_[:], ps[:, 0:256])
                    for h in range(8):
                        g = h // 4
                        lab, dtm = LAb[h % 2], DT[h % 2]
                        Vcp(lab[:], dp["la"][:, h:h + 1].to_broadcast([128, 128]))
                        pd = PS()
                        mm(pd[:, 0:128], lab[:], K("iu"), start=True, stop=False)
                        mm(pd[:, 0:128], ident, K("negt"), start=False, stop=True)
                        A(dtm[:], pd[:, 0:128], AF.Exp, bias=dp["ng"][:, h:h + 1])
                        Vtt(P[:, h, :], sc_[:, g * 128:(g + 1) * 128], dtm[:], ALU.mult)
                        Vts(xdt2[:, h * 64:(h + 1) * 64], xdt[:, h * 64:(h + 1) * 64], dtm[:, 127:128], ALU.mult)
                    pyi = PS()
                    for g in range(2):
                        mm(pyi[:, g * 256:(g + 1) * 256], xbc[:, 6 + g, cs], st_sd[g][:])
                    Vtt(yi[:].rearrange("p (h d) -> p h d", h=8), pyi[:, 0:512].rearrange("p (h d) -> p h d", h=8),
                        dp["eg"][:, 0:8].unsqueeze(2).to_broadcast([128, 8, 64]), ALU.mult)
                    py = PS()
                    for h in range(8):
                        mm(py[:, h * 64:(h + 1) * 64], P[:, h, :], xdt[:, h * 64:(h + 1) * 64])
                    Vtt(y[:], py[:, 0:512], yi[:], ALU.add)
                    Vtt(yi[:].rearrange("p (h d) -> p h d", h=8), x_tm[:].rearrange("p (h d) -> p h d", h=8),
                        RPv("ssdD")[:, 0:8].unsqueeze(2).to_broadcast([128, 8, 64]), ALU.mult)
                    Vtt(y[:], y[:], yi[:], ALU.add)
                    Vtt(y[:], y[:], zs[c][:], ALU.mult)
                    y3 = y[:].rearrange("p (h d) -> p h d", h=2)
                    rstd = head_rms(y3, 2, 256, 1e-6, "sd")
                    Vtt(y3, y3, rstd[:, 0:2].unsqueeze(2).to_broadcast([128, 2, 256]), ALU.mult)
                    Vtt(y[:], y[:], RPv("ssdnw"), ALU.mult)
                    tm_to_fm_bf16(y, 8, c)
                    for g in range(2):
                        ps = PS()
                        mm(ps[:, 0:256], b_tm[:, g * 128:(g + 1) * 128], xdt2[:, g * 256:(g + 1) * 256])
                        Vtt(st_sd[g][:].rearrange("p (h d) -> p h d", h=4), st_sd[g][:].rearrange("p (h d) -> p h d", h=4),
                            egl[:, 4 * g:4 * g + 4].unsqueeze(2).to_broadcast([128, 4, 64]), ALU.mult)
                        Vtt(st_sd[g][:], st_sd[g][:], ps[:, 0:256], ALU.add)
                scopes.pop()

        def neumann_apply(Nm, NTm, Z, nh, width, tag, levels=7):
            N2 = [T(f"nm_n2_{tag}{h}", [128, 128]) for h in range(nh)]
            NT2 = [T(f"nm_nt2_{tag}{h}", [128, 128]) for h in range(nh)]
            cur, curT, nxt, nxtT = Nm, NTm, N2, NT2
            for lev in range(levels):
                ps = PS()
                for h in range(nh):
                    mm(ps[:, h * width:(h + 1) * width], curT[h][:], Z[:, h * width:(h + 1) * width])
                dZ = T(f"nm_dz_{tag}", [128, nh * width])
                Acp(dZ[:], ps[:, 0:nh * width])
                if lev < levels - 1:
                    for h in range(nh):
                        pq = PS()
                        mm(pq[:, 0:128], curT[h][:], cur[h][:])
                        mm(pq[:, 128:256], cur[h][:], curT[h][:])
                        cpe = Acp if (h % 2 == 0) else Vcp
                        cpe(nxt[h][:], pq[:, 0:128])
                        cpe(nxtT[h][:], pq[:, 128:256])
                Vtt(Z[:, 0:nh * width], Z[:, 0:nh * width], dZ[:], ALU.add)
                cur, curT, nxt, nxtT = nxt, nxtT, cur, curT

        def mixer_gdn(l, mt):
            S_.barrier()
            with contextlib.ExitStack() as sc:
                scopes.append(sc)
                qkv = T("gd_qkv", [128, 12, NT])
                rbuf = T("gd_rbuf", [128, NT + 3])
                acc = T("gd_acc", [128, NT])
                sq = T("gd_sq", [128, NT])
                for part in range(3):
                    Wx = W(("in", l, O_GDN + 512 * part, 512))
                    for b4 in range(4):
                        bi = part * 4 + b4
                        ps = PS()
                        proj_fm(Wx, b4 * 128, 128, ps)
                        conv_block(ps, carry_gd, bi, PP["gcw"], 12, None, qkv[:, bi, :], rbuf, acc)
                        if part < 2:
                            Vtt(sq[:], qkv[:, bi, :], qkv[:, bi, :], ALU.mult)
                            ps2 = PS()
                            mm(ps2[:, 0:NT], K("ones"), sq[:])
                            rsqrt_(sq[:], ps2[:, 0:NT], 1.0, 1e-6)
                            if part == 0:
                                Vstt(qkv[:, bi, :], qkv[:, bi, :], float(128 ** -0.5), sq[:], ALU.mult, ALU.mult)
                            else:
                                Vtt(qkv[:, bi, :], qkv[:, bi, :], sq[:], ALU.mult)
                import os
                GDSTOP = int(os.environ.get("GDSTOP", "9"))
                Wz = W(("in", l, O_GDN + 1536, 512))
                zs = [T(f"gd_z{c}", [128, 512]) for c in range(NCH)]
                for c in range(NCH):
                    ps = PS()
                    proj_tm(Wz, 0, 512, c, ps)
                    A(zs[c][:], ps[:, 0:512], AF.Silu)
                Wba = W(("in", l, O_GDN + 2048, 8))
                v_tm = T("gd_v", [128, 512])
                ktail = T("gd_ktail", [128, 512])
                beta = T("gd_beta", [128, 4])
                lnb = T("gd_lnb", [128, 4])
                gb = T("gd_gb", [128, 4])
                neg = T("gd_neg", [128, 4])
                egl = T("gd_egl", [128, 4])
                LAb = [T(f"gd_lab{h}", [128, 128]) for h in range(4)]
                DT = [T(f"gd_dt{h}", [128, 128]) for h in range(4)]
                DB = [T(f"gd_db{h}", [128, 128]) for h in range(4)]
                Nm = [T(f"gd_n{h}", [128, 128]) for h in range(4)]
                NTm = [T(f"gd_nt{h}", [128, 128]) for h in range(4)]
                attnT = [T(f"gd_at{h}", [128, 128]) for h in range(4)]
                Z = T("gd_Z", [128, 512])
                o1 = T("gd_o1", [128, 512])
                o = T("gd_o", [128, 512])
                for c in range(NCH):
                    cs = slice(c * C, (c + 1) * C)
                    if GDSTOP <= 1:
                        continue
                    ps = PS()
                    proj_tm(Wba, 0, 8, c, ps)
                    ba = T("gd_ba", [128, 8])
                    Acp(ba[:], ps[:, 0:8])
                    A(beta[:], ba[:, 0:4], AF.Sigmoid)
                    A(lnb[:], beta[:], AF.Ln)
                    dp = decay_prep(ba[:, 4:8], 4, RPv("gdndtb"), RPv("gdnA"), "gd")
                    Vtt(gb[:], dp["g"][:], lnb[:], ALU.add)
                    Vts(neg[:], dp["eg"][:], -1.0, ALU.mult)
                    ps = PS()
                    for h in range(4):
                        tp(ps[:, h * 128:(h + 1) * 128], qkv[:, 8 + h, cs], ident)
                    Acp(v_tm[:], ps[:, 0:512])
                    pk_ = PS()
                    for h in range(4):
                        tp(pk_[:, h * 128:(h + 1) * 128], qkv[:, 4 + h, cs], ident)
                    pk = T("gd_ktm", [128, 512])
                    Vcp(pk[:], pk_[:, 0:512])
                    for h in range(4):
                        kT = qkv[:, 4 + h, cs]
                        qT = qkv[:, h, cs]
                        Vcp(LAb[h][:], dp["la"][:, h:h + 1].to_broadcast([128, 128]))
                        pd = PS()
                        mm(pd[:, 0:128], LAb[h][:], K("iu"), start=True, stop=False)
                        mm(pd[:, 0:128], ident, K("negt"), start=False, stop=True)
                        mm(pd[:, 128:256], LAb[h][:], K("niu"), start=True, stop=False)
                        mm(pd[:, 128:256], ident, K("negs"), start=False, stop=True)
                        A(DT[h][:], pd[:, 0:128], AF.Exp, bias=dp["ng"][:, h:h + 1])
                        A(egl[:, h:h + 1], pd[:, 127:128], AF.Exp)
                        A(DB[h][:], pd[:, 128:256], AF.Exp, bias=gb[:, h:h + 1])
                        Vts(ktail[:, h * 128:(h + 1) * 128], pk[:, h * 128:(h + 1) * 128], DT[h][:, 127:128], ALU.mult)
                        pq = PS()
                        mm(pq[:, 0:128], kT, kT)
                        mm(pq[:, 128:256], kT, qT)
                        mm(pq[:, 256:384], kT, st_gd[h][:])
                        Vstt(Nm[h][:], pq[:, 0:128], -1.0, DB[h][:], ALU.mult, ALU.mult)
                        Vtt(attnT[h][:], pq[:, 128:256], DT[h][:], ALU.mult)
                        pt = PS()
                        tp(pt[:, 0:128], Nm[h][:], ident)
                        Acp(NTm[h][:], pt[:, 0:128])
                        Vstt(Z[:, h * 128:(h + 1) * 128], pq[:, 256:384], neg[:, h:h + 1], v_tm[:, h * 128:(h + 1) * 128], ALU.mult, ALU.add)
                        Vts(Z[:, h * 128:(h + 1) * 128], Z[:, h * 128:(h + 1) * 128], beta[:, h:h + 1], ALU.mult)
                    if GDSTOP <= 2:
                        continue
                    neumann_apply(Nm, NTm, Z, 4, 128, "gd")
                    if GDSTOP <= 3:
                        continue
                    po1 = PS()
                    po2 = PS()
                    for h in range(4):
                        mm(po1[:, h * 128:(h + 1) * 128], qkv[:, h, cs], st_gd[h][:])
                        mm(po2[:, h * 128:(h + 1) * 128], attnT[h][:], Z[:, h * 128:(h + 1) * 128])
                    Vtt(o1[:].rearrange("p (h d) -> p h d", h=4), po1[:, 0:512].rearrange("p (h d) -> p h d", h=4),
                        dp["eg"][:, 0:4].unsqueeze(2).to_broadcast([128, 4, 128]), ALU.mult)
                    Vtt(o[:], o1[:], po2[:, 0:512], ALU.add)
                    o3 = o[:].rearrange("p (h d) -> p h d", h=4)
                    rstd = head_rms(o3, 4, 128, 1e-6, "gd")
                    Vtt(o3, o3, rstd[:, 0:4].unsqueeze(2).to_broadcast([128, 4, 128]), ALU.mult)
                    Vtt(o3, o3, RPv("gdnnw").unsqueeze(1).to_broadcast([128, 4, 128]), ALU.mult)
                    Vtt(o[:], o[:], zs[c][:], ALU.mult)
                    tm_to_fm_bf16(o, 12, c)
                    for h in range(4):
                        ps = PS()
                        mm(ps[:, 0:128], ktail[:, h * 128:(h + 1) * 128], Z[:, h * 128:(h + 1) * 128])
                        Vstt(st_gd[h][:], st_gd[h][:], egl[:, h:h + 1], ps[:, 0:128], ALU.mult, ALU.add)
                scopes.pop()

        def mixer_rwkv(l, mt):
            S_.barrier()
            with contextlib.ExitStack() as sc:
                scopes.append(sc)
                rkv = T("rw_rkv", [128, 12, NT])
                wam = T("rw_wam", [128, NT])
                rbuf = T("rw_rbuf", [128, NT + 1])
                dtl = T("rw_d", [128, NT])

                def shift_block(ps, idx, out_ap):
                    Acp(rbuf[:, 0:1], carry_rw[:, idx:idx + 1])
                    Acp(rbuf[:, 1:NT + 1], ps[:, 0:NT])
                    Vtt(dtl[:], rbuf[:, 0:NT], rbuf[:, 1:NT + 1], ALU.subtract)
                    Vstt(out_ap, dtl[:], pp[:, PP["mu"] + idx:PP["mu"] + idx + 1], rbuf[:, 1:NT + 1], ALU.mult, ALU.add)
                    Acp(carry_rw[:, idx:idx + 1], rbuf[:, NT:NT + 1])
                for j in range(3):
                    Wj = W(("in", l, O_RW + 512 * j, 512))
                    for b in range(4):
                        ps = PS()
                        proj_fm(Wj, b * 128, 128, ps)
                        shift_block(ps, 4 * j + b, rkv[:, 4 * j + b, :])
                Wwa = W(("in", l, O_RW + 1536, 128))
                ps = PS()
                proj_fm(Wwa, 0, 128, ps)
                shift_block(ps, 12, wam[:])
                A(wam[0:64, :], wam[0:64, :], AF.Tanh)
                Wz = W(("in", l, O_RW + 1664, 512))
                zs = [T(f"rw_z{c}", [128, 512]) for c in range(NCH)]
                for c in range(NCH):
                    ps = PS()
                    proj_tm(Wz, 0, 512, c, ps)
                    A(zs[c][:], ps[:, 0:512], AF.Silu)
                iclr = T("rw_iclr", [128, 4, NT])
                kk = T("rw_kk", [128, 4, NT])
                sq = T("rw_sq", [128, NT])
                rkr = T("rw_rkr", [128, 4, NT])
                for b in range(4):
                    ps = PS()
                    mm(ps[:, 0:NT], wau[64:128, b * 128:(b + 1) * 128], wam[64:128, :])
                    A(iclr[:, b, :], ps[:, 0:NT], AF.Sigmoid, bias=pp[:, PP["a0"] + b:PP["a0"] + b + 1])
                    km = rkv[:, 4 + b, :]
                    Vts(kk[:, b, :], km, pp[:, PP["kk"] + b:PP["kk"] + b + 1], ALU.mult)
                    Vtt(sq[:], kk[:, b, :], kk[:, b, :], ALU.mult)
                    ps2 = PS()
                    mm(ps2[:, 0:NT], K("bones"), sq[:])
                    rsqrt_(sq[:], ps2[:, 0:NT], 1.0, 1e-6)
                    Vtt(kk[:, b, :], kk[:, b, :], sq[:], ALU.mult)
                    Vts(sq[:], iclr[:, b, :], -1.0, ALU.add, pp[:, PP["ka"] + b:PP["ka"] + b + 1], ALU.mult)
                    Vstt(km, sq[:], 1.0, km, ALU.add, ALU.mult)
                    Vtt(iclr[:, b, :], iclr[:, b, :], kk[:, b, :], ALU.mult)
                    Vstt(rkr[:, b, :], rkv[:, b, :], pp[:, PP["rk"] + b:PP["rk"] + b + 1], km, ALU.mult, ALU.mult)
                bm = iclr
                lw = T("rw_lw", [128, 512])
                Gx = T("rw_G", [128, 4, 132])
                eG = T("rw_eG", [128, 4, 128])
                enG = T("rw_enG", [128, 4, 128])
                lwf = T("rw_lwf", [128, 4, 128])
                AR = T("rw_AR", [128, 4, 256])
                kp = T("rw_kp", [128, 4, 128])
                bp = T("rw_bp", [128, 4, 128])
                sc1 = T("rw_sc1", [128, 4])
                sc2 = T("rw_sc2", [128, 4])
                sc3 = T("rw_sc3", [128, 4])
                A0s = [T(f"rw_a0s{b}", [128, 128]) for b in range(4)]
                kpT = T("rw_kpT", [128, 512])
                bpT = T("rw_bpT", [128, 512])
                v_tm = T("rw_v", [128, 512])
                SC = [T(f"rw_SC{h}", [128, 512]) for h in range(8)]
                Nm = [T(f"rw_N{h}", [128, 128]) for h in range(8)]
                NTm = [T(f"rw_NT{h}", [128, 128]) for h in range(8)]
                Z = T("rw_Z", [128, 512])
                y = T("rw_y", [128, 512])
                bon = T("rw_bon", [128, 8])
                mv = T("rw_mv", [128, 8])
                tmpb = T("rw_tmpb", [128, 128])
                for c in range(NCH):
                    cs = slice(c * C, (c + 1) * C)
                    ps = PS()
                    mm(ps[:, 0:512], wam[0:64, cs], wau[0:64, :])
                    Vtt(lw[:], ps[:, 0:512], RPv("w0"), ALU.add)
                    A(lw[:], lw[:], AF.Sigmoid)
                    Vts(lw[:], lw[:], float(-np.exp(-0.5)), ALU.mult)
                    for b in range(4):
                        ps = PS()
                        mm(ps[:, 0:132], lw[:, b * 128:(b + 1) * 128], K("tmat"))
                        Acp(Gx[:, b, :], ps[:, 0:132])
                        ps2 = PS()
                        tp(ps2[:, 0:128], lw[:, b * 128:(b + 1) * 128], ident)
                        Vcp(lwf[:, b, :], ps2[:, 0:128])
                    A(eG[:], Gx[:, :, 0:128], AF.Exp)
                    A(enG[:], Gx[:, :, 0:128], AF.Exp, scale=-1.0)
                    A(sc1[:], Gx[:, :, 128], AF.Exp, scale=-1.0)
                    A(sc2[:], Gx[:, :, 127], AF.Exp)
                    Vtt(sc3[:], sc1[:], sc2[:], ALU.mult)
                    Vtt(AR[:, :, 128:256], rkv[:, 0:4, cs], eG[:], ALU.mult)
                    Vtt(kp[:], rkv[:, 4:8, cs], enG[:], ALU.mult)
                    Vtt(bp[:], bm[:, :, cs], enG[:], ALU.mult)
                    A(lwf[:], lwf[:], AF.Exp, scale=-1.0)
                    Vtt(lwf[:], lwf[:], eG[:], ALU.mult)
                    Vstt(AR[:, :, 0:128], kk[:, :, cs], -1.0, lwf[:], ALU.mult, ALU.mult)
                    for b in range(4):
                        Vts(A0s[b][:], st_rw[b][:], sc1[:, b:b + 1], ALU.mult)
                    ps = PS()
                    ps2 = PS()
                    ps3 = PS()
                    for b in range(4):
                        tp(ps[:, b * 128:(b + 1) * 128], kp[:, b, :], ident)
                        tp(ps2[:, b * 128:(b + 1) * 128], bp[:, b, :], ident)
                        tp(ps3[:, b * 128:(b + 1) * 128], rkv[:, 8 + b, cs], ident)
                    Acp(kpT[:], ps[:, 0:512])
                    Vcp(bpT[:], ps2[:, 0:512])
                    Acp(v_tm[:], ps3[:, 0:512])
                    psb = PS()
                    for b in range(4):
                        mm(psb[:, 2 * b:2 * b + 2], rkr[:, b, cs], K("sel2")[:, 0:2])
                    Acp(bon[:], psb[:, 0:8])
                    for h in range(8):
                        b, r0 = h // 2, 64 * (h % 2)
                        rows = slice(r0, r0 + 64)
                        pa = PS()
                        mm(pa[:, 0:256], bp[rows, b, :], AR[rows, b, :])
                        mm(pa[:, 256:512], kp[rows, b, :], AR[rows, b, :])
                        Vtt(SC[h][:], pa[:, 0:512], K("maskA"), ALU.mult)
                        pn = PS()
                        mm(pn[:, 0:128], AR[rows, b, 0:128], bp[rows, b, :])
                        Vtt(Nm[h][:], pn[:, 0:128], K("sl"), ALU.mult)
                        Acp(NTm[h][:], SC[h][:, 0:128])
                    pz = PS()
                    for b in range(4):
                        mm(pz[:, b * 128:(b + 1) * 128], AR[:, b, 0:128], A0s[b][:], start=True, stop=False)
                        for hh in range(2):
                            h = 2 * b + hh
                            mm(pz[:, h * 64:(h + 1) * 64], SC[h][:, 256:384], v_tm[:, h * 64:(h + 1) * 64], start=False, stop=(hh == 1))
                    Acp(Z[:], pz[:, 0:512])
                    import os
                    if os.environ.get("RWDBG", "") == "pv":
                        pvs = T("rw_pvs", [128, 512])
                        Vcp(pvs[:], Z[:])
                    neumann_apply(Nm, NTm, Z, 8, 64, "rw")
                    py = PS()
                    for b in range(4):
                        mm(py[:, b * 128:(b + 1) * 128], AR[:, b, 128:256], A0s[b][:], start=True, stop=False)
                        for hh in range(2):
                            h = 2 * b + hh
                            mm(py[:, h * 64:(h + 1) * 64], SC[h][:, 384:512], v_tm[:, h * 64:(h + 1) * 64], start=False, stop=False)
                            mm(py[:, h * 64:(h + 1) * 64], SC[h][:, 128:256], Z[:, h * 64:(h + 1) * 64], start=False, stop=(hh == 1))
                    Acp(y[:], py[:, 0:512])
                    for b in range(4):
                        ps = PS()
                        mm(ps[:, 0:128], kpT[:, b * 128:(b + 1) * 128], v_tm[:, b * 128:(b + 1) * 128], start=True, stop=False)
                        mm(ps[:, 0:128], bpT[:, b * 128:(b + 1) * 128], Z[:, b * 128:(b + 1) * 128], start=False, stop=True)
                        Vstt(tmpb[:], ps[:, 0:128], sc2[:, b:b + 1], K("bones"), ALU.mult, ALU.mult)
                        Vstt(st_rw[b][:], st_rw[b][:], sc3[:, b:b + 1], tmpb[:], ALU.mult, ALU.add)
                    y3 = y[:].rearrange("p (h d) -> p h d", h=8)
                    Vred(mv[:], y3)
                    Vts(mv[:], mv[:], float(-1.0 / 64), ALU.mult)
                    Vtt(y3, y3, mv[:, 0:8].unsqueeze(2).to_broadcast([128, 8, 64]), ALU.add)
                    rstd = head_rms(y3, 8, 64, 64e-5, "rw", sq=kpT)
                    Vtt(y3, y3, rstd[:, 0:8].unsqueeze(2).to_broadcast([128, 8, 64]), ALU.mult)
                    Vtt(y[:], y[:], RPv("lnw"), ALU.mult)
                    Vtt(y[:], y[:], RPv("lnb"), ALU.add)
                    v3 = v_tm[:].rearrange("p (h d) -> p h d", h=8)
                    Vtt(Z[:].rearrange("p (h d) -> p h d", h=8), v3, bon[:, 0:8].unsqueeze(2).to_broadcast([128, 8, 64]), ALU.mult) if False else None
                    bz = bpT
                    Vtt(bz[:].rearrange("p (h d) -> p h d", h=8), v3, bon[:, 0:8].unsqueeze(2).to_broadcast([128, 8, 64]), ALU.mult)
                    Vtt(y[:], y[:], bz[:], ALU.add)
                    Vtt(y[:], y[:], zs[c][:], ALU.mult)
                    import os
                    dsel = os.environ.get("RWDBG", "")
                    if dsel == "py":
                        Acp(y[:], py[:, 0:512])
                    elif dsel == "z":
                        Vcp(y[:], Z[:])
                    elif dsel == "v":
                        Vcp(y[:], v_tm[:])
                    elif dsel == "lw":
                        Vcp(y[:], lw[:])
                    elif dsel == "kpT":
                        Vcp(y[:], kpT[:])
                    elif dsel == "pv":
                        Vcp(y[:], pvs[:])
                    elif dsel == "bon":
                        Vcp(y[:], bz[:])
                    tm_to_fm_bf16(y, 0, c)
                scopes.pop()

        def merge_out(l, mt, xsrc, xdst, is_last):
            S_.barrier()
            with contextlib.ExitStack() as sc:
                scopes.append(sc)
                macc = T("mg_acc", [128, 8, NT])
                mT = T("mg_mT", [128, 8, NT], BF16)
                sg = T("mg_sg", [128, NT])
                tt_ = T("mg_t", [128, NT])
                for br in range(4):
                    Wb = W(("br", l, br, 0))
                    Wg = [None, None]
                    for half in range(2):
                        Wg[half] = W(("in", l, O_GATE + br * 1024 + 512 * half, 512), live=1 + half)
                        for d4 in range(4):
                            dmb = half * 4 + d4
                            pg = PS()
                            proj_fm(Wg[half], d4 * 128, 128, pg)
                            A(sg[:], pg[:, 0:NT], AF.Sigmoid)
                            pb_ = PS()
                            for kc in range(4):
                                mm(pb_[:, 0:NT], Wb[:, kc, dmb * 128:(dmb + 1) * 128], u_all[:, br * 4 + kc, :], start=(kc == 0), stop=(kc == 3))
                            if br == 0:
                                Vtt(macc[:, dmb, :], sg[:], pb_[:, 0:NT], ALU.mult)
                            elif br < 3:
                                Vtt(tt_[:], sg[:], pb_[:, 0:NT], ALU.mult)
                                Vtt(macc[:, dmb, :], macc[:, dmb, :], tt_[:], ALU.add)
                            else:
                                Vtt(tt_[:], sg[:], pb_[:, 0:NT], ALU.mult)
                                Vtt(mT[:, dmb, :], macc[:, dmb, :], tt_[:], ALU.add)
                for half in range(2):
                    Wo = W(("out", l, 512 * half, 512))
                    for c in range(NCH):
                        ps = PS()
                        for kc in range(8):
                            mm(ps[:, 0:512], mT[:, kc, c * C:(c + 1) * C], Wo[:, kc, :], start=(kc == 0), stop=(kc == 7))
                        Vtt(xt[:, c, half * 512:(half + 1) * 512], xt[:, c, half * 512:(half + 1) * 512], ps[:, 0:512], ALU.add)
                if is_last and final_norm:
                    junk = T("fn_junk", [128, D])
                    ssf = T("fn_ss", [128, NCH])
                    for c in range(NCH):
                        A(junk[:], xt[:, c, :], AF.Square, accum=ssf[:, c:c + 1])
                    rsqrt_(ssf[:], ssf[:], 1.0 / D, 1e-6)
                    for c in range(NCH):
                        Vstt(xt[:, c, :], xt[:, c, :], ssf[:, c:c + 1], finw[:], ALU.mult, ALU.mult)
                rows = slice(mt * NT, (mt + 1) * NT)
                dma("sp", xdst[rows, :].rearrange("(c p) d -> p c d", p=128), xt[:], sem_o, ins=[xt], outs=[("xd", id(xdst), mt)])
                scopes.pop()

        nl = len(layers)
        for li, l in enumerate(layers):
            load_params(l)
            xsrc = dr["x"] if li == 0 else scr[(li - 1) % 2]
            is_last = li == nl - 1
            xdst = out if is_last else scr[li % 2]
            for mt in range(NMT):
                rows = slice(mt * NT, (mt + 1) * NT)
                dma("sp", xt[:], xsrc[rows, :].rearrange("(c p) d -> p c d", p=128), sem_x,
                    ins=[("xd", id(xsrc), mt)], outs=[xt])
                dma("sp", ropet[:], dr["rope"][:, :, rows], sem_r, outs=[ropet])
                with contextlib.ExitStack() as sc:
                    scopes.append(sc)
                    S_.barrier()
                    junk = T("n_junk", [128, D])
                    ss = T("n_ss", [128, NCH])
                    hb = T("n_hb", [128, D], BF16)
                    for c in range(NCH):
                        A(junk[:], xt[:, c, :], AF.Square, accum=ss[:, c:c + 1])
                    rsqrt_(ss[:], ss[:], 1.0 / D, 1e-6)
                    for c in range(NCH):
                        Vstt(hb[:], xt[:, c, :], ss[:, c:c + 1], normw[:], ALU.mult, ALU.mult)
                        for kc in range(8):
                            tp(pbt[:, kc * 128:(kc + 1) * 128], hb[:, kc * 128:(kc + 1) * 128], identb[:])
                        CP(hT[:, :, c * C:(c + 1) * C], pbt[:, 0:1024].rearrange("p (k t) -> p k t", k=8))
                    scopes.pop()
                if "rwkv" in mixers:
                    mixer_rwkv(l, mt)
                if "ret" in mixers:
                    mixer_ret(l, mt)
                if "ssd" in mixers:
                    mixer_ssd(l, mt)
                if "gdn" in mixers:
                    mixer_gdn(l, mt)
                if dbg and is_last:
                    dma("sp", dbg_u[:, rows].rearrange("(b p) t -> p b t", p=128), u_all[:], sem_d, ins=[u_all], outs=[("dbg", mt)])
                merge_out(l, mt, xsrc, xdst, is_last)
        fin_ins = [("xd", id(out), mt) for mt in range(NMT)]
        if dbg:
            fin_ins += [("dbg", mt) for mt in range(NMT)]
        add("sp", lambda e: e.nop(), ins=fin_ins)
        assert jstate["used"] == len(jobs)
        S_.emit()
        build.stats = S_.stats
    return nc


NT_DEFAULT = 256


def kernel(**inputs):
    x = np.ascontiguousarray(np.asarray(inputs["x"], dtype=np.float32))
    B, S, _ = x.shape
    cst, rope = make_consts(S)
    params = {n: np.ascontiguousarray(np.asarray(inputs[n], dtype=np.float32)) for n in PARAM_NAMES}
    nc = build(S, list(range(DEPTH)), NT_DEFAULT, True)
    in_maps = []
    for b in range(B):
        m = {"x": x[b], "cst": cst, "rope": rope}
        m.update(params)
        in_maps.append(m)
    res = run_bass_kernel_spmd(nc, in_maps, core_ids=list(range(B)))
    return np.stack([np.asarray(r["out"], dtype=np.float32) for r in res.results], axis=0)
```

```python
import contextlib
import numpy as np
import concourse.bass as bass
import concourse.mybir as mybir
from concourse.bass_utils import run_bass_kernel_spmd

F32 = mybir.dt.float32
F32R = mybir.dt.float32r
BF16 = mybir.dt.bfloat16
ALU = mybir.AluOpType
AF = mybir.ActivationFunctionType
AX = mybir.AxisListType

D = 1024
NIN = 11408
DEPTH = 4
SEQ = 4096
BATCH = 8
C = 128
O_RW, O_RET, O_SSD, O_GDN, O_GATE = 0, 2176, 3712, 5256, 7312


class Op:
    __slots__ = ("eng", "fn", "deps", "needed", "dma_sem", "token", "group", "is_dma")


class Group:
    def __init__(self):
        self.n = 0
        self.token = None


class Sched:
    ROT = 3500
    DROT = 3488

    def __init__(self, nc, stack, same_sync=("act", "dve", "pool")):
        self.nc = nc
        self.stack = stack
        self.ops = []
        self.w = {}
        self.r = {}
        self.same_sync = set(same_sync)
        self.nsem = 0

    def new_sem(self, name):
        self.nsem += 1
        return self.stack.enter_context(self.nc.semaphore(f"{name}_{self.nsem}"))

    @staticmethod
    def key(a):
        if isinstance(a, (tuple, str)):
            return a
        return a.name

    def add(self, eng, fn, ins=(), outs=(), dma_sem=None, group=None):
        op = Op()
        op.eng = eng
        op.fn = fn
        op.needed = False
        op.dma_sem = dma_sem
        op.is_dma = dma_sem is not None
        op.group = group
        op.token = None
        if group is not None:
            group.n += 1
        ikeys = [self.key(a) for a in ins if a is not None]
        okeys = [self.key(a) for a in outs if a is not None]
        ikeys.append("EPOCH")
        deps = []
        for k in ikeys:
            deps += self.w.get(k, [])
        for k in okeys:
            deps += self.w.get(k, [])
            rd = self.r.get(k)
            if rd:
                deps += list(rd[0].values())
                deps += rd[1]
        fdeps = []
        seen = set()
        for d in deps:
            if id(d) in seen or d is op:
                continue
            seen.add(id(d))
            if group is not None and d.group is group:
                continue
            if (not d.is_dma) and d.eng == eng and eng not in self.same_sync:
                continue
            d.needed = True
            fdeps.append(d)
        op.deps = fdeps
        for k in ikeys:
            rd = self.r.setdefault(k, [{}, []])
            if op.is_dma:
                rd[1].append(op)
            else:
                rd[0][eng] = op
        for k in okeys:
            cur = self.w.get(k, [])
            if group is not None and cur and all(c.group is group for c in cur):
                cur.append(op)
                self.w[k] = cur
            else:
                self.w[k] = [op]
            self.r[k] = [{}, []]
        self.ops.append(op)
        return op

    def barrier(self):
        self.add("dve", lambda e: e.engine_nop(), outs=["EPOCH"])

    def emit(self):
        nc = self.nc
        cnt, cursem, dcount, waited, dsem = {}, {}, {}, {}, {}
        per_eng = {e: [] for e in ("pe", "act", "dve", "pool", "sp")}
        nwaits = 0
        for op in self.ops:
            waits = {}
            for d in op.deps:
                sem, val = d.token
                k = id(sem)
                if k not in waits or waits[k][1] < val:
                    waits[k] = (sem, val)
            wl = []
            for k, (sem, val) in waits.items():
                wk = (op.eng, k)
                if waited.get(wk, 0) >= val:
                    continue
                waited[wk] = val
                wl.append((sem, val))
            nwaits += len(wl)
            inc = None
            if op.is_dma:
                fam = id(op.dma_sem)
                g = op.group
                need = 16 * (g.n if (g is not None and g.token is None) else 1)
                if fam not in dsem:
                    dsem[fam] = op.dma_sem
                    dcount[fam] = 0
                if (g is None or g.token is None) and dcount[fam] + need > self.DROT:
                    dsem[fam] = self.new_sem("dr")
                    dcount[fam] = 0
                sem = dsem[fam]
                if g is not None:
                    if g.token is None:
                        g.token = (sem, dcount[fam] + 16 * g.n)
                    sem = g.token[0]
                    dcount[fam] += 16
                    op.token = g.token
                else:
                    dcount[fam] += 16
                    op.token = (sem, dcount[fam])
                inc = (sem, 16)
            elif op.needed:
                e = op.eng
                if e not in cursem or cnt[e] >= self.ROT:
                    cursem[e] = self.new_sem("e" + e)
                    cnt[e] = 0
                cnt[e] += 1
                op.token = (cursem[e], cnt[e])
                inc = (cursem[e], 1)
            per_eng[op.eng].append((wl, op.fn, inc))
            op.deps = None
        self.stats = dict(n_ops=len(self.ops), n_waits=nwaits, n_sems=self.nsem,
                          per_eng={e: len(v) for e, v in per_eng.items()})

        def run(e, lst):
            for wl, fn, inc in lst:
                for sem, val in wl:
                    e.wait_ge(sem, val)
                ins = fn(e)
                if inc is not None:
                    ins.then_inc(inc[0], inc[1])

        with nc.Block() as block:
            @block.tensor
            def _(e):
                run(e, per_eng["pe"])

            @block.scalar
            def _(e):
                run(e, per_eng["act"])

            @block.vector
            def _(e):
                run(e, per_eng["dve"])

            @block.gpsimd
            def _(e):
                run(e, per_eng["pool"])

            @block.sync
            def _(e):
                run(e, per_eng["sp"])


CST = {}


def _cst_layout():
    off = 0
    for name, n in [("ident", 128), ("maskA", 512), ("iu", 128), ("niu", 128), ("negt", 128),
                    ("negs", 128), ("sl", 128), ("tmat", 132), ("ones", 128), ("bones", 128),
                    ("sel2", 8), ("swapp", 128), ("dm2", 1152), ("qdec", 384), ("ktbl", 8),
                    ("tblc", 768), ("mask01", 256)]:
        CST[name] = (off, n)
        off += n
    return off


NCST = _cst_layout()


def make_consts(S):
    c = np.zeros((128, NCST), np.float32)

    def put(name, arr):
        o, n = CST[name]
        c[:, o:o + arr.shape[1]] = arr

    i = np.arange(128)
    su = (i[:, None] < i[None, :]).astype(np.float32)
    iu = (i[:, None] <= i[None, :]).astype(np.float32)
    put("ident", np.eye(128, dtype=np.float32))
    put("maskA", np.concatenate([su, iu, su, iu], axis=1))
    put("iu", iu)
    put("niu", -iu)
    put("negt", np.where(i[:, None] <= i[None, :], 0.0, -1e30).astype(np.float32))
    put("negs", np.where(i[None, :] < i[:, None], 0.0, -1e30).astype(np.float32))
    put("sl", su.T.copy())
    tm = np.zeros((128, 132), np.float32)
    m = 63
    tm[:, :128] = iu - (i[:, None] <= m).astype(np.float32)
    tm[:, 128] = -(i <= m).astype(np.float32)
    put("tmat", tm)
    put("ones", np.ones((128, 128), np.float32))
    bo = np.zeros((128, 128), np.float32)
    bo[:64, :64] = 1
    bo[64:, 64:] = 1
    put("bones", bo)
    s2 = np.zeros((128, 8), np.float32)
    s2[:64, 0] = 1
    s2[64:, 1] = 1
    put("sel2", s2)
    sp = np.zeros((128, 128), np.float32)
    sp[i, i ^ 1] = 1
    put("swapp", sp)
    gam = 1.0 - np.exp2(-5.0 - np.arange(8, dtype=np.float64))
    lg = np.log(gam)
    scale = 32 ** -0.5
    dm2 = np.zeros((128, 3, 3, 128), np.float64)
    for h in range(8):
        r, bq = h % 3, h // 3
        dm2[:, r, bq, :] = np.where(i[:, None] <= i[None, :], np.exp(lg[h] * (i[None, :] - i[:, None])), 0.0) * scale
    put("dm2", dm2.reshape(128, 1152).astype(np.float32))
    qd = np.ones((128, 3, 128), np.float64)
    for bq in range(3):
        for p in range(96):
            h = bq * 3 + p // 32
            if h < 8:
                qd[p, bq, :] = np.exp(lg[h] * (i + 1))
    put("qdec", qd.reshape(128, 384).astype(np.float32))
    kt = np.zeros((128, 8), np.float64)
    for h in range(8):
        kt[:, h] = np.exp(lg[h] * (127 - i)) * scale
    put("ktbl", kt.astype(np.float32))
    tc_ = np.zeros((128, 3, 256), np.float64)
    m01 = np.zeros((128, 256), np.float32)
    for hh in range(4):
        m01[hh * 32:(hh + 1) * 32, hh * 64:(hh + 1) * 64] = 1
    for bq in range(3):
        for hh in range(3):
            h = bq * 3 + hh
            if h < 8:
                tc_[hh * 32:(hh + 1) * 32, bq, hh * 64:(hh + 1) * 64] = np.exp(lg[h] * 128)
    put("tblc", tc_.reshape(128, 768).astype(np.float32))
    put("mask01", m01)
    half = 16
    angle = 1.0 / (10000.0 ** np.linspace(0.0, 1.0, half, dtype=np.float32)).astype(np.float32)
    theta = np.arange(S, dtype=np.float32)[:, None] * angle[None, :]
    cos = np.cos(theta).astype(np.float32)
    sin = np.sin(theta).astype(np.float32)
    rope = np.zeros((128, 2, S), np.float32)
    for p in range(128):
        ii = (p % 32) // 2
        rope[p, 0, :] = cos[:, ii]
        rope[p, 1, :] = (-sin[:, ii]) if (p % 2 == 0) else sin[:, ii]
    return c, rope


PARAM_NAMES = ["norm_w", "w_in", "rwkv_mu_rkv", "rwkv_mu_wa", "rwkv_w_up", "rwkv_w0", "rwkv_a_up", "rwkv_a0",
               "rwkv_k_k", "rwkv_k_a", "rwkv_r_k", "rwkv_ln_w", "rwkv_ln_b", "ret_norm_w", "ssd_conv_w",
               "ssd_conv_b", "ssd_dt_bias", "ssd_A_log", "ssd_D", "ssd_norm_w", "gdn_conv_w", "gdn_dt_bias",
               "gdn_A_log", "gdn_norm_w", "w_branch", "w_out", "final_norm_w"]
PARAM_SHAPES = {
    "norm_w": [4, 1024], "w_in": [4, 1024, NIN], "rwkv_mu_rkv": [4, 3, 512], "rwkv_mu_wa": [4, 2, 64],
    "rwkv_w_up": [4, 64, 512], "rwkv_w0": [4, 512], "rwkv_a_up": [4, 64, 512], "rwkv_a0": [4, 512],
    "rwkv_k_k": [4, 512], "rwkv_k_a": [4, 512], "rwkv_r_k": [4, 8, 64], "rwkv_ln_w": [4, 512],
    "rwkv_ln_b": [4, 512], "ret_norm_w": [4, 512], "ssd_conv_w": [4, 4, 1024], "ssd_conv_b": [4, 1024],
    "ssd_dt_bias": [4, 8], "ssd_A_log": [4, 8], "ssd_D": [4, 8], "ssd_norm_w": [4, 512],
    "gdn_conv_w": [4, 4, 1536], "gdn_dt_bias": [4, 4], "gdn_A_log": [4, 4], "gdn_norm_w": [4, 128],
    "w_branch": [4, 4, 512, 1024], "w_out": [4, 1024, 1024], "final_norm_w": [1024],
}

RP = {}


def _rp_layout():
    off = 0
    for name, n in [("w0", 512), ("lnw", 512), ("lnb", 512), ("retnw", 512), ("ssdnw", 512), ("gdnnw", 128),
                    ("ssdD", 8), ("ssddtb", 8), ("ssdA", 8), ("gdndtb", 4), ("gdnA", 4)]:
        RP[name] = (off, n)
        off += n
    return off


NRP = _rp_layout()
PP = {"mu": 0, "kk": 13, "ka": 17, "rk": 21, "a0": 25, "scw": 29, "scb": 61, "gcw": 69}
NPP = 128


def build(S, layers, NT, final_norm, mixers=("rwkv", "ret", "ssd", "gdn"), dbg=False, same_sync=True,
          nslot=3, n_param_layers=DEPTH, lmap=None):
    lmap = lmap or {l: l for l in range(DEPTH)}
    NCH = NT // C
    NMT = S // NT
    nc = bass.Bass("TRN2", target_bir_lowering=False)
    dr = {}
    dr["x"] = nc.dram_tensor("x", [S, D], F32, kind="ExternalInput").ap()
    for n in PARAM_NAMES:
        shp = list(PARAM_SHAPES[n])
        if n != "final_norm_w":
            shp[0] = n_param_layers
        dr[n] = nc.dram_tensor(n, shp, F32, kind="ExternalInput").ap()
    dr["cst"] = nc.dram_tensor("cst", [128, NCST], F32, kind="ExternalInput").ap()
    dr["rope"] = nc.dram_tensor("rope", [128, 2, S], F32, kind="ExternalInput").ap()
    out = nc.dram_tensor("out", [S, D], F32, kind="ExternalOutput").ap()
    scr = [nc.dram_tensor(f"scr{i}", [S, D], F32, kind="Internal").ap() for i in range(2)] if len(layers) > 1 else []
    if dbg:
        dbg_u = nc.dram_tensor("dbg_u", [16 * 128, S], BF16, kind="ExternalOutput").ap()

    with contextlib.ExitStack() as st:
        S_ = Sched(nc, st, same_sync=("act", "dve", "pool") if same_sync else ())
        add = S_.add
        scopes = [st]
        tcount = [0]
        tcache = {}

        def T(name, shape, dt=F32):
            ck = (id(scopes[-1]), name)
            if ck in tcache:
                return tcache[ck]
            tcount[0] += 1
            t_ = scopes[-1].enter_context(nc.sbuf_tensor(f"s{tcount[0]}_{name}", shape, dt))
            tcache[ck] = t_
            return t_

        pbs = [st.enter_context(nc.psum_tensor(f"pb{i}", [128, 512], F32)) for i in range(7)]
        pbt = st.enter_context(nc.psum_tensor("pbt", [128, 1024], BF16))
        pstate = [0]

        def PS():
            p = pbs[pstate[0] % 7]
            pstate[0] += 1
            return p

        def mm(out_, lhsT, rhs, start=True, stop=True):
            add("pe", lambda e: e.matmul(out_, lhsT, rhs, start=start, stop=stop), ins=[lhsT, rhs], outs=[out_])

        def tp(out_, in_, ident):
            add("pe", lambda e: e.transpose(out_, in_, ident), ins=[in_, ident], outs=[out_])

        def A(out_, in_, func, bias=None, scale=None, accum=None):
            kw = {}
            ins = [in_]
            if bias is not None:
                kw["bias"] = bias
                if not isinstance(bias, float):
                    ins.append(bias)
            if scale is not None:
                kw["scale"] = scale
                if not isinstance(scale, float):
                    ins.append(scale)
            outs = [out_]
            if accum is not None:
                kw["accum_out"] = accum
                outs.append(accum)
            add("act", lambda e: e.activation(out_, in_, func, **kw), ins=ins, outs=outs)

        def Acp(out_, in_):
            add("act", lambda e: e.copy(out_, in_), ins=[in_], outs=[out_])

        def Vcp(out_, in_):
            add("dve", lambda e: e.tensor_copy(out_, in_), ins=[in_], outs=[out_])

        def Vtt(out_, a, b, op):
            add("dve", lambda e: e.tensor_tensor(out_, a, b, op), ins=[a, b], outs=[out_])

        def Vts(out_, a, s1, op0, s2=None, op1=None):
            ins = [a] + [s for s in (s1, s2) if s is not None and not isinstance(s, float)]
            if op1 is None:
                add("dve", lambda e: e.tensor_scalar(out_, a, s1, None, op0), ins=ins, outs=[out_])
            else:
                add("dve", lambda e: e.tensor_scalar(out_, a, s1, s2, op0, op1), ins=ins, outs=[out_])

        def Vstt(out_, in0, scalar, in1, op0, op1):
            ins = [in0, in1] + ([] if isinstance(scalar, float) else [scalar])
            add("dve", lambda e: e.scalar_tensor_tensor(out_, in0, scalar, in1, op0, op1), ins=ins, outs=[out_])

        def Vred(out_, in_):
            add("dve", lambda e: e.reduce_sum(out_, in_, AX.X), ins=[in_], outs=[out_])

        def Vrec(out_, in_):
            add("dve", lambda e: e.reciprocal(out_, in_), ins=[in_], outs=[out_])

        def Vset(out_, val):
            add("dve", lambda e: e.memset(out_, val), outs=[out_])

        def rsqrt_(out_, in_, mult, eps):
            Vts(out_, in_, mult, ALU.mult, eps, ALU.add)
            Vrec(out_, out_)
            A(out_, out_, AF.Sqrt)

        cpflip = [0]

        def CP(out_, in_):
            cpflip[0] ^= 1
            (Acp if cpflip[0] else Vcp)(out_, in_)

        def dma(eng, out_, in_, sem, ins=(), outs=(), group=None, slow=False):
            if slow:
                add(eng, lambda e: e.dma_start(out=out_, in_=in_, allow_slow_non_contiguous=True), ins=ins, outs=outs, dma_sem=sem, group=group)
            else:
                add(eng, lambda e: e.dma_start(out=out_, in_=in_), ins=ins, outs=outs, dma_sem=sem, group=group)

        cst = T("cst", [128, NCST])
        sem_c = S_.new_sem("cst")
        g0 = Group()
        dma("sp", cst[:, 0:NCST // 2], dr["cst"][:, 0:NCST // 2], sem_c, outs=[cst], group=g0)
        dma("sp", cst[:, NCST // 2:NCST], dr["cst"][:, NCST // 2:NCST], sem_c, outs=[cst], group=g0)

        def K(name, a=0, b=None):
            o, n = CST[name]
            if b is None:
                b = n
            return cst[:, o + a:o + b]

        ident = K("ident")
        identb = T("identb", [128, 128], BF16)
        Vcp(identb[:], ident)
        finw = T("finw", [128, D])
        sem_f = S_.new_sem("finw")
        if final_norm:
            dma("sp", finw[:], dr["final_norm_w"].partition_broadcast(128), sem_f, outs=[finw])

        xt = T("xt", [128, NCH, D])
        hT = T("hT", [128, 8, NT], BF16)
        u_all = T("u_all", [128, 16, NT], BF16)
        normw = T("normw", [128, D])
        pp = T("pp", [128, NPP])
        rp = T("rp", [128, NRP])
        wau = T("wau", [128, 512])
        ropet = T("ropet", [128, 2, NT])
        slots = [T(f"wslot{i}", [128, 4096], BF16) for i in range(nslot)]
        slot_sem = [S_.new_sem(f"ws{i}") for i in range(nslot)]
        sem_x = S_.new_sem("x")
        sem_o = S_.new_sem("o")
        sem_p = S_.new_sem("p")
        sem_r = S_.new_sem("rope")
        sem_d = S_.new_sem("dbg")
        carry_rw = T("carry_rw", [128, 16])
        carry_sd = T("carry_sd", [128, 8, 3])
        carry_gd = T("carry_gd", [128, 12, 3])
        st_rw = [T(f"st_rw{b}", [128, 128]) for b in range(4)]
        st_ret = [T(f"st_ret{b}", [128, 256]) for b in range(3)]
        st_sd = [T(f"st_sd{g}", [128, 256]) for g in range(2)]
        st_gd = [T(f"st_gd{h}", [128, 128]) for h in range(4)]

        if len(mixers) < 4:
            Vset(u_all[:], 0.0)

        def RPv(name, a=0, b=None):
            o, n = RP[name]
            if b is None:
                b = n
            return rp[:, o + a:o + b]

        jobs = []
        for l in layers:
            for mt in range(NMT):
                if "rwkv" in mixers:
                    jobs += [("in", l, O_RW + 0, 512), ("in", l, O_RW + 512, 512), ("in", l, O_RW + 1024, 512),
                             ("in", l, O_RW + 1536, 128), ("in", l, O_RW + 1664, 512)]
                if "ret" in mixers:
                    jobs += [("in", l, O_RET, 512), ("in", l, O_RET + 512, 512), ("in", l, O_RET + 1024, 512)]
                if "ssd" in mixers:
                    jobs += [("in", l, O_SSD, 512), ("in", l, O_SSD + 512, 512), ("in", l, O_SSD + 1024, 512),
                             ("in", l, O_SSD + 1536, 8)]
                if "gdn" in mixers:
                    jobs += [("in", l, O_GDN, 512), ("in", l, O_GDN + 512, 512), ("in", l, O_GDN + 1024, 512),
                             ("in", l, O_GDN + 1536, 512), ("in", l, O_GDN + 2048, 8)]
                for br in range(4):
                    jobs += [("br", l, br, 0), ("in", l, O_GATE + br * 1024, 512), ("in", l, O_GATE + br * 1024 + 512, 512)]
                jobs += [("out", l, 0, 512), ("out", l, 512, 512)]
        jstate = {"issued": 0, "used": 0}
        stg = [T(f"wstg{i}", [128, 2048]) for i in range(2)]
        stg_sem = [S_.new_sem(f"wstg{i}") for i in range(2)]

        def job_src(j, hh):
            kind, l, a, n = jobs[j]
            l = lmap[l]
            if kind == "in":
                src = dr["w_in"][l][:, a:a + n].rearrange("(k p) c -> p k c", p=128)
                return src[:, hh * 4:(hh + 1) * 4, :], 4, n
            if kind == "br":
                src = dr["w_branch"][l][a].rearrange("(k p) c -> p k c", p=128)
                return src[:, hh * 2:(hh + 1) * 2, :], 2, 1024
            src = dr["w_out"][l][:, a:a + n].rearrange("(k p) c -> p k c", p=128)
            return src[:, hh * 4:(hh + 1) * 4, :], 4, n

        def issue_dma(j):
            for hh in range(2):
                src, nk, n = job_src(j, hh)
                sv = stg[hh][:, 0:nk * n].rearrange("p (k c) -> p k c", k=nk)
                dma("sp", sv, src, stg_sem[hh], outs=[stg[hh]])

        def issue_cast(j):
            sl = slots[j % nslot]
            for hh in range(2):
                src, nk, n = job_src(j, hh)
                w_ = nk * n
                (Acp if hh == 0 else Vcp)(sl[:, hh * w_:(hh + 1) * w_], stg[hh][:, 0:w_])

        def W(desc, live=0):
            j = jstate["used"]
            assert jobs[j] == desc, (jobs[j], desc)
            assert live < nslot - 0
            if j == 0:
                issue_dma(0)
            issue_cast(j)
            if j + 1 < len(jobs):
                issue_dma(j + 1)
            jstate["used"] += 1
            kind, l, a, n = desc
            sl = slots[j % nslot]
            if kind == "br":
                return sl[:, 0:4096].rearrange("p (k c) -> p k c", k=4)
            return sl[:, 0:8 * n].rearrange("p (k c) -> p k c", k=8)

        def proj_fm(Wv, col0, nrows, ps, t0=0, nt=None):
            nt = NT if nt is None else nt
            for kc in range(8):
                mm(ps[:nrows, :nt], Wv[:, kc, col0:col0 + nrows], hT[:, kc, t0:t0 + nt], start=(kc == 0), stop=(kc == 7))

        def proj_tm(Wv, col0, ncols, c, ps):
            for kc in range(8):
                mm(ps[:, :ncols], hT[:, kc, c * C:(c + 1) * C], Wv[:, kc, col0:col0 + ncols], start=(kc == 0), stop=(kc == 7))

        def u_store(u_tm, blk0):
            raise NotImplementedError

        def tm_to_fm_bf16(src_tm, blk0, c):
            ps = PS()
            for b in range(4):
                tp(ps[:, b * 128:(b + 1) * 128], src_tm[:, b * 128:(b + 1) * 128], ident)
            CP(u_all[:, blk0:blk0 + 4, c * C:(c + 1) * C], ps[:, 0:512].rearrange("p (b t) -> p b t", b=4))

        def head_rms(y_ap3, nh, hd, eps, tagscope, sq=None):
            if sq is None:
                sq = T(f"sq_{tagscope}", [128, nh * hd])
            Vtt(sq[:].rearrange("p (h d) -> p h d", h=nh), y_ap3, y_ap3, ALU.mult)
            ss = T(f"ss_{tagscope}", [128, nh])
            Vred(ss[:], sq[:].rearrange("p (h d) -> p h d", h=nh))
            rsqrt_(ss[:], ss[:], 1.0 / hd, eps)
            return ss

        def load_params(l):
            l = lmap[l]
            g = Group()
            dma("sp", normw[:], dr["norm_w"][l].partition_broadcast(128), sem_p, outs=[normw], group=g)
            for name, src in [("w0", dr["rwkv_w0"][l]), ("lnw", dr["rwkv_ln_w"][l]), ("lnb", dr["rwkv_ln_b"][l]),
                              ("retnw", dr["ret_norm_w"][l]), ("ssdnw", dr["ssd_norm_w"][l]),
                              ("gdnnw", dr["gdn_norm_w"][l]), ("ssdD", dr["ssd_D"][l]),
                              ("ssddtb", dr["ssd_dt_bias"][l]), ("ssdA", dr["ssd_A_log"][l]),
                              ("gdndtb", dr["gdn_dt_bias"][l]), ("gdnA", dr["gdn_A_log"][l])]:
                dma("sp", RPv(name), src.partition_broadcast(128), sem_p, outs=[rp], group=g)
            dma("sp", wau[0:64, :], dr["rwkv_w_up"][l], sem_p, outs=[wau], group=g)
            dma("sp", wau[64:128, :], dr["rwkv_a_up"][l], sem_p, outs=[wau], group=g)

            def ppl(col, src, nb):
                dma("sp", pp[:, col:col + nb], src.rearrange("(b p) -> p b", p=128), sem_p, outs=[pp], group=g, slow=True)
            for j in range(3):
                ppl(PP["mu"] + 4 * j, dr["rwkv_mu_rkv"][l][j], 4)
            ppl(PP["mu"] + 12, dr["rwkv_mu_wa"][l].rearrange("a b -> (a b)"), 1)
            ppl(PP["kk"], dr["rwkv_k_k"][l], 4)
            ppl(PP["ka"], dr["rwkv_k_a"][l], 4)
            ppl(PP["rk"], dr["rwkv_r_k"][l].rearrange("a b -> (a b)"), 4)
            ppl(PP["a0"], dr["rwkv_a0"][l], 4)
            for j in range(4):
                ppl(PP["scw"] + 8 * j, dr["ssd_conv_w"][l][j], 8)
            ppl(PP["scb"], dr["ssd_conv_b"][l], 8)
            for j in range(4):
                ppl(PP["gcw"] + 12 * j, dr["gdn_conv_w"][l][j], 12)
            A(RPv("ssdA"), RPv("ssdA"), AF.Exp)
            Vts(RPv("ssdA"), RPv("ssdA"), -1.0, ALU.mult)
            A(RPv("gdnA"), RPv("gdnA"), AF.Exp)
            Vts(RPv("gdnA"), RPv("gdnA"), -1.0, ALU.mult)
            for t_ in [carry_rw, carry_sd, carry_gd] + st_rw + st_ret + st_sd + st_gd:
                Vset(t_[:], 0.0)

        def conv_block(ps, carry, bi, wcol, nblk, bias, out_ap, rbuf, acc):
            Acp(rbuf[:, 0:3], carry[:, bi, :])
            Acp(rbuf[:, 3:3 + NT], ps[:, 0:NT])
            Vts(acc[:], rbuf[:, 0:NT], pp[:, wcol + bi:wcol + bi + 1], ALU.mult)
            for j in range(1, 4):
                c0 = wcol + nblk * j + bi
                Vstt(acc[:], rbuf[:, j:j + NT], pp[:, c0:c0 + 1], acc[:], ALU.mult, ALU.add)
            Acp(carry[:, bi, :], rbuf[:, NT:NT + 3])
            if bias is None:
                A(out_ap, acc[:], AF.Silu)
            else:
                A(out_ap, acc[:], AF.Silu, bias=bias)

        def mixer_ret(l, mt):
            S_.barrier()
            HB = [(0, 3), (3, 3), (6, 2)]
            with contextlib.ExitStack() as sc:
                scopes.append(sc)
                Wqk = W(("in", l, O_RET, 512))
                qk_raw = T("rt_qkraw", [128, NT])
                qk = T("rt_qk", [128, 6, NT])
                t1 = T("rt_t1", [128, NT])
                for b in range(6):
                    h0, nh = HB[b % 3]
                    nr = 32 * nh
                    col0 = (0 if b < 3 else 256) + 32 * h0
                    ps = PS()
                    proj_fm(Wqk, col0, nr, ps)
                    Acp(qk_raw[0:nr, :], ps[0:nr, 0:NT])
                    ps2 = PS()
                    mm(ps2[0:nr, 0:NT], K("swapp")[0:nr, 0:nr], qk_raw[0:nr, :])
                    Vtt(t1[0:nr, :], qk_raw[0:nr, :], ropet[0:nr, 0, :], ALU.mult)
                    Vtt(qk[0:nr, b, :], ps2[0:nr, 0:NT], ropet[0:nr, 1, :], ALU.mult)
                    Vtt(qk[0:nr, b, :], qk[0:nr, b, :], t1[0:nr, :], ALU.add)
                Wv = W(("in", l, O_RET + 512, 512))
                v_tm = [T(f"rt_v{c}", [128, 512]) for c in range(NCH)]
                for c in range(NCH):
                    ps = PS()
                    proj_tm(Wv, 0, 512, c, ps)
                    Acp(v_tm[c][:], ps[:, 0:512])
                Wz = W(("in", l, O_RET + 1024, 512))
                zs = [T(f"rt_z{c}", [128, 512]) for c in range(NCH)]
                for c in range(NCH):
                    ps = PS()
                    proj_tm(Wz, 0, 512, c, ps)
                    A(zs[c][:], ps[:, 0:512], AF.Silu)
                ktail = T("rt_ktail", [128, 256])
                P = T("rt_P", [128, 3, 384])
                qd = T("rt_qd", [128, 3, 128])
                y = T("rt_y", [128, 512])
                tmp = T("rt_tmp", [128, 256])
                for c in range(NCH):
                    cs = slice(c * C, (c + 1) * C)
                    ps = PS()
                    for bq in range(3):
                        h0, nh = HB[bq]
                        nr = 32 * nh
                        tp(ps[:, 32 * h0:32 * h0 + nr], qk[0:nr, 3 + bq, cs], ident[0:nr, 0:nr])
                    Vtt(ktail[:].rearrange("p (h d) -> p h d", h=8), ps[:, 0:256].rearrange("p (h d) -> p h d", h=8),
                        K("ktbl")[:, 0:8].unsqueeze(2).to_broadcast([128, 8, 32]), ALU.mult)
                    pr = [PS() for _ in range(3)]
                    for r in range(3):
                        for bq in range(3):
                            if bq * 3 + r >= 8:
                                continue
                            mm(pr[r][:, bq * 128:(bq + 1) * 128], qk[32 * r:32 * r + 32, 3 + bq, cs], qk[32 * r:32 * r + 32, bq, cs])
                    for r in range(3):
                        w_ = 384 if r < 2 else 256
                        Vtt(P[:, r, 0:w_], pr[r][:, 0:w_], K("dm2")[:, r * 384:r * 384 + w_], ALU.mult)
                    for bq in range(3):
                        nr = 32 * HB[bq][1]
                        Vtt(qd[0:nr, bq, :], qk[0:nr, bq, cs], K("qdec")[0:nr, bq * 128:(bq + 1) * 128], ALU.mult)
                    py = PS()
                    for bq in range(3):
                        h0, nh = HB[bq]
                        nr = 32 * nh
                        mm(py[:, 64 * h0:64 * (h0 + nh)], qd[0:nr, bq, :], st_ret[bq][0:nr, 0:64 * nh], start=True, stop=False)
                        for r in range(nh):
                            h = h0 + r
                            mm(py[:, h * 64:(h + 1) * 64], P[:, r, bq * 128:(bq + 1) * 128], v_tm[c][:, h * 64:(h + 1) * 64],
                               start=False, stop=(r == nh - 1))
                    Acp(y[:], py[:, 0:512])
                    y3 = y[:].rearrange("p (h d) -> p h d", h=8)
                    rstd = head_rms(y3, 8, 64, 1e-6, "rt")
                    Vtt(y3, y3, rstd[:, 0:8].unsqueeze(2).to_broadcast([128, 8, 64]), ALU.mult)
                    Vtt(y[:], y[:], RPv("retnw"), ALU.mult)
                    Vtt(y[:], y[:], zs[c][:], ALU.mult)
                    tm_to_fm_bf16(y, 4, c)
                    for bq in range(3):
                        h0, nh = HB[bq]
                        nr, nv = 32 * nh, 64 * nh
                        ps = PS()
                        mm(ps[0:nr, 0:nv], ktail[:, 32 * h0:32 * h0 + nr], v_tm[c][:, 64 * h0:64 * h0 + nv])
                        Vtt(tmp[0:nr, 0:nv], ps[0:nr, 0:nv], K("mask01")[0:nr, 0:nv], ALU.mult)
                        Vtt(st_ret[bq][0:nr, 0:nv], st_ret[bq][0:nr, 0:nv], K("tblc")[0:nr, bq * 256:bq * 256 + nv], ALU.mult)
                        Vtt(st_ret[bq][0:nr, 0:nv], st_ret[bq][0:nr, 0:nv], tmp[0:nr, 0:nv], ALU.add)
                scopes.pop()

        def decay_prep(ps_raw, nh, dtb, Aneg, tag):
            dt = T(f"dp_dt_{tag}", [128, nh])
            Vtt(dt[:], ps_raw, dtb, ALU.add)
            A(dt[:], dt[:], AF.Exp)
            A(dt[:], dt[:], AF.Ln, bias=1.0)
            la = T(f"dp_la_{tag}", [128, nh])
            Vtt(la[:], dt[:], Aneg, ALU.mult)
            ps = PS()
            mm(ps[:, 0:nh], K("iu"), la[:])
            g = T(f"dp_g_{tag}", [128, nh])
            Acp(g[:], ps[:, 0:nh])
            ng = T(f"dp_ng_{tag}", [128, nh])
            Vts(ng[:], g[:], -1.0, ALU.mult)
            eg = T(f"dp_eg_{tag}", [128, nh])
            A(eg[:], g[:], AF.Exp)
            return dict(dt=dt, la=la, g=g, ng=ng, eg=eg)

        def mixer_ssd(l, mt):
            S_.barrier()
            with contextlib.ExitStack() as sc:
                scopes.append(sc)
                xbc = T("sd_xbc", [128, 8, NT])
                rbuf = T("sd_rbuf", [128, NT + 3])
                acc = T("sd_acc", [128, NT])
                for half in range(2):
                    Wx = W(("in", l, O_SSD + 512 * half, 512))
                    for b4 in range(4):
                        bi = half * 4 + b4
                        ps = PS()
                        proj_fm(Wx, b4 * 128, 128, ps)
                        conv_block(ps, carry_sd, bi, PP["scw"], 8, pp[:, PP["scb"] + bi:PP["scb"] + bi + 1], xbc[:, bi, :], rbuf, acc)
                Wz = W(("in", l, O_SSD + 1024, 512))
                zs = [T(f"sd_z{c}", [128, 512]) for c in range(NCH)]
                for c in range(NCH):
                    ps = PS()
                    proj_tm(Wz, 0, 512, c, ps)
                    A(zs[c][:], ps[:, 0:512], AF.Silu)
                Wdt = W(("in", l, O_SSD + 1536, 8))
                x_tm = T("sd_x", [128, 512])
                b_tm = T("sd_b", [128, 256])
                xdt = T("sd_xdt", [128, 512])
                xdt2 = T("sd_xdt2", [128, 512])
                sc_ = T("sd_sc", [128, 256])
                LAb = [T(f"sd_lab{i}", [128, 128]) for i in range(2)]
                DT = [T(f"sd_dt{i}", [128, 128]) for i in range(2)]
                P = T("sd_P", [128, 8, 128])
                yi = T("sd_yi", [128, 512])
                y = T("sd_y", [128, 512])
                egl = T("sd_egl", [128, 8])
                for c in range(NCH):
                    cs = slice(c * C, (c + 1) * C)
                    ps = PS()
                    proj_tm(Wdt, 0, 8, c, ps)
                    dp = decay_prep(ps[:, 0:8], 8, RPv("ssddtb"), RPv("ssdA"), "sd")
                    ps = PS()
                    mm(ps[:, 0:8], K("ones"), dp["la"][:])
                    A(egl[:], ps[:, 0:8], AF.Exp)
                    ps = PS()
                    for b in range(4):
                        tp(ps[:, b * 128:(b + 1) * 128], xbc[:, b, cs], ident)
                    Acp(x_tm[:], ps[:, 0:512])
                    ps = PS()
                    for g in range(2):
                        tp(ps[:, g * 128:(g + 1) * 128], xbc[:, 4 + g, cs], ident)
                    Vcp(b_tm[:], ps[:, 0:256])
                    Vtt(xdt[:].rearrange("p (h d) -> p h d", h=8), x_tm[:].rearrange("p (h d) -> p h d", h=8),
                        dp["dt"][:, 0:8].unsqueeze(2).to_broadcast([128, 8, 64]), ALU.mult)
                    ps = PS()
                    for g in range(2):
                        mm(ps[:, g * 128:(g + 1) * 128], xbc[:, 4 + g, cs], xbc[:, 6 + g, cs])
                    Acp(sc_[:], ps[:, 0:256])
                    for h in range(8):
                        g = h // 4
                        lab, dtm = LAb[h % 2], DT[h % 2]
                        Vcp(lab[:], dp["la"][:, h:h + 1].to_broadcast([128, 128]))
                        pd = PS()
                        mm(pd[:, 0:128], lab[:], K("iu"), start=True, stop=False)
                        mm(pd[:, 0:128], ident, K("negt"), start=False, stop=True)
                        A(dtm[:], pd[:, 0:128], AF.Exp, bias=dp["ng"][:, h:h + 1])
                        Vtt(P[:, h, :], sc_[:, g * 128:(g + 1) * 128], dtm[:], ALU.mult)
                        Vts(xdt2[:, h * 64:(h + 1) * 64], xdt[:, h * 64:(h + 1) * 64], dtm[:, 127:128], ALU.mult)
                    pyi = PS()
                    for g in range(2):
                        mm(pyi[:, g * 256:(g + 1) * 256], xbc[:, 6 + g, cs], st_sd[g][:])
                    Vtt(yi[:].rearrange("p (h d) -> p h d", h=8), pyi[:, 0:512].rearrange("p (h d) -> p h d", h=8),
                        dp["eg"][:, 0:8].unsqueeze(2).to_broadcast([128, 8, 64]), ALU.mult)
                    py = PS()
                    for h in range(8):
                        mm(py[:, h * 64:(h + 1) * 64], P[:, h, :], xdt[:, h * 64:(h + 1) * 64])
                    Vtt(y[:], py[:, 0:512], yi[:], ALU.add)
                    Vtt(yi[:].rearrange("p (h d) -> p h d", h=8), x_tm[:].rearrange("p (h d) -> p h d", h=8),
                        RPv("ssdD")[:, 0:8].unsqueeze(2).to_broadcast([128, 8, 64]), ALU.mult)
                    Vtt(y[:], y[:], yi[:], ALU.add)
                    Vtt(y[:], y[:], zs[c][:], ALU.mult)
                    y3 = y[:].rearrange("p (h d) -> p h d", h=2)
                    rstd = head_rms(y3, 2, 256, 1e-6, "sd")
                    Vtt(y3, y3, rstd[:, 0:2].unsqueeze(2).to_broadcast([128, 2, 256]), ALU.mult)
                    Vtt(y[:], y[:], RPv("ssdnw"), ALU.mult)
                    tm_to_fm_bf16(y, 8, c)
                    for g in range(2):
                        ps = PS()
                        mm(ps[:, 0:256], b_tm[:, g * 128:(g + 1) * 128], xdt2[:, g * 256:(g + 1) * 256])
                        Vtt(st_sd[g][:].rearrange("p (h d) -> p h d", h=4), st_sd[g][:].rearrange("p (h d) -> p h d", h=4),
                            egl[:, 4 * g:4 * g + 4].unsqueeze(2).to_broadcast([128, 4, 64]), ALU.mult)
                        Vtt(st_sd[g][:], st_sd[g][:], ps[:, 0:256], ALU.add)
                scopes.pop()

        def neumann_apply(Nm, NTm, Z, nh, width, tag, levels=7):
            N2 = [T(f"nm_n2_{tag}{h}", [128, 128], F32R) for h in range(nh)]
            NT2 = [T(f"nm_nt2_{tag}{h}", [128, 128], F32R) for h in range(nh)]
            cur, curT, nxt, nxtT = Nm, NTm, N2, NT2
            for lev in range(levels):
                ps = PS()
                for h in range(nh):
                    mm(ps[:, h * width:(h + 1) * width], curT[h][:], Z[:, h * width:(h + 1) * width])
                dZ = T(f"nm_dz_{tag}", [128, nh * width], F32R)
                Acp(dZ[:], ps[:, 0:nh * width])
                if lev < levels - 1:
                    for h in range(nh):
                        pq = PS()
                        mm(pq[:, 0:128], curT[h][:], cur[h][:])
                        mm(pq[:, 128:256], cur[h][:], curT[h][:])
                        cpe = Acp if (h % 2 == 0) else Vcp
                        cpe(nxt[h][:], pq[:, 0:128])
                        cpe(nxtT[h][:], pq[:, 128:256])
                Vtt(Z[:, 0:nh * width], Z[:, 0:nh * width], dZ[:], ALU.add)
                cur, curT, nxt, nxtT = nxt, nxtT, cur, curT

        def mixer_gdn(l, mt):
            S_.barrier()
            with contextlib.ExitStack() as sc:
                scopes.append(sc)
                qkv = T("gd_qkv", [128, 12, NT])
                rbuf = T("gd_rbuf", [128, NT + 3])
                acc = T("gd_acc", [128, NT])
                sq = T("gd_sq", [128, NT])
                for part in range(3):
                    Wx = W(("in", l, O_GDN + 512 * part, 512))
                    for b4 in range(4):
                        bi = part * 4 + b4
                        ps = PS()
                        proj_fm(Wx, b4 * 128, 128, ps)
                        conv_block(ps, carry_gd, bi, PP["gcw"], 12, None, qkv[:, bi, :], rbuf, acc)
                        if part < 2:
                            Vtt(sq[:], qkv[:, bi, :], qkv[:, bi, :], ALU.mult)
                            ps2 = PS()
                            mm(ps2[:, 0:NT], K("ones"), sq[:])
                            rsqrt_(sq[:], ps2[:, 0:NT], 1.0, 1e-6)
                            if part == 0:
                                Vstt(qkv[:, bi, :], qkv[:, bi, :], float(128 ** -0.5), sq[:], ALU.mult, ALU.mult)
                            else:
                                Vtt(qkv[:, bi, :], qkv[:, bi, :], sq[:], ALU.mult)
                import os
                GDSTOP = int(os.environ.get("GDSTOP", "9"))
                Wz = W(("in", l, O_GDN + 1536, 512))
                zs = [T(f"gd_z{c}", [128, 512]) for c in range(NCH)]
                for c in range(NCH):
                    ps = PS()
                    proj_tm(Wz, 0, 512, c, ps)
                    A(zs[c][:], ps[:, 0:512], AF.Silu)
                Wba = W(("in", l, O_GDN + 2048, 8))
                v_tm = T("gd_v", [128, 512])
                ktail = T("gd_ktail", [128, 512])
                beta = T("gd_beta", [128, 4])
                lnb = T("gd_lnb", [128, 4])
                gb = T("gd_gb", [128, 4])
                neg = T("gd_neg", [128, 4])
                egl = T("gd_egl", [128, 4])
                LAb = [T(f"gd_lab{h}", [128, 128]) for h in range(4)]
                DT = [T(f"gd_dt{h}", [128, 128]) for h in range(4)]
                DB = [T(f"gd_db{h}", [128, 128]) for h in range(4)]
                Nm = [T(f"gd_n{h}", [128, 128], F32R) for h in range(4)]
                NTm = [T(f"gd_nt{h}", [128, 128], F32R) for h in range(4)]
                attnT = [T(f"gd_at{h}", [128, 128]) for h in range(4)]
                Z = T("gd_Z", [128, 512], F32R)
                o1 = T("gd_o1", [128, 512])
                o = T("gd_o", [128, 512])
                for c in range(NCH):
                    cs = slice(c * C, (c + 1) * C)
                    if GDSTOP <= 1:
                        continue
                    ps = PS()
                    proj_tm(Wba, 0, 8, c, ps)
                    ba = T("gd_ba", [128, 8])
                    Acp(ba[:], ps[:, 0:8])
                    A(beta[:], ba[:, 0:4], AF.Sigmoid)
                    A(lnb[:], beta[:], AF.Ln)
                    dp = decay_prep(ba[:, 4:8], 4, RPv("gdndtb"), RPv("gdnA"), "gd")
                    Vtt(gb[:], dp["g"][:], lnb[:], ALU.add)
                    Vts(neg[:], dp["eg"][:], -1.0, ALU.mult)
                    ps = PS()
                    for h in range(4):
                        tp(ps[:, h * 128:(h + 1) * 128], qkv[:, 8 + h, cs], ident)
                    Acp(v_tm[:], ps[:, 0:512])
                    pk_ = PS()
                    for h in range(4):
                        tp(pk_[:, h * 128:(h + 1) * 128], qkv[:, 4 + h, cs], ident)
                    pk = T("gd_ktm", [128, 512])
                    Vcp(pk[:], pk_[:, 0:512])
                    for h in range(4):
                        kT = qkv[:, 4 + h, cs]
                        qT = qkv[:, h, cs]
                        Vcp(LAb[h][:], dp["la"][:, h:h + 1].to_broadcast([128, 128]))
                        pd = PS()
                        mm(pd[:, 0:128], LAb[h][:], K("iu"), start=True, stop=False)
                        mm(pd[:, 0:128], ident, K("negt"), start=False, stop=True)
                        mm(pd[:, 128:256], LAb[h][:], K("niu"), start=True, stop=False)
                        mm(pd[:, 128:256], ident, K("negs"), start=False, stop=True)
                        A(DT[h][:], pd[:, 0:128], AF.Exp, bias=dp["ng"][:, h:h + 1])
                        A(egl[:, h:h + 1], pd[:, 127:128], AF.Exp)
                        A(DB[h][:], pd[:, 128:256], AF.Exp, bias=gb[:, h:h + 1])
                        Vts(ktail[:, h * 128:(h + 1) * 128], pk[:, h * 128:(h + 1) * 128], DT[h][:, 127:128], ALU.mult)
                        pq = PS()
                        mm(pq[:, 0:128], kT, kT)
                        mm(pq[:, 128:256], kT, qT)
                        mm(pq[:, 256:384], kT, st_gd[h][:])
                        Vstt(Nm[h][:], pq[:, 0:128], -1.0, DB[h][:], ALU.mult, ALU.mult)
                        Vtt(attnT[h][:], pq[:, 128:256], DT[h][:], ALU.mult)
                        pt = PS()
                        tp(pt[:, 0:128], Nm[h][:].bitcast(F32), ident)
                        Acp(NTm[h][:], pt[:, 0:128])
                        Vstt(Z[:, h * 128:(h + 1) * 128], pq[:, 256:384], neg[:, h:h + 1], v_tm[:, h * 128:(h + 1) * 128], ALU.mult, ALU.add)
                        Vts(Z[:, h * 128:(h + 1) * 128], Z[:, h * 128:(h + 1) * 128], beta[:, h:h + 1], ALU.mult)
                    if GDSTOP <= 2:
                        continue
                    neumann_apply(Nm, NTm, Z, 4, 128, "gd")
                    if GDSTOP <= 3:
                        continue
                    po1 = PS()
                    po2 = PS()
                    for h in range(4):
                        mm(po1[:, h * 128:(h + 1) * 128], qkv[:, h, cs], st_gd[h][:])
                        mm(po2[:, h * 128:(h + 1) * 128], attnT[h][:], Z[:, h * 128:(h + 1) * 128].bitcast(F32))
                    Vtt(o1[:].rearrange("p (h d) -> p h d", h=4), po1[:, 0:512].rearrange("p (h d) -> p h d", h=4),
                        dp["eg"][:, 0:4].unsqueeze(2).to_broadcast([128, 4, 128]), ALU.mult)
                    Vtt(o[:], o1[:], po2[:, 0:512], ALU.add)
                    o3 = o[:].rearrange("p (h d) -> p h d", h=4)
                    rstd = head_rms(o3, 4, 128, 1e-6, "gd")
                    Vtt(o3, o3, rstd[:, 0:4].unsqueeze(2).to_broadcast([128, 4, 128]), ALU.mult)
                    Vtt(o3, o3, RPv("gdnnw").unsqueeze(1).to_broadcast([128, 4, 128]), ALU.mult)
                    Vtt(o[:], o[:], zs[c][:], ALU.mult)
                    tm_to_fm_bf16(o, 12, c)
                    for h in range(4):
                        ps = PS()
                        mm(ps[:, 0:128], ktail[:, h * 128:(h + 1) * 128], Z[:, h * 128:(h + 1) * 128].bitcast(F32))
                        Vstt(st_gd[h][:], st_gd[h][:], egl[:, h:h + 1], ps[:, 0:128], ALU.mult, ALU.add)
                scopes.pop()

        def mixer_rwkv(l, mt):
            S_.barrier()
            with contextlib.ExitStack() as sc:
                scopes.append(sc)
                rkv = T("rw_rkv", [128, 12, NT])
                wam = T("rw_wam", [128, NT])
                rbuf = T("rw_rbuf", [128, NT + 1])
                dtl = T("rw_d", [128, NT])

                def shift_block(ps, idx, out_ap):
                    Acp(rbuf[:, 0:1], carry_rw[:, idx:idx + 1])
                    Acp(rbuf[:, 1:NT + 1], ps[:, 0:NT])
                    Vtt(dtl[:], rbuf[:, 0:NT], rbuf[:, 1:NT + 1], ALU.subtract)
                    Vstt(out_ap, dtl[:], pp[:, PP["mu"] + idx:PP["mu"] + idx + 1], rbuf[:, 1:NT + 1], ALU.mult, ALU.add)
                    Acp(carry_rw[:, idx:idx + 1], rbuf[:, NT:NT + 1])
                for j in range(3):
                    Wj = W(("in", l, O_RW + 512 * j, 512))
                    for b in range(4):
                        ps = PS()
                        proj_fm(Wj, b * 128, 128, ps)
                        shift_block(ps, 4 * j + b, rkv[:, 4 * j + b, :])
                Wwa = W(("in", l, O_RW + 1536, 128))
                ps = PS()
                proj_fm(Wwa, 0, 128, ps)
                shift_block(ps, 12, wam[:])
                A(wam[0:64, :], wam[0:64, :], AF.Tanh)
                Wz = W(("in", l, O_RW + 1664, 512))
                zs = [T(f"rw_z{c}", [128, 512]) for c in range(NCH)]
                for c in range(NCH):
                    ps = PS()
                    proj_tm(Wz, 0, 512, c, ps)
                    A(zs[c][:], ps[:, 0:512], AF.Silu)
                iclr = T("rw_iclr", [128, 4, NT])
                kk = T("rw_kk", [128, 4, NT])
                sq = T("rw_sq", [128, NT])
                rkr = T("rw_rkr", [128, 4, NT])
                for b in range(4):
                    ps = PS()
                    mm(ps[:, 0:NT], wau[64:128, b * 128:(b + 1) * 128], wam[64:128, :])
                    A(iclr[:, b, :], ps[:, 0:NT], AF.Sigmoid, bias=pp[:, PP["a0"] + b:PP["a0"] + b + 1])
                    km = rkv[:, 4 + b, :]
                    Vts(kk[:, b, :], km, pp[:, PP["kk"] + b:PP["kk"] + b + 1], ALU.mult)
                    Vtt(sq[:], kk[:, b, :], kk[:, b, :], ALU.mult)
                    ps2 = PS()
                    mm(ps2[:, 0:NT], K("bones"), sq[:])
                    rsqrt_(sq[:], ps2[:, 0:NT], 1.0, 1e-6)
                    Vtt(kk[:, b, :], kk[:, b, :], sq[:], ALU.mult)
                    Vts(sq[:], iclr[:, b, :], -1.0, ALU.add, pp[:, PP["ka"] + b:PP["ka"] + b + 1], ALU.mult)
                    Vstt(km, sq[:], 1.0, km, ALU.add, ALU.mult)
                    Vtt(iclr[:, b, :], iclr[:, b, :], kk[:, b, :], ALU.mult)
                    Vstt(rkr[:, b, :], rkv[:, b, :], pp[:, PP["rk"] + b:PP["rk"] + b + 1], km, ALU.mult, ALU.mult)
                bm = iclr
                lw = T("rw_lw", [128, 512])
                Gx = T("rw_G", [128, 4, 132])
                eG = T("rw_eG", [128, 4, 128])
                enG = T("rw_enG", [128, 4, 128])
                lwf = T("rw_lwf", [128, 4, 128])
                AR = T("rw_AR", [128, 4, 256])
                kp = T("rw_kp", [128, 4, 128])
                bp = T("rw_bp", [128, 4, 128])
                sc1 = T("rw_sc1", [128, 4])
                sc2 = T("rw_sc2", [128, 4])
                sc3 = T("rw_sc3", [128, 4])
                A0s = [T(f"rw_a0s{b}", [128, 128]) for b in range(4)]
                kpT = T("rw_kpT", [128, 512])
                bpT = T("rw_bpT", [128, 512])
                v_tm = T("rw_v", [128, 512])
                SC = [T(f"rw_SC{h}", [128, 512]) for h in range(8)]
                Nm = [T(f"rw_N{h}", [128, 128], F32R) for h in range(8)]
                NTm = [T(f"rw_NT{h}", [128, 128], F32R) for h in range(8)]
                Z = T("rw_Z", [128, 512], F32R)
                y = T("rw_y", [128, 512])
                bon = T("rw_bon", [128, 8])
                mv = T("rw_mv", [128, 8])
                tmpb = T("rw_tmpb", [128, 128])
                for c in range(NCH):
                    cs = slice(c * C, (c + 1) * C)
                    ps = PS()
                    mm(ps[:, 0:512], wam[0:64, cs], wau[0:64, :])
                    Vtt(lw[:], ps[:, 0:512], RPv("w0"), ALU.add)
                    A(lw[:], lw[:], AF.Sigmoid)
                    Vts(lw[:], lw[:], float(-np.exp(-0.5)), ALU.mult)
                    for b in range(4):
                        ps = PS()
                        mm(ps[:, 0:132], lw[:, b * 128:(b + 1) * 128], K("tmat"))
                        Acp(Gx[:, b, :], ps[:, 0:132])
                        ps2 = PS()
                        tp(ps2[:, 0:128], lw[:, b * 128:(b + 1) * 128], ident)
                        Vcp(lwf[:, b, :], ps2[:, 0:128])
                    A(eG[:], Gx[:, :, 0:128], AF.Exp)
                    A(enG[:], Gx[:, :, 0:128], AF.Exp, scale=-1.0)
                    A(sc1[:], Gx[:, :, 128], AF.Exp, scale=-1.0)
                    A(sc2[:], Gx[:, :, 127], AF.Exp)
                    Vtt(sc3[:], sc1[:], sc2[:], ALU.mult)
                    Vtt(AR[:, :, 128:256], rkv[:, 0:4, cs], eG[:], ALU.mult)
                    Vtt(kp[:], rkv[:, 4:8, cs], enG[:], ALU.mult)
                    Vtt(bp[:], bm[:, :, cs], enG[:], ALU.mult)
                    A(lwf[:], lwf[:], AF.Exp, scale=-1.0)
                    Vtt(lwf[:], lwf[:], eG[:], ALU.mult)
                    Vstt(AR[:, :, 0:128], kk[:, :, cs], -1.0, lwf[:], ALU.mult, ALU.mult)
                    for b in range(4):
                        Vts(A0s[b][:], st_rw[b][:], sc1[:, b:b + 1], ALU.mult)
                    ps = PS()
                    ps2 = PS()
                    ps3 = PS()
                    for b in range(4):
                        tp(ps[:, b * 128:(b + 1) * 128], kp[:, b, :], ident)
                        tp(ps2[:, b * 128:(b + 1) * 128], bp[:, b, :], ident)
                        tp(ps3[:, b * 128:(b + 1) * 128], rkv[:, 8 + b, cs], ident)
                    Acp(kpT[:], ps[:, 0:512])
                    Vcp(bpT[:], ps2[:, 0:512])
                    Acp(v_tm[:], ps3[:, 0:512])
                    psb = PS()
                    for b in range(4):
                        mm(psb[:, 2 * b:2 * b + 2], rkr[:, b, cs], K("sel2")[:, 0:2])
                    Acp(bon[:], psb[:, 0:8])
                    for h in range(8):
                        b, r0 = h // 2, 64 * (h % 2)
                        rows = slice(r0, r0 + 64)
                        pa = PS()
                        mm(pa[:, 0:256], bp[rows, b, :], AR[rows, b, :])
                        mm(pa[:, 256:512], kp[rows, b, :], AR[rows, b, :])
                        Vtt(SC[h][:], pa[:, 0:512], K("maskA"), ALU.mult)
                        pn = PS()
                        mm(pn[:, 0:128], AR[rows, b, 0:128], bp[rows, b, :])
                        Vtt(Nm[h][:], pn[:, 0:128], K("sl"), ALU.mult)
                        Acp(NTm[h][:], SC[h][:, 0:128])
                    pz = PS()
                    for b in range(4):
                        mm(pz[:, b * 128:(b + 1) * 128], AR[:, b, 0:128], A0s[b][:], start=True, stop=False)
                        for hh in range(2):
                            h = 2 * b + hh
                            mm(pz[:, h * 64:(h + 1) * 64], SC[h][:, 256:384], v_tm[:, h * 64:(h + 1) * 64], start=False, stop=(hh == 1))
                    Acp(Z[:], pz[:, 0:512])
                    import os
                    if os.environ.get("RWDBG", "") == "pv":
                        pvs = T("rw_pvs", [128, 512])
                        Vcp(pvs[:], Z[:])
                    neumann_apply(Nm, NTm, Z, 8, 64, "rw")
                    py = PS()
                    for b in range(4):
                        mm(py[:, b * 128:(b + 1) * 128], AR[:, b, 128:256], A0s[b][:], start=True, stop=False)
                        for hh in range(2):
                            h = 2 * b + hh
                            mm(py[:, h * 64:(h + 1) * 64], SC[h][:, 384:512], v_tm[:, h * 64:(h + 1) * 64], start=False, stop=False)
                            mm(py[:, h * 64:(h + 1) * 64], SC[h][:, 128:256], Z[:, h * 64:(h + 1) * 64].bitcast(F32), start=False, stop=(hh == 1))
                    Acp(y[:], py[:, 0:512])
                    for b in range(4):
                        ps = PS()
                        mm(ps[:, 0:128], kpT[:, b * 128:(b + 1) * 128], v_tm[:, b * 128:(b + 1) * 128], start=True, stop=False)
                        mm(ps[:, 0:128], bpT[:, b * 128:(b + 1) * 128], Z[:, b * 128:(b + 1) * 128].bitcast(F32), start=False, stop=True)
                        Vstt(tmpb[:], ps[:, 0:128], sc2[:, b:b + 1], K("bones"), ALU.mult, ALU.mult)
                        Vstt(st_rw[b][:], st_rw[b][:], sc3[:, b:b + 1], tmpb[:], ALU.mult, ALU.add)
                    y3 = y[:].rearrange("p (h d) -> p h d", h=8)
                    Vred(mv[:], y3)
                    Vts(mv[:], mv[:], float(-1.0 / 64), ALU.mult)
                    Vtt(y3, y3, mv[:, 0:8].unsqueeze(2).to_broadcast([128, 8, 64]), ALU.add)
                    rstd = head_rms(y3, 8, 64, 64e-5, "rw", sq=kpT)
                    Vtt(y3, y3, rstd[:, 0:8].unsqueeze(2).to_broadcast([128, 8, 64]), ALU.mult)
                    Vtt(y[:], y[:], RPv("lnw"), ALU.mult)
                    Vtt(y[:], y[:], RPv("lnb"), ALU.add)
                    v3 = v_tm[:].rearrange("p (h d) -> p h d", h=8)
                    Vtt(Z[:].rearrange("p (h d) -> p h d", h=8), v3, bon[:, 0:8].unsqueeze(2).to_broadcast([128, 8, 64]), ALU.mult) if False else None
                    bz = bpT
                    Vtt(bz[:].rearrange("p (h d) -> p h d", h=8), v3, bon[:, 0:8].unsqueeze(2).to_broadcast([128, 8, 64]), ALU.mult)
                    Vtt(y[:], y[:], bz[:], ALU.add)
                    Vtt(y[:], y[:], zs[c][:], ALU.mult)
                    import os
                    dsel = os.environ.get("RWDBG", "")
                    if dsel == "py":
                        Acp(y[:], py[:, 0:512])
                    elif dsel == "z":
                        Vcp(y[:], Z[:])
                    elif dsel == "v":
                        Vcp(y[:], v_tm[:])
                    elif dsel == "lw":
                        Vcp(y[:], lw[:])
                    elif dsel == "kpT":
                        Vcp(y[:], kpT[:])
                    elif dsel == "pv":
                        Vcp(y[:], pvs[:])
                    elif dsel == "bon":
                        Vcp(y[:], bz[:])
                    tm_to_fm_bf16(y, 0, c)
                scopes.pop()

        def merge_out(l, mt, xsrc, xdst, is_last):
            S_.barrier()
            with contextlib.ExitStack() as sc:
                scopes.append(sc)
                macc = T("mg_acc", [128, 8, NT])
                mT = T("mg_mT", [128, 8, NT], BF16)
                sg = T("mg_sg", [128, NT])
                tt_ = T("mg_t", [128, NT])
                for br in range(4):
                    Wb = W(("br", l, br, 0))
                    Wg = [None, None]
                    for half in range(2):
                        Wg[half] = W(("in", l, O_GATE + br * 1024 + 512 * half, 512), live=1 + half)
                        for d4 in range(4):
                            dmb = half * 4 + d4
                            pg = PS()
                            proj_fm(Wg[half], d4 * 128, 128, pg)
                            A(sg[:], pg[:, 0:NT], AF.Sigmoid)
                            pb_ = PS()
                            for kc in range(4):
                                mm(pb_[:, 0:NT], Wb[:, kc, dmb * 128:(dmb + 1) * 128], u_all[:, br * 4 + kc, :], start=(kc == 0), stop=(kc == 3))
                            if br == 0:
                                Vtt(macc[:, dmb, :], sg[:], pb_[:, 0:NT], ALU.mult)
                            elif br < 3:
                                Vtt(tt_[:], sg[:], pb_[:, 0:NT], ALU.mult)
                                Vtt(macc[:, dmb, :], macc[:, dmb, :], tt_[:], ALU.add)
                            else:
                                Vtt(tt_[:], sg[:], pb_[:, 0:NT], ALU.mult)
                                Vtt(mT[:, dmb, :], macc[:, dmb, :], tt_[:], ALU.add)
                for half in range(2):
                    Wo = W(("out", l, 512 * half, 512))
                    for c in range(NCH):
                        ps = PS()
                        for kc in range(8):
                            mm(ps[:, 0:512], mT[:, kc, c * C:(c + 1) * C], Wo[:, kc, :], start=(kc == 0), stop=(kc == 7))
                        Vtt(xt[:, c, half * 512:(half + 1) * 512], xt[:, c, half * 512:(half + 1) * 512], ps[:, 0:512], ALU.add)
                if is_last and final_norm:
                    junk = T("fn_junk", [128, D])
                    ssf = T("fn_ss", [128, NCH])
                    for c in range(NCH):
                        A(junk[:], xt[:, c, :], AF.Square, accum=ssf[:, c:c + 1])
                    rsqrt_(ssf[:], ssf[:], 1.0 / D, 1e-6)
                    for c in range(NCH):
                        Vstt(xt[:, c, :], xt[:, c, :], ssf[:, c:c + 1], finw[:], ALU.mult, ALU.mult)
                rows = slice(mt * NT, (mt + 1) * NT)
                dma("sp", xdst[rows, :].rearrange("(c p) d -> p c d", p=128), xt[:], sem_o, ins=[xt], outs=[("xd", id(xdst), mt)])
                scopes.pop()

        nl = len(layers)
        for li, l in enumerate(layers):
            load_params(l)
            xsrc = dr["x"] if li == 0 else scr[(li - 1) % 2]
            is_last = li == nl - 1
            xdst = out if is_last else scr[li % 2]
            for mt in range(NMT):
                rows = slice(mt * NT, (mt + 1) * NT)
                dma("sp", xt[:], xsrc[rows, :].rearrange("(c p) d -> p c d", p=128), sem_x,
                    ins=[("xd", id(xsrc), mt)], outs=[xt])
                dma("sp", ropet[:], dr["rope"][:, :, rows], sem_r, outs=[ropet])
                with contextlib.ExitStack() as sc:
                    scopes.append(sc)
                    S_.barrier()
                    junk = T("n_junk", [128, D])
                    ss = T("n_ss", [128, NCH])
                    hb = T("n_hb", [128, D], BF16)
                    for c in range(NCH):
                        A(junk[:], xt[:, c, :], AF.Square, accum=ss[:, c:c + 1])
                    rsqrt_(ss[:], ss[:], 1.0 / D, 1e-6)
                    for c in range(NCH):
                        Vstt(hb[:], xt[:, c, :], ss[:, c:c + 1], normw[:], ALU.mult, ALU.mult)
                        for kc in range(8):
                            tp(pbt[:, kc * 128:(kc + 1) * 128], hb[:, kc * 128:(kc + 1) * 128], identb[:])
                        CP(hT[:, :, c * C:(c + 1) * C], pbt[:, 0:1024].rearrange("p (k t) -> p k t", k=8))
                    scopes.pop()
                if "rwkv" in mixers:
                    mixer_rwkv(l, mt)
                if "ret" in mixers:
                    mixer_ret(l, mt)
                if "ssd" in mixers:
                    mixer_ssd(l, mt)
                if "gdn" in mixers:
                    mixer_gdn(l, mt)
                if dbg and is_last:
                    dma("sp", dbg_u[:, rows].rearrange("(b p) t -> p b t", p=128), u_all[:], sem_d, ins=[u_all], outs=[("dbg", mt)])
                merge_out(l, mt, xsrc, xdst, is_last)
        fin_ins = [("xd", id(out), mt) for mt in range(NMT)]
        if dbg:
            fin_ins += [("dbg", mt) for mt in range(NMT)]
        add("sp", lambda e: e.nop(), ins=fin_ins)
        assert jstate["used"] == len(jobs)
        S_.emit()
        build.stats = S_.stats
    return nc


NT_DEFAULT = 256


def kernel(**inputs):
    x = np.ascontiguousarray(np.asarray(inputs["x"], dtype=np.float32))
    B, S, _ = x.shape
    cst, rope = make_consts(S)
    params = {n: np.ascontiguousarray(np.asarray(inputs[n], dtype=np.float32)) for n in PARAM_NAMES}
    nc = build(S, list(range(DEPTH)), NT_DEFAULT, True)
    in_maps = []
    for b in range(B):
        m = {"x": x[b], "cst": cst, "rope": rope}
        m.update(params)
        in_maps.append(m)
    res = run_bass_kernel_spmd(nc, in_maps, core_ids=list(range(B)))
    return np.stack([np.asarray(r["out"], dtype=np.float32) for r in res.results], axis=0)
```

```python
import contextlib
import numpy as np
import concourse.bass as bass
import concourse.mybir as mybir
from concourse.bass_utils import run_bass_kernel_spmd

F32 = mybir.dt.float32
F32R = mybir.dt.float32r
BF16 = mybir.dt.bfloat16
ALU = mybir.AluOpType
AF = mybir.ActivationFunctionType
AX = mybir.AxisListType

D = 1024
NIN = 11408
DEPTH = 4
SEQ = 4096
BATCH = 8
C = 128
O_RW, O_RET, O_SSD, O_GDN, O_GATE = 0, 2176, 3712, 5256, 7312


class Op:
    __slots__ = ("eng", "fn", "deps", "needed", "dma_sem", "token", "group", "is_dma")


class Group:
    def __init__(self):
        self.n = 0
        self.token = None


class Sched:
    ROT = 3500
    DROT = 3488

    def __init__(self, nc, stack, same_sync=("act", "dve", "pool")):
        self.nc = nc
        self.stack = stack
        self.ops = []
        self.w = {}
        self.r = {}
        self.same_sync = set(same_sync)
        self.nsem = 0

    def new_sem(self, name):
        self.nsem += 1
        return self.stack.enter_context(self.nc.semaphore(f"{name}_{self.nsem}"))

    @staticmethod
    def key(a):
        if isinstance(a, (tuple, str)):
            return a
        return a.name

    def add(self, eng, fn, ins=(), outs=(), dma_sem=None, group=None):
        op = Op()
        op.eng = eng
        op.fn = fn
        op.needed = False
        op.dma_sem = dma_sem
        op.is_dma = dma_sem is not None
        op.group = group
        op.token = None
        if group is not None:
            group.n += 1
        ikeys = [self.key(a) for a in ins if a is not None]
        okeys = [self.key(a) for a in outs if a is not None]
        ikeys.append("EPOCH")
        deps = []
        for k in ikeys:
            deps += self.w.get(k, [])
        for k in okeys:
            deps += self.w.get(k, [])
            rd = self.r.get(k)
            if rd:
                deps += list(rd[0].values())
                deps += rd[1]
        fdeps = []
        seen = set()
        for d in deps:
            if id(d) in seen or d is op:
                continue
            seen.add(id(d))
            if group is not None and d.group is group:
                continue
            if (not d.is_dma) and d.eng == eng and eng not in self.same_sync:
                continue
            d.needed = True
            fdeps.append(d)
        op.deps = fdeps
        for k in ikeys:
            rd = self.r.setdefault(k, [{}, []])
            if op.is_dma:
                rd[1].append(op)
            else:
                rd[0][eng] = op
        for k in okeys:
            cur = self.w.get(k, [])
            if group is not None and cur and all(c.group is group for c in cur):
                cur.append(op)
                self.w[k] = cur
            else:
                self.w[k] = [op]
            self.r[k] = [{}, []]
        self.ops.append(op)
        return op

    def barrier(self):
        self.add("dve", lambda e: e.engine_nop(), outs=["EPOCH"])

    def emit(self):
        nc = self.nc
        cnt, cursem, dcount, waited, dsem = {}, {}, {}, {}, {}
        per_eng = {e: [] for e in ("pe", "act", "dve", "pool", "sp")}
        nwaits = 0
        for op in self.ops:
            waits = {}
            for d in op.deps:
                sem, val = d.token
                k = id(sem)
                if k not in waits or waits[k][1] < val:
                    waits[k] = (sem, val)
            wl = []
            for k, (sem, val) in waits.items():
                wk = (op.eng, k)
                if waited.get(wk, 0) >= val:
                    continue
                waited[wk] = val
                wl.append((sem, val))
            nwaits += len(wl)
            inc = None
            if op.is_dma:
                fam = id(op.dma_sem)
                g = op.group
                need = 16 * (g.n if (g is not None and g.token is None) else 1)
                if fam not in dsem:
                    dsem[fam] = op.dma_sem
                    dcount[fam] = 0
                if (g is None or g.token is None) and dcount[fam] + need > self.DROT:
                    dsem[fam] = self.new_sem("dr")
                    dcount[fam] = 0
                sem = dsem[fam]
                if g is not None:
                    if g.token is None:
                        g.token = (sem, dcount[fam] + 16 * g.n)
                    sem = g.token[0]
                    dcount[fam] += 16
                    op.token = g.token
                else:
                    dcount[fam] += 16
                    op.token = (sem, dcount[fam])
                inc = (sem, 16)
            elif op.needed:
                e = op.eng
                if e not in cursem or cnt[e] >= self.ROT:
                    cursem[e] = self.new_sem("e" + e)
                    cnt[e] = 0
                cnt[e] += 1
                op.token = (cursem[e], cnt[e])
                inc = (cursem[e], 1)
            per_eng[op.eng].append((wl, op.fn, inc))
            op.deps = None
        self.stats = dict(n_ops=len(self.ops), n_waits=nwaits, n_sems=self.nsem,
                          per_eng={e: len(v) for e, v in per_eng.items()})

        def run(e, lst):
            for wl, fn, inc in lst:
                for sem, val in wl:
                    e.wait_ge(sem, val)
                ins = fn(e)
                if inc is not None:
                    ins.then_inc(inc[0], inc[1])

        with nc.Block() as block:
            @block.tensor
            def _(e):
                run(e, per_eng["pe"])

            @block.scalar
            def _(e):
                run(e, per_eng["act"])

            @block.vector
            def _(e):
                run(e, per_eng["dve"])

            @block.gpsimd
            def _(e):
                run(e, per_eng["pool"])

            @block.sync
            def _(e):
                run(e, per_eng["sp"])


CST = {}


def _cst_layout():
    off = 0
    for name, n in [("ident", 128), ("maskA", 512), ("iu", 128), ("niu", 128), ("negt", 128),
                    ("negs", 128), ("sl", 128), ("tmat", 132), ("ones", 128), ("bones", 128),
                    ("sel2", 8), ("swapp", 128), ("dm2", 1152), ("qdec", 384), ("ktbl", 8),
                    ("tblc", 768), ("mask01", 256)]:
        CST[name] = (off, n)
        off += n
    return off


NCST = _cst_layout()


def make_consts(S):
    c = np.zeros((128, NCST), np.float32)

    def put(name, arr):
        o, n = CST[name]
        c[:, o:o + arr.shape[1]] = arr

    i = np.arange(128)
    su = (i[:, None] < i[None, :]).astype(np.float32)
    iu = (i[:, None] <= i[None, :]).astype(np.float32)
    put("ident", np.eye(128, dtype=np.float32))
    put("maskA", np.concatenate([su, iu, su, iu], axis=1))
    put("iu", iu)
    put("niu", -iu)
    put("negt", np.where(i[:, None] <= i[None, :], 0.0, -1e30).astype(np.float32))
    put("negs", np.where(i[None, :] < i[:, None], 0.0, -1e30).astype(np.float32))
    put("sl", su.T.copy())
    tm = np.zeros((128, 132), np.float32)
    m = 63
    tm[:, :128] = iu - (i[:, None] <= m).astype(np.float32)
    tm[:, 128] = -(i <= m).astype(np.float32)
    put("tmat", tm)
    put("ones", np.ones((128, 128), np.float32))
    bo = np.zeros((128, 128), np.float32)
    bo[:64, :64] = 1
    bo[64:, 64:] = 1
    put("bones", bo)
    s2 = np.zeros((128, 8), np.float32)
    s2[:64, 0] = 1
    s2[64:, 1] = 1
    put("sel2", s2)
    sp = np.zeros((128, 128), np.float32)
    sp[i, i ^ 1] = 1
    put("swapp", sp)
    gam = 1.0 - np.exp2(-5.0 - np.arange(8, dtype=np.float64))
    lg = np.log(gam)
    scale = 32 ** -0.5
    dm2 = np.zeros((128, 3, 3, 128), np.float64)
    for h in range(8):
        r, bq = h % 3, h // 3
        dm2[:, r, bq, :] = np.where(i[:, None] <= i[None, :], np.exp(lg[h] * (i[None, :] - i[:, None])), 0.0) * scale
    put("dm2", dm2.reshape(128, 1152).astype(np.float32))
    qd = np.ones((128, 3, 128), np.float64)
    for bq in range(3):
        for p in range(96):
            h = bq * 3 + p // 32
            if h < 8:
                qd[p, bq, :] = np.exp(lg[h] * (i + 1))
    put("qdec", qd.reshape(128, 384).astype(np.float32))
    kt = np.zeros((128, 8), np.float64)
    for h in range(8):
        kt[:, h] = np.exp(lg[h] * (127 - i)) * scale
    put("ktbl", kt.astype(np.float32))
    tc_ = np.zeros((128, 3, 256), np.float64)
    m01 = np.zeros((128, 256), np.float32)
    for hh in range(4):
        m01[hh * 32:(hh + 1) * 32, hh * 64:(hh + 1) * 64] = 1
    for bq in range(3):
        for hh in range(3):
            h = bq * 3 + hh
            if h < 8:
                tc_[hh * 32:(hh + 1) * 32, bq, hh * 64:(hh + 1) * 64] = np.exp(lg[h] * 128)
    put("tblc", tc_.reshape(128, 768).astype(np.float32))
    put("mask01", m01)
    half = 16
    angle = 1.0 / (10000.0 ** np.linspace(0.0, 1.0, half, dtype=np.float32)).astype(np.float32)
    theta = np.arange(S, dtype=np.float32)[:, None] * angle[None, :]
    cos = np.cos(theta).astype(np.float32)
    sin = np.sin(theta).astype(np.float32)
    rope = np.zeros((128, 2, S), np.float32)
    for p in range(128):
        ii = (p % 32) // 2
        rope[p, 0, :] = cos[:, ii]
        rope[p, 1, :] = (-sin[:, ii]) if (p % 2 == 0) else sin[:, ii]
    return c, rope


PARAM_NAMES = ["norm_w", "w_in", "rwkv_mu_rkv", "rwkv_mu_wa", "rwkv_w_up", "rwkv_w0", "rwkv_a_up", "rwkv_a0",
               "rwkv_k_k", "rwkv_k_a", "rwkv_r_k", "rwkv_ln_w", "rwkv_ln_b", "ret_norm_w", "ssd_conv_w",
               "ssd_conv_b", "ssd_dt_bias", "ssd_A_log", "ssd_D", "ssd_norm_w", "gdn_conv_w", "gdn_dt_bias",
               "gdn_A_log", "gdn_norm_w", "w_branch", "w_out", "final_norm_w"]
PARAM_SHAPES = {
    "norm_w": [4, 1024], "w_in": [4, 1024, NIN], "rwkv_mu_rkv": [4, 3, 512], "rwkv_mu_wa": [4, 2, 64],
    "rwkv_w_up": [4, 64, 512], "rwkv_w0": [4, 512], "rwkv_a_up": [4, 64, 512], "rwkv_a0": [4, 512],
    "rwkv_k_k": [4, 512], "rwkv_k_a": [4, 512], "rwkv_r_k": [4, 8, 64], "rwkv_ln_w": [4, 512],
    "rwkv_ln_b": [4, 512], "ret_norm_w": [4, 512], "ssd_conv_w": [4, 4, 1024], "ssd_conv_b": [4, 1024],
    "ssd_dt_bias": [4, 8], "ssd_A_log": [4, 8], "ssd_D": [4, 8], "ssd_norm_w": [4, 512],
    "gdn_conv_w": [4, 4, 1536], "gdn_dt_bias": [4, 4], "gdn_A_log": [4, 4], "gdn_norm_w": [4, 128],
    "w_branch": [4, 4, 512, 1024], "w_out": [4, 1024, 1024], "final_norm_w": [1024],
}

RP = {}


def _rp_layout():
    off = 0
    for name, n in [("w0", 512), ("lnw", 512), ("lnb", 512), ("retnw", 512), ("ssdnw", 512), ("gdnnw", 128),
                    ("ssdD", 8), ("ssddtb", 8), ("ssdA", 8), ("gdndtb", 4), ("gdnA", 4)]:
        RP[name] = (off, n)
        off += n
    return off


NRP = _rp_layout()
PP = {"mu": 0, "kk": 13, "ka": 17, "rk": 21, "a0": 25, "scw": 29, "scb": 61, "gcw": 69}
NPP = 128


def build(S, layers, NT, final_norm, mixers=("rwkv", "ret", "ssd", "gdn"), dbg=False, same_sync=True,
          nslot=3, n_param_layers=DEPTH, lmap=None):
    lmap = lmap or {l: l for l in range(DEPTH)}
    NCH = NT // C
    NMT = S // NT
    nc = bass.Bass("TRN2", target_bir_lowering=False)
    dr = {}
    dr["x"] = nc.dram_tensor("x", [S, D], F32, kind="ExternalInput").ap()
    for n in PARAM_NAMES:
        shp = list(PARAM_SHAPES[n])
        if n != "final_norm_w":
            shp[0] = n_param_layers
        dr[n] = nc.dram_tensor(n, shp, F32, kind="ExternalInput").ap()
    dr["cst"] = nc.dram_tensor("cst", [128, NCST], F32, kind="ExternalInput").ap()
    dr["rope"] = nc.dram_tensor("rope", [128, 2, S], F32, kind="ExternalInput").ap()
    out = nc.dram_tensor("out", [S, D], F32, kind="ExternalOutput").ap()
    scr = [nc.dram_tensor(f"scr{i}", [S, D], F32, kind="Internal").ap() for i in range(2)] if len(layers) > 1 else []
    if dbg:
        dbg_u = nc.dram_tensor("dbg_u", [16 * 128, S], BF16, kind="ExternalOutput").ap()

    with contextlib.ExitStack() as st:
        S_ = Sched(nc, st, same_sync=("act", "dve", "pool") if same_sync else ())
        add = S_.add
        scopes = [st]
        tcount = [0]
        tcache = {}

        def T(name, shape, dt=F32):
            ck = (id(scopes[-1]), name)
            if ck in tcache:
                return tcache[ck]
            tcount[0] += 1
            t_ = scopes[-1].enter_context(nc.sbuf_tensor(f"s{tcount[0]}_{name}", shape, dt))
            tcache[ck] = t_
            return t_

        pbs = [st.enter_context(nc.psum_tensor(f"pb{i}", [128, 512], F32)) for i in range(7)]
        pbt = st.enter_context(nc.psum_tensor("pbt", [128, 1024], BF16))
        pstate = [0]

        def PS():
            p = pbs[pstate[0] % 7]
            pstate[0] += 1
            return p

        def mm(out_, lhsT, rhs, start=True, stop=True):
            add("pe", lambda e: e.matmul(out_, lhsT, rhs, start=start, stop=stop), ins=[lhsT, rhs], outs=[out_])

        def tp(out_, in_, ident):
            add("pe", lambda e: e.transpose(out_, in_, ident), ins=[in_, ident], outs=[out_])

        def A(out_, in_, func, bias=None, scale=None, accum=None):
            kw = {}
            ins = [in_]
            if bias is not None:
                kw["bias"] = bias
                if not isinstance(bias, float):
                    ins.append(bias)
            if scale is not None:
                kw["scale"] = scale
                if not isinstance(scale, float):
                    ins.append(scale)
            outs = [out_]
            if accum is not None:
                kw["accum_out"] = accum
                outs.append(accum)
            add("act", lambda e: e.activation(out_, in_, func, **kw), ins=ins, outs=outs)

        def Acp(out_, in_):
            add("act", lambda e: e.copy(out_, in_), ins=[in_], outs=[out_])

        def Vcp(out_, in_):
            add("dve", lambda e: e.tensor_copy(out_, in_), ins=[in_], outs=[out_])

        def Vtt(out_, a, b, op):
            add("dve", lambda e: e.tensor_tensor(out_, a, b, op), ins=[a, b], outs=[out_])

        def Vts(out_, a, s1, op0, s2=None, op1=None):
            ins = [a] + [s for s in (s1, s2) if s is not None and not isinstance(s, float)]
            if op1 is None:
                add("dve", lambda e: e.tensor_scalar(out_, a, s1, None, op0), ins=ins, outs=[out_])
            else:
                add("dve", lambda e: e.tensor_scalar(out_, a, s1, s2, op0, op1), ins=ins, outs=[out_])

        def Vstt(out_, in0, scalar, in1, op0, op1):
            ins = [in0, in1] + ([] if isinstance(scalar, float) else [scalar])
            add("dve", lambda e: e.scalar_tensor_tensor(out_, in0, scalar, in1, op0, op1), ins=ins, outs=[out_])

        def Vred(out_, in_):
            add("dve", lambda e: e.reduce_sum(out_, in_, AX.X), ins=[in_], outs=[out_])

        def Vrec(out_, in_):
            add("dve", lambda e: e.reciprocal(out_, in_), ins=[in_], outs=[out_])

        def Vset(out_, val):
            add("dve", lambda e: e.memset(out_, val), outs=[out_])

        def rsqrt_(out_, in_, mult, eps):
            Vts(out_, in_, mult, ALU.mult, eps, ALU.add)
            Vrec(out_, out_)
            A(out_, out_, AF.Sqrt)

        cpflip = [0]

        def CP(out_, in_):
            cpflip[0] ^= 1
            (Acp if cpflip[0] else Vcp)(out_, in_)

        def dma(eng, out_, in_, sem, ins=(), outs=(), group=None, slow=False):
            if slow:
                add(eng, lambda e: e.dma_start(out=out_, in_=in_, allow_slow_non_contiguous=True), ins=ins, outs=outs, dma_sem=sem, group=group)
            else:
                add(eng, lambda e: e.dma_start(out=out_, in_=in_), ins=ins, outs=outs, dma_sem=sem, group=group)

        cst = T("cst", [128, NCST])
        sem_c = S_.new_sem("cst")
        g0 = Group()
        dma("sp", cst[:, 0:NCST // 2], dr["cst"][:, 0:NCST // 2], sem_c, outs=[cst], group=g0)
        dma("sp", cst[:, NCST // 2:NCST], dr["cst"][:, NCST // 2:NCST], sem_c, outs=[cst], group=g0)

        def K(name, a=0, b=None):
            o, n = CST[name]
            if b is None:
                b = n
            return cst[:, o + a:o + b]

        ident = K("ident")
        identb = T("identb", [128, 128], BF16)
        Vcp(identb[:], ident)
        finw = T("finw", [128, D])
        sem_f = S_.new_sem("finw")
        if final_norm:
            dma("sp", finw[:], dr["final_norm_w"].partition_broadcast(128), sem_f, outs=[finw])

        xt = T("xt", [128, NCH, D])
        hT = T("hT", [128, 8, NT], BF16)
        u_all = T("u_all", [128, 16, NT], BF16)
        normw = T("normw", [128, D])
        pp = T("pp", [128, NPP])
        rp = T("rp", [128, NRP])
        wau = T("wau", [128, 512])
        ropet = T("ropet", [128, 2, NT])
        slots = [T(f"wslot{i}", [128, 4096], BF16) for i in range(nslot)]
        slot_sem = [S_.new_sem(f"ws{i}") for i in range(nslot)]
        sem_x = S_.new_sem("x")
        sem_o = S_.new_sem("o")
        sem_p = S_.new_sem("p")
        sem_r = S_.new_sem("rope")
        sem_d = S_.new_sem("dbg")
        carry_rw = T("carry_rw", [128, 16])
        carry_sd = T("carry_sd", [128, 8, 3])
        carry_gd = T("carry_gd", [128, 12, 3])
        st_rw = [T(f"st_rw{b}", [128, 128]) for b in range(4)]
        st_ret = [T(f"st_ret{b}", [128, 256]) for b in range(3)]
        st_sd = [T(f"st_sd{g}", [128, 256]) for g in range(2)]
        st_gd = [T(f"st_gd{h}", [128, 128]) for h in range(4)]

        if len(mixers) < 4:
            Vset(u_all[:], 0.0)

        def RPv(name, a=0, b=None):
            o, n = RP[name]
            if b is None:
                b = n
            return rp[:, o + a:o + b]

        jobs = []
        for l in layers:
            for mt in range(NMT):
                if "rwkv" in mixers:
                    jobs += [("in", l, O_RW + 0, 512), ("in", l, O_RW + 512, 512), ("in", l, O_RW + 1024, 512),
                             ("in", l, O_RW + 1536, 128), ("in", l, O_RW + 1664, 512)]
                if "ret" in mixers:
                    jobs += [("in", l, O_RET, 512), ("in", l, O_RET + 512, 512), ("in", l, O_RET + 1024, 512)]
                if "ssd" in mixers:
                    jobs += [("in", l, O_SSD, 512), ("in", l, O_SSD + 512, 512), ("in", l, O_SSD + 1024, 512),
                             ("in", l, O_SSD + 1536, 8)]
                if "gdn" in mixers:
                    jobs += [("in", l, O_GDN, 512), ("in", l, O_GDN + 512, 512), ("in", l, O_GDN + 1024, 512),
                             ("in", l, O_GDN + 1536, 512), ("in", l, O_GDN + 2048, 8)]
                for br in range(4):
                    jobs += [("br", l, br, 0), ("in", l, O_GATE + br * 1024, 512), ("in", l, O_GATE + br * 1024 + 512, 512)]
                jobs += [("out", l, 0, 512), ("out", l, 512, 512)]
        jstate = {"issued": 0, "used": 0}
        stg = [T(f"wstg{i}", [128, 2048]) for i in range(2)]
        stg_sem = [S_.new_sem(f"wstg{i}") for i in range(2)]

        def job_src(j, hh):
            kind, l, a, n = jobs[j]
            l = lmap[l]
            if kind == "in":
                src = dr["w_in"][l][:, a:a + n].rearrange("(k p) c -> p k c", p=128)
                return src[:, hh * 4:(hh + 1) * 4, :], 4, n
            if kind == "br":
                src = dr["w_branch"][l][a].rearrange("(k p) c -> p k c", p=128)
                return src[:, hh * 2:(hh + 1) * 2, :], 2, 1024
            src = dr["w_out"][l][:, a:a + n].rearrange("(k p) c -> p k c", p=128)
            return src[:, hh * 4:(hh + 1) * 4, :], 4, n

        def issue_dma(j):
            for hh in range(2):
                src, nk, n = job_src(j, hh)
                sv = stg[hh][:, 0:nk * n].rearrange("p (k c) -> p k c", k=nk)
                dma("sp", sv, src, stg_sem[hh], outs=[stg[hh]])

        def issue_cast(j):
            sl = slots[j % nslot]
            for hh in range(2):
                src, nk, n = job_src(j, hh)
                w_ = nk * n
                (Acp if hh == 0 else Vcp)(sl[:, hh * w_:(hh + 1) * w_], stg[hh][:, 0:w_])

        def W(desc, live=0):
            j = jstate["used"]
            assert jobs[j] == desc, (jobs[j], desc)
            assert live < nslot - 0
            if j == 0:
                issue_dma(0)
            issue_cast(j)
            if j + 1 < len(jobs):
                issue_dma(j + 1)
            jstate["used"] += 1
            kind, l, a, n = desc
            sl = slots[j % nslot]
            if kind == "br":
                return sl[:, 0:4096].rearrange("p (k c) -> p k c", k=4)
            return sl[:, 0:8 * n].rearrange("p (k c) -> p k c", k=8)

        def proj_fm(Wv, col0, nrows, ps, t0=0, nt=None):
            nt = NT if nt is None else nt
            for kc in range(8):
                mm(ps[:nrows, :nt], Wv[:, kc, col0:col0 + nrows], hT[:, kc, t0:t0 + nt], start=(kc == 0), stop=(kc == 7))

        def proj_tm(Wv, col0, ncols, c, ps):
            for kc in range(8):
                mm(ps[:, :ncols], hT[:, kc, c * C:(c + 1) * C], Wv[:, kc, col0:col0 + ncols], start=(kc == 0), stop=(kc == 7))

        def u_store(u_tm, blk0):
            raise NotImplementedError

        def tm_to_fm_bf16(src_tm, blk0, c):
            ps = PS()
            for b in range(4):
                tp(ps[:, b * 128:(b + 1) * 128], src_tm[:, b * 128:(b + 1) * 128], ident)
            CP(u_all[:, blk0:blk0 + 4, c * C:(c + 1) * C], ps[:, 0:512].rearrange("p (b t) -> p b t", b=4))

        def head_rms(y_ap3, nh, hd, eps, tagscope, sq=None):
            if sq is None:
                sq = T(f"sq_{tagscope}", [128, nh * hd])
            Vtt(sq[:].rearrange("p (h d) -> p h d", h=nh), y_ap3, y_ap3, ALU.mult)
            ss = T(f"ss_{tagscope}", [128, nh])
            Vred(ss[:], sq[:].rearrange("p (h d) -> p h d", h=nh))
            rsqrt_(ss[:], ss[:], 1.0 / hd, eps)
            return ss

        def load_params(l):
            l = lmap[l]
            g = Group()
            dma("sp", normw[:], dr["norm_w"][l].partition_broadcast(128), sem_p, outs=[normw], group=g)
            for name, src in [("w0", dr["rwkv_w0"][l]), ("lnw", dr["rwkv_ln_w"][l]), ("lnb", dr["rwkv_ln_b"][l]),
                              ("retnw", dr["ret_norm_w"][l]), ("ssdnw", dr["ssd_norm_w"][l]),
                              ("gdnnw", dr["gdn_norm_w"][l]), ("ssdD", dr["ssd_D"][l]),
                              ("ssddtb", dr["ssd_dt_bias"][l]), ("ssdA", dr["ssd_A_log"][l]),
                              ("gdndtb", dr["gdn_dt_bias"][l]), ("gdnA", dr["gdn_A_log"][l])]:
                dma("sp", RPv(name), src.partition_broadcast(128), sem_p, outs=[rp], group=g)
            dma("sp", wau[0:64, :], dr["rwkv_w_up"][l], sem_p, outs=[wau], group=g)
            dma("sp", wau[64:128, :], dr["rwkv_a_up"][l], sem_p, outs=[wau], group=g)

            def ppl(col, src, nb):
                dma("sp", pp[:, col:col + nb], src.rearrange("(b p) -> p b", p=128), sem_p, outs=[pp], group=g, slow=True)
            for j in range(3):
                ppl(PP["mu"] + 4 * j, dr["rwkv_mu_rkv"][l][j], 4)
            ppl(PP["mu"] + 12, dr["rwkv_mu_wa"][l].rearrange("a b -> (a b)"), 1)
            ppl(PP["kk"], dr["rwkv_k_k"][l], 4)
            ppl(PP["ka"], dr["rwkv_k_a"][l], 4)
            ppl(PP["rk"], dr["rwkv_r_k"][l].rearrange("a b -> (a b)"), 4)
            ppl(PP["a0"], dr["rwkv_a0"][l], 4)
            for j in range(4):
                ppl(PP["scw"] + 8 * j, dr["ssd_conv_w"][l][j], 8)
            ppl(PP["scb"], dr["ssd_conv_b"][l], 8)
            for j in range(4):
                ppl(PP["gcw"] + 12 * j, dr["gdn_conv_w"][l][j], 12)
            A(RPv("ssdA"), RPv("ssdA"), AF.Exp)
            Vts(RPv("ssdA"), RPv("ssdA"), -1.0, ALU.mult)
            A(RPv("gdnA"), RPv("gdnA"), AF.Exp)
            Vts(RPv("gdnA"), RPv("gdnA"), -1.0, ALU.mult)
            for t_ in [carry_rw, carry_sd, carry_gd] + st_rw + st_ret + st_sd + st_gd:
                Vset(t_[:], 0.0)

        def conv_block(ps, carry, bi, wcol, nblk, bias, out_ap, rbuf, acc):
            Acp(rbuf[:, 0:3], carry[:, bi, :])
            Acp(rbuf[:, 3:3 + NT], ps[:, 0:NT])
            Vts(acc[:], rbuf[:, 0:NT], pp[:, wcol + bi:wcol + bi + 1], ALU.mult)
            for j in range(1, 4):
                c0 = wcol + nblk * j + bi
                Vstt(acc[:], rbuf[:, j:j + NT], pp[:, c0:c0 + 1], acc[:], ALU.mult, ALU.add)
            Acp(carry[:, bi, :], rbuf[:, NT:NT + 3])
            if bias is None:
                A(out_ap, acc[:], AF.Silu)
            else:
                A(out_ap, acc[:], AF.Silu, bias=bias)

        def mixer_ret(l, mt):
            S_.barrier()
            HB = [(0, 3), (3, 3), (6, 2)]
            with contextlib.ExitStack() as sc:
                scopes.append(sc)
                Wqk = W(("in", l, O_RET, 512))
                qk_raw = T("rt_qkraw", [128, NT])
                qk = T("rt_qk", [128, 6, NT])
                t1 = T("rt_t1", [128, NT])
                for b in range(6):
                    h0, nh = HB[b % 3]
                    nr = 32 * nh
                    col0 = (0 if b < 3 else 256) + 32 * h0
                    ps = PS()
                    proj_fm(Wqk, col0, nr, ps)
                    Acp(qk_raw[0:nr, :], ps[0:nr, 0:NT])
                    ps2 = PS()
                    mm(ps2[0:nr, 0:NT], K("swapp")[0:nr, 0:nr], qk_raw[0:nr, :])
                    Vtt(t1[0:nr, :], qk_raw[0:nr, :], ropet[0:nr, 0, :], ALU.mult)
                    Vtt(qk[0:nr, b, :], ps2[0:nr, 0:NT], ropet[0:nr, 1, :], ALU.mult)
                    Vtt(qk[0:nr, b, :], qk[0:nr, b, :], t1[0:nr, :], ALU.add)
                Wv = W(("in", l, O_RET + 512, 512))
                v_tm = [T(f"rt_v{c}", [128, 512]) for c in range(NCH)]
                for c in range(NCH):
                    ps = PS()
                    proj_tm(Wv, 0, 512, c, ps)
                    Acp(v_tm[c][:], ps[:, 0:512])
                Wz = W(("in", l, O_RET + 1024, 512))
                zs = [T(f"rt_z{c}", [128, 512]) for c in range(NCH)]
                for c in range(NCH):
                    ps = PS()
                    proj_tm(Wz, 0, 512, c, ps)
                    A(zs[c][:], ps[:, 0:512], AF.Silu)
                ktail = T("rt_ktail", [128, 256])
                P = T("rt_P", [128, 3, 384])
                qd = T("rt_qd", [128, 3, 128])
                y = T("rt_y", [128, 512])
                tmp = T("rt_tmp", [128, 256])
                for c in range(NCH):
                    cs = slice(c * C, (c + 1) * C)
                    ps = PS()
                    for bq in range(3):
                        h0, nh = HB[bq]
                        nr = 32 * nh
                        tp(ps[:, 32 * h0:32 * h0 + nr], qk[0:nr, 3 + bq, cs], ident[0:nr, 0:nr])
                    Vtt(ktail[:].rearrange("p (h d) -> p h d", h=8), ps[:, 0:256].rearrange("p (h d) -> p h d", h=8),
                        K("ktbl")[:, 0:8].unsqueeze(2).to_broadcast([128, 8, 32]), ALU.mult)
                    pr = [PS() for _ in range(3)]
                    for r in range(3):
                        for bq in range(3):
                            if bq * 3 + r >= 8:
                                continue
                            mm(pr[r][:, bq * 128:(bq + 1) * 128], qk[32 * r:32 * r + 32, 3 + bq, cs], qk[32 * r:32 * r + 32, bq, cs])
                    for r in range(3):
                        w_ = 384 if r < 2 else 256
                        Vtt(P[:, r, 0:w_], pr[r][:, 0:w_], K("dm2")[:, r * 384:r * 384 + w_], ALU.mult)
                    for bq in range(3):
                        nr = 32 * HB[bq][1]
                        Vtt(qd[0:nr, bq, :], qk[0:nr, bq, cs], K("qdec")[0:nr, bq * 128:(bq + 1) * 128], ALU.mult)
                    py = PS()
                    for bq in range(3):
                        h0, nh = HB[bq]
                        nr = 32 * nh
                        mm(py[:, 64 * h0:64 * (h0 + nh)], qd[0:nr, bq, :], st_ret[bq][0:nr, 0:64 * nh], start=True, stop=False)
                        for r in range(nh):
                            h = h0 + r
                            mm(py[:, h * 64:(h + 1) * 64], P[:, r, bq * 128:(bq + 1) * 128], v_tm[c][:, h * 64:(h + 1) * 64],
                               start=False, stop=(r == nh - 1))
                    Acp(y[:], py[:, 0:512])
                    y3 = y[:].rearrange("p (h d) -> p h d", h=8)
                    rstd = head_rms(y3, 8, 64, 1e-6, "rt")
                    Vtt(y3, y3, rstd[:, 0:8].unsqueeze(2).to_broadcast([128, 8, 64]), ALU.mult)
                    Vtt(y[:], y[:], RPv("retnw"), ALU.mult)
                    Vtt(y[:], y[:], zs[c][:], ALU.mult)
                    tm_to_fm_bf16(y, 4, c)
                    for bq in range(3):
                        h0, nh = HB[bq]
                        nr, nv = 32 * nh, 64 * nh
                        ps = PS()
                        mm(ps[0:nr, 0:nv], ktail[:, 32 * h0:32 * h0 + nr], v_tm[c][:, 64 * h0:64 * h0 + nv])
                        Vtt(tmp[0:nr, 0:nv], ps[0:nr, 0:nv], K("mask01")[0:nr, 0:nv], ALU.mult)
                        Vtt(st_ret[bq][0:nr, 0:nv], st_ret[bq][0:nr, 0:nv], K("tblc")[0:nr, bq * 256:bq * 256 + nv], ALU.mult)
                        Vtt(st_ret[bq][0:nr, 0:nv], st_ret[bq][0:nr, 0:nv], tmp[0:nr, 0:nv], ALU.add)
                scopes.pop()

        def decay_prep(ps_raw, nh, dtb, Aneg, tag):
            dt = T(f"dp_dt_{tag}", [128, nh])
            Vtt(dt[:], ps_raw, dtb, ALU.add)
            A(dt[:], dt[:], AF.Exp)
            A(dt[:], dt[:], AF.Ln, bias=1.0)
            la = T(f"dp_la_{tag}", [128, nh])
            Vtt(la[:], dt[:], Aneg, ALU.mult)
            ps = PS()
            mm(ps[:, 0:nh], K("iu"), la[:])
            g = T(f"dp_g_{tag}", [128, nh])
            Acp(g[:], ps[:, 0:nh])
            ng = T(f"dp_ng_{tag}", [128, nh])
            Vts(ng[:], g[:], -1.0, ALU.mult)
            eg = T(f"dp_eg_{tag}", [128, nh])
            A(eg[:], g[:], AF.Exp)
            return dict(dt=dt, la=la, g=g, ng=ng, eg=eg)

        def mixer_ssd(l, mt):
            S_.barrier()
            with contextlib.ExitStack() as sc:
                scopes.append(sc)
                xbc = T("sd_xbc", [128, 8, NT])
                rbuf = T("sd_rbuf", [128, NT + 3])
                acc = T("sd_acc", [128, NT])
                for half in range(2):
                    Wx = W(("in", l, O_SSD + 512 * half, 512))
                    for b4 in range(4):
                        bi = half * 4 + b4
                        ps = PS()
                        proj_fm(Wx, b4 * 128, 128, ps)
                        conv_block(ps, carry_sd, bi, PP["scw"], 8, pp[:, PP["scb"] + bi:PP["scb"] + bi + 1], xbc[:, bi, :], rbuf, acc)
                Wz = W(("in", l, O_SSD + 1024, 512))
                zs = [T(f"sd_z{c}", [128, 512]) for c in range(NCH)]
                for c in range(NCH):
                    ps = PS()
                    proj_tm(Wz, 0, 512, c, ps)
                    A(zs[c][:], ps[:, 0:512], AF.Silu)
                Wdt = W(("in", l, O_SSD + 1536, 8))
                x_tm = T("sd_x", [128, 512])
                b_tm = T("sd_b", [128, 256])
                xdt = T("sd_xdt", [128, 512])
                xdt2 = T("sd_xdt2", [128, 512])
                sc_ = T("sd_sc", [128, 256])
                LAb = [T(f"sd_lab{i}", [128, 128]) for i in range(2)]
                DT = [T(f"sd_dt{i}", [128, 128]) for i in range(2)]
                P = T("sd_P", [128, 8, 128])
                yi = T("sd_yi", [128, 512])
                y = T("sd_y", [128, 512])
                egl = T("sd_egl", [128, 8])
                for c in range(NCH):
                    cs = slice(c * C, (c + 1) * C)
                    ps = PS()
                    proj_tm(Wdt, 0, 8, c, ps)
                    dp = decay_prep(ps[:, 0:8], 8, RPv("ssddtb"), RPv("ssdA"), "sd")
                    ps = PS()
                    mm(ps[:, 0:8], K("ones"), dp["la"][:])
                    A(egl[:], ps[:, 0:8], AF.Exp)
                    ps = PS()
                    for b in range(4):
                        tp(ps[:, b * 128:(b + 1) * 128], xbc[:, b, cs], ident)
                    Acp(x_tm[:], ps[:, 0:512])
                    ps = PS()
                    for g in range(2):
                        tp(ps[:, g * 128:(g + 1) * 128], xbc[:, 4 + g, cs], ident)
                    Vcp(b_tm[:], ps[:, 0:256])
                    Vtt(xdt[:].rearrange("p (h d) -> p h d", h=8), x_tm[:].rearrange("p (h d) -> p h d", h=8),
                        dp["dt"][:, 0:8].unsqueeze(2).to_broadcast([128, 8, 64]), ALU.mult)
                    ps = PS()
                    for g in range(2):
                        mm(ps[:, g * 128:(g + 1) * 128], xbc[:, 4 + g, cs], xbc[:, 6 + g, cs])
                    Acp(sc_[:], ps[:, 0:256])
                    for h in range(8):
                        g = h // 4
                        lab, dtm = LAb[h % 2], DT[h % 2]
                        Vcp(lab[:], dp["la"][:, h:h + 1].to_broadcast([128, 128]))
                        pd = PS()
                        mm(pd[:, 0:128], lab[:], K("iu"), start=True, stop=False)
                        mm(pd[:, 0:128], ident, K("negt"), start=False, stop=True)
                        A(dtm[:], pd[:, 0:128], AF.Exp, bias=dp["ng"][:, h:h + 1])
                        Vtt(P[:, h, :], sc_[:, g * 128:(g + 1) * 128], dtm[:], ALU.mult)
                        Vts(xdt2[:, h * 64:(h + 1) * 64], xdt[:, h * 64:(h + 1) * 64], dtm[:, 127:128], ALU.mult)
                    pyi = PS()
                    for g in range(2):
                        mm(pyi[:, g * 256:(g + 1) * 256], xbc[:, 6 + g, cs], st_sd[g][:])
                    Vtt(yi[:].rearrange("p (h d) -> p h d", h=8), pyi[:, 0:512].rearrange("p (h d) -> p h d", h=8),
                        dp["eg"][:, 0:8].unsqueeze(2).to_broadcast([128, 8, 64]), ALU.mult)
                    py = PS()
                    for h in range(8):
                        mm(py[:, h * 64:(h + 1) * 64], P[:, h, :], xdt[:, h * 64:(h + 1) * 64])
                    Vtt(y[:], py[:, 0:512], yi[:], ALU.add)
                    Vtt(yi[:].rearrange("p (h d) -> p h d", h=8), x_tm[:].rearrange("p (h d) -> p h d", h=8),
                        RPv("ssdD")[:, 0:8].unsqueeze(2).to_broadcast([128, 8, 64]), ALU.mult)
                    Vtt(y[:], y[:], yi[:], ALU.add)
                    Vtt(y[:], y[:], zs[c][:], ALU.mult)
                    y3 = y[:].rearrange("p (h d) -> p h d", h=2)
                    rstd = head_rms(y3, 2, 256, 1e-6, "sd")
                    Vtt(y3, y3, rstd[:, 0:2].unsqueeze(2).to_broadcast([128, 2, 256]), ALU.mult)
                    Vtt(y[:], y[:], RPv("ssdnw"), ALU.mult)
                    tm_to_fm_bf16(y, 8, c)
                    for g in range(2):
                        ps = PS()
                        mm(ps[:, 0:256], b_tm[:, g * 128:(g + 1) * 128], xdt2[:, g * 256:(g + 1) * 256])
                        Vtt(st_sd[g][:].rearrange("p (h d) -> p h d", h=4), st_sd[g][:].rearrange("p (h d) -> p h d", h=4),
                            egl[:, 4 * g:4 * g + 4].unsqueeze(2).to_broadcast([128, 4, 64]), ALU.mult)
                        Vtt(st_sd[g][:], st_sd[g][:], ps[:, 0:256], ALU.add)
                scopes.pop()

        def neumann_apply(Nm, NTm, Z, nh, width, tag, levels=7):
            N2 = [T(f"nm_n2_{tag}{h}", [128, 128], F32R) for h in range(nh)]
            NT2 = [T(f"nm_nt2_{tag}{h}", [128, 128], F32R) for h in range(nh)]
            cur, curT, nxt, nxtT = Nm, NTm, N2, NT2
            for lev in range(levels):
                ps = PS()
                for h in range(nh):
                    mm(ps[:, h * width:(h + 1) * width], curT[h][:], Z[:, h * width:(h + 1) * width])
                dZ = T(f"nm_dz_{tag}", [128, nh * width], F32R)
                Acp(dZ[:], ps[:, 0:nh * width])
                if lev < levels - 1:
                    for h in range(nh):
                        pq = PS()
                        mm(pq[:, 0:128], curT[h][:], cur[h][:])
                        mm(pq[:, 128:256], cur[h][:], curT[h][:])
                        cpe = Acp if (h % 2 == 0) else Vcp
                        cpe(nxt[h][:], pq[:, 0:128])
                        cpe(nxtT[h][:], pq[:, 128:256])
                Vtt(Z[:, 0:nh * width], Z[:, 0:nh * width], dZ[:], ALU.add)
                cur, curT, nxt, nxtT = nxt, nxtT, cur, curT

        def mixer_gdn(l, mt):
            S_.barrier()
            with contextlib.ExitStack() as sc:
                scopes.append(sc)
                qkv = T("gd_qkv", [128, 12, NT])
                rbuf = T("gd_rbuf", [128, NT + 3])
                acc = T("gd_acc", [128, NT])
                sq = T("gd_sq", [128, NT])
                for part in range(3):
                    Wx = W(("in", l, O_GDN + 512 * part, 512))
                    for b4 in range(4):
                        bi = part * 4 + b4
                        ps = PS()
                        proj_fm(Wx, b4 * 128, 128, ps)
                        conv_block(ps, carry_gd, bi, PP["gcw"], 12, None, qkv[:, bi, :], rbuf, acc)
                        if part < 2:
                            Vtt(sq[:], qkv[:, bi, :], qkv[:, bi, :], ALU.mult)
                            ps2 = PS()
                            mm(ps2[:, 0:NT], K("ones"), sq[:])
                            rsqrt_(sq[:], ps2[:, 0:NT], 1.0, 1e-6)
                            if part == 0:
                                Vstt(qkv[:, bi, :], qkv[:, bi, :], float(128 ** -0.5), sq[:], ALU.mult, ALU.mult)
                            else:
                                Vtt(qkv[:, bi, :], qkv[:, bi, :], sq[:], ALU.mult)
                import os
                GDSTOP = int(os.environ.get("GDSTOP", "9"))
                Wz = W(("in", l, O_GDN + 1536, 512))
                zs = [T(f"gd_z{c}", [128, 512]) for c in range(NCH)]
                for c in range(NCH):
                    ps = PS()
                    proj_tm(Wz, 0, 512, c, ps)
                    A(zs[c][:], ps[:, 0:512], AF.Silu)
                Wba = W(("in", l, O_GDN + 2048, 8))
                v_tm = T("gd_v", [128, 512])
                ktail = T("gd_ktail", [128, 512])
                beta = T("gd_beta", [128, 4])
                lnb = T("gd_lnb", [128, 4])
                gb = T("gd_gb", [128, 4])
                neg = T("gd_neg", [128, 4])
                egl = T("gd_egl", [128, 4])
                LAb = [T(f"gd_lab{h}", [128, 128]) for h in range(4)]
                DT = [T(f"gd_dt{h}", [128, 128]) for h in range(4)]
                DB = [T(f"gd_db{h}", [128, 128]) for h in range(4)]
                Nm = [T(f"gd_n{h}", [128, 128], F32R) for h in range(4)]
                NTm = [T(f"gd_nt{h}", [128, 128], F32R) for h in range(4)]
                attnT = [T(f"gd_at{h}", [128, 128]) for h in range(4)]
                Z = T("gd_Z", [128, 512], F32R)
                o1 = T("gd_o1", [128, 512])
                o = T("gd_o", [128, 512])
                for c in range(NCH):
                    cs = slice(c * C, (c + 1) * C)
                    if GDSTOP <= 1:
                        continue
                    ps = PS()
                    proj_tm(Wba, 0, 8, c, ps)
                    ba = T("gd_ba", [128, 8])
                    Acp(ba[:], ps[:, 0:8])
                    A(beta[:], ba[:, 0:4], AF.Sigmoid)
                    A(lnb[:], beta[:], AF.Ln)
                    dp = decay_prep(ba[:, 4:8], 4, RPv("gdndtb"), RPv("gdnA"), "gd")
                    Vtt(gb[:], dp["g"][:], lnb[:], ALU.add)
                    Vts(neg[:], dp["eg"][:], -1.0, ALU.mult)
                    ps = PS()
                    for h in range(4):
                        tp(ps[:, h * 128:(h + 1) * 128], qkv[:, 8 + h, cs], ident)
                    Acp(v_tm[:], ps[:, 0:512])
                    pk_ = PS()
                    for h in range(4):
                        tp(pk_[:, h * 128:(h + 1) * 128], qkv[:, 4 + h, cs], ident)
                    pk = T("gd_ktm", [128, 512])
                    Vcp(pk[:], pk_[:, 0:512])
                    for h in range(4):
                        kT = qkv[:, 4 + h, cs]
                        qT = qkv[:, h, cs]
                        Vcp(LAb[h][:], dp["la"][:, h:h + 1].to_broadcast([128, 128]))
                        pd = PS()
                        mm(pd[:, 0:128], LAb[h][:], K("iu"), start=True, stop=False)
                        mm(pd[:, 0:128], ident, K("negt"), start=False, stop=True)
                        mm(pd[:, 128:256], LAb[h][:], K("niu"), start=True, stop=False)
                        mm(pd[:, 128:256], ident, K("negs"), start=False, stop=True)
                        A(DT[h][:], pd[:, 0:128], AF.Exp, bias=dp["ng"][:, h:h + 1])
                        A(egl[:, h:h + 1], pd[:, 127:128], AF.Exp)
                        A(DB[h][:], pd[:, 128:256], AF.Exp, bias=gb[:, h:h + 1])
                        Vts(ktail[:, h * 128:(h + 1) * 128], pk[:, h * 128:(h + 1) * 128], DT[h][:, 127:128], ALU.mult)
                        pq = PS()
                        mm(pq[:, 0:128], kT, kT)
                        mm(pq[:, 128:256], kT, qT)
                        mm(pq[:, 256:384], kT, st_gd[h][:])
                        Vstt(Nm[h][:], pq[:, 0:128], -1.0, DB[h][:], ALU.mult, ALU.mult)
                        Vtt(attnT[h][:], pq[:, 128:256], DT[h][:], ALU.mult)
                        pt = PS()
                        tp(pt[:, 0:128], Nm[h][:].bitcast(F32), ident)
                        Acp(NTm[h][:], pt[:, 0:128])
                        Vstt(Z[:, h * 128:(h + 1) * 128], pq[:, 256:384], neg[:, h:h + 1], v_tm[:, h * 128:(h + 1) * 128], ALU.mult, ALU.add)
                        Vts(Z[:, h * 128:(h + 1) * 128], Z[:, h * 128:(h + 1) * 128], beta[:, h:h + 1], ALU.mult)
                    if GDSTOP <= 2:
                        continue
                    neumann_apply(Nm, NTm, Z, 4, 128, "gd")
                    if GDSTOP <= 3:
                        continue
                    po1 = PS()
                    po2 = PS()
                    for h in range(4):
                        mm(po1[:, h * 128:(h + 1) * 128], qkv[:, h, cs], st_gd[h][:])
                        mm(po2[:, h * 128:(h + 1) * 128], attnT[h][:], Z[:, h * 128:(h + 1) * 128].bitcast(F32))
                    Vtt(o1[:].rearrange("p (h d) -> p h d", h=4), po1[:, 0:512].rearrange("p (h d) -> p h d", h=4),
                        dp["eg"][:, 0:4].unsqueeze(2).to_broadcast([128, 4, 128]), ALU.mult)
                    Vtt(o[:], o1[:], po2[:, 0:512], ALU.add)
                    o3 = o[:].rearrange("p (h d) -> p h d", h=4)
                    rstd = head_rms(o3, 4, 128, 1e-6, "gd")
                    Vtt(o3, o3, rstd[:, 0:4].unsqueeze(2).to_broadcast([128, 4, 128]), ALU.mult)
                    Vtt(o3, o3, RPv("gdnnw").unsqueeze(1).to_broadcast([128, 4, 128]), ALU.mult)
                    Vtt(o[:], o[:], zs[c][:], ALU.mult)
                    tm_to_fm_bf16(o, 12, c)
                    for h in range(4):
                        ps = PS()
                        mm(ps[:, 0:128], ktail[:, h * 128:(h + 1) * 128], Z[:, h * 128:(h + 1) * 128].bitcast(F32))
                        Vstt(st_gd[h][:], st_gd[h][:], egl[:, h:h + 1], ps[:, 0:128], ALU.mult, ALU.add)
                scopes.pop()

        def mixer_rwkv(l, mt):
            S_.barrier()
            with contextlib.ExitStack() as sc:
                scopes.append(sc)
                rkv = T("rw_rkv", [128, 12, NT])
                wam = T("rw_wam", [128, NT])
                rbuf = T("rw_rbuf", [128, NT + 1])
                dtl = T("rw_d", [128, NT])

                def shift_block(ps, idx, out_ap):
                    Acp(rbuf[:, 0:1], carry_rw[:, idx:idx + 1])
                    Acp(rbuf[:, 1:NT + 1], ps[:, 0:NT])
                    Vtt(dtl[:], rbuf[:, 0:NT], rbuf[:, 1:NT + 1], ALU.subtract)
                    Vstt(out_ap, dtl[:], pp[:, PP["mu"] + idx:PP["mu"] + idx + 1], rbuf[:, 1:NT + 1], ALU.mult, ALU.add)
                    Acp(carry_rw[:, idx:idx + 1], rbuf[:, NT:NT + 1])
                for j in range(3):
                    Wj = W(("in", l, O_RW + 512 * j, 512))
                    for b in range(4):
                        ps = PS()
                        proj_fm(Wj, b * 128, 128, ps)
                        shift_block(ps, 4 * j + b, rkv[:, 4 * j + b, :])
                Wwa = W(("in", l, O_RW + 1536, 128))
                ps = PS()
                proj_fm(Wwa, 0, 128, ps)
                shift_block(ps, 12, wam[:])
                A(wam[0:64, :], wam[0:64, :], AF.Tanh)
                Wz = W(("in", l, O_RW + 1664, 512))
                zs = [T(f"rw_z{c}", [128, 512]) for c in range(NCH)]
                for c in range(NCH):
                    ps = PS()
                    proj_tm(Wz, 0, 512, c, ps)
                    A(zs[c][:], ps[:, 0:512], AF.Silu)
                iclr = T("rw_iclr", [128, 4, NT])
                kk = T("rw_kk", [128, 4, NT])
                sq = T("rw_sq", [128, NT])
                rkr = T("rw_rkr", [128, 4, NT])
                for b in range(4):
                    ps = PS()
                    mm(ps[:, 0:NT], wau[64:128, b * 128:(b + 1) * 128], wam[64:128, :])
                    A(iclr[:, b, :], ps[:, 0:NT], AF.Sigmoid, bias=pp[:, PP["a0"] + b:PP["a0"] + b + 1])
                    km = rkv[:, 4 + b, :]
                    Vts(kk[:, b, :], km, pp[:, PP["kk"] + b:PP["kk"] + b + 1], ALU.mult)
                    Vtt(sq[:], kk[:, b, :], kk[:, b, :], ALU.mult)
                    ps2 = PS()
                    mm(ps2[:, 0:NT], K("bones"), sq[:])
                    rsqrt_(sq[:], ps2[:, 0:NT], 1.0, 1e-6)
                    Vtt(kk[:, b, :], kk[:, b, :], sq[:], ALU.mult)
                    Vts(sq[:], iclr[:, b, :], -1.0, ALU.add, pp[:, PP["ka"] + b:PP["ka"] + b + 1], ALU.mult)
                    Vstt(km, sq[:], 1.0, km, ALU.add, ALU.mult)
                    Vtt(iclr[:, b, :], iclr[:, b, :], kk[:, b, :], ALU.mult)
                    Vstt(rkr[:, b, :], rkv[:, b, :], pp[:, PP["rk"] + b:PP["rk"] + b + 1], km, ALU.mult, ALU.mult)
                bm = iclr
                lw = T("rw_lw", [128, 512])
                Gx = T("rw_G", [128, 4, 132])
                eG = T("rw_eG", [128, 4, 128])
                enG = T("rw_enG", [128, 4, 128])
                lwf = T("rw_lwf", [128, 4, 128])
                AR = T("rw_AR", [128, 4, 256])
                kp = T("rw_kp", [128, 4, 128])
                bp = T("rw_bp", [128, 4, 128])
                sc1 = T("rw_sc1", [128, 4])
                sc2 = T("rw_sc2", [128, 4])
                sc3 = T("rw_sc3", [128, 4])
                A0s = [T(f"rw_a0s{b}", [128, 128]) for b in range(4)]
                kpT = T("rw_kpT", [128, 512])
                bpT = T("rw_bpT", [128, 512])
                v_tm = T("rw_v", [128, 512])
                SC = [T(f"rw_SC{h}", [128, 512]) for h in range(8)]
                Nm = [T(f"rw_N{h}", [128, 128], F32R) for h in range(8)]
                NTm = [T(f"rw_NT{h}", [128, 128], F32R) for h in range(8)]
                Z = T("rw_Z", [128, 512], F32R)
                y = T("rw_y", [128, 512])
                bon = T("rw_bon", [128, 8])
                mv = T("rw_mv", [128, 8])
                tmpb = T("rw_tmpb", [128, 128])
                for c in range(NCH):
                    cs = slice(c * C, (c + 1) * C)
                    ps = PS()
                    mm(ps[:, 0:512], wam[0:64, cs], wau[0:64, :])
                    Vtt(lw[:], ps[:, 0:512], RPv("w0"), ALU.add)
                    A(lw[:], lw[:], AF.Sigmoid)
                    Vts(lw[:], lw[:], float(-np.exp(-0.5)), ALU.mult)
                    for b in range(4):
                        ps = PS()
                        mm(ps[:, 0:132], lw[:, b * 128:(b + 1) * 128], K("tmat"))
                        Acp(Gx[:, b, :], ps[:, 0:132])
                        ps2 = PS()
                        tp(ps2[:, 0:128], lw[:, b * 128:(b + 1) * 128], ident)
                        Vcp(lwf[:, b, :], ps2[:, 0:128])
                    A(eG[:], Gx[:, :, 0:128], AF.Exp)
                    A(enG[:], Gx[:, :, 0:128], AF.Exp, scale=-1.0)
                    A(sc1[:], Gx[:, :, 128], AF.Exp, scale=-1.0)
                    A(sc2[:], Gx[:, :, 127], AF.Exp)
                    Vtt(sc3[:], sc1[:], sc2[:], ALU.mult)
                    Vtt(AR[:, :, 128:256], rkv[:, 0:4, cs], eG[:], ALU.mult)
                    Vtt(kp[:], rkv[:, 4:8, cs], enG[:], ALU.mult)
                    Vtt(bp[:], bm[:, :, cs], enG[:], ALU.mult)
                    A(lwf[:], lwf[:], AF.Exp, scale=-1.0)
                    Vtt(lwf[:], lwf[:], eG[:], ALU.mult)
                    Vstt(AR[:, :, 0:128], kk[:, :, cs], -1.0, lwf[:], ALU.mult, ALU.mult)
                    for b in range(4):
                        Vts(A0s[b][:], st_rw[b][:], sc1[:, b:b + 1], ALU.mult)
                    ps = PS()
                    ps2 = PS()
                    ps3 = PS()
                    for b in range(4):
                        tp(ps[:, b * 128:(b + 1) * 128], kp[:, b, :], ident)
                        tp(ps2[:, b * 128:(b + 1) * 128], bp[:, b, :], ident)
                        tp(ps3[:, b * 128:(b + 1) * 128], rkv[:, 8 + b, cs], ident)
                    Acp(kpT[:], ps[:, 0:512])
                    Vcp(bpT[:], ps2[:, 0:512])
                    Acp(v_tm[:], ps3[:, 0:512])
                    psb = PS()
                    for b in range(4):
                        mm(psb[:, 2 * b:2 * b + 2], rkr[:, b, cs], K("sel2")[:, 0:2])
                    Acp(bon[:], psb[:, 0:8])
                    for h in range(8):
                        b, r0 = h // 2, 64 * (h % 2)
                        rows = slice(r0, r0 + 64)
                        pa = PS()
                        mm(pa[:, 0:256], bp[rows, b, :], AR[rows, b, :])
                        mm(pa[:, 256:512], kp[rows, b, :], AR[rows, b, :])
                        Vtt(SC[h][:], pa[:, 0:512], K("maskA"), ALU.mult)
                        pn = PS()
                        mm(pn[:, 0:128], AR[rows, b, 0:128], bp[rows, b, :])
                        Vtt(Nm[h][:], pn[:, 0:128], K("sl"), ALU.mult)
                        Acp(NTm[h][:], SC[h][:, 0:128])
                    pz = PS()
                    for b in range(4):
                        mm(pz[:, b * 128:(b + 1) * 128], AR[:, b, 0:128], A0s[b][:], start=True, stop=False)
                        for hh in range(2):
                            h = 2 * b + hh
                            mm(pz[:, h * 64:(h + 1) * 64], SC[h][:, 256:384], v_tm[:, h * 64:(h + 1) * 64], start=False, stop=(hh == 1))
                    Acp(Z[:], pz[:, 0:512])
                    import os
                    if os.environ.get("RWDBG", "") == "pv":
                        pvs = T("rw_pvs", [128, 512])
                        Vcp(pvs[:], Z[:])
                    neumann_apply(Nm, NTm, Z, 8, 64, "rw")
                    py = PS()
                    for b in range(4):
                        mm(py[:, b * 128:(b + 1) * 128], AR[:, b, 128:256], A0s[b][:], start=True, stop=False)
                        for hh in range(2):
                            h = 2 * b + hh
                            mm(py[:, h * 64:(h + 1) * 64], SC[h][:, 384:512], v_tm[:, h * 64:(h + 1) * 64], start=False, stop=False)
                            mm(py[:, h * 64:(h + 1) * 64], SC[h][:, 128:256], Z[:, h * 64:(h + 1) * 64].bitcast(F32), start=False, stop=(hh == 1))
                    Acp(y[:], py[:, 0:512])
                    for b in range(4):
                        ps = PS()
                        mm(ps[:, 0:128], kpT[:, b * 128:(b + 1) * 128], v_tm[:, b * 128:(b + 1) * 128], start=True, stop=False)
                        mm(ps[:, 0:128], bpT[:, b * 128:(b + 1) * 128], Z[:, b * 128:(b + 1) * 128].bitcast(F32), start=False, stop=True)
                        Vstt(tmpb[:], ps[:, 0:128], sc2[:, b:b + 1], K("bones"), ALU.mult, ALU.mult)
                        Vstt(st_rw[b][:], st_rw[b][:], sc3[:, b:b + 1], tmpb[:], ALU.mult, ALU.add)
                    y3 = y[:].rearrange("p (h d) -> p h d", h=8)
                    Vred(mv[:], y3)
                    Vts(mv[:], mv[:], float(-1.0 / 64), ALU.mult)
                    Vtt(y3, y3, mv[:, 0:8].unsqueeze(2).to_broadcast([128, 8, 64]), ALU.add)
                    rstd = head_rms(y3, 8, 64, 64e-5, "rw", sq=kpT)
                    Vtt(y3, y3, rstd[:, 0:8].unsqueeze(2).to_broadcast([128, 8, 64]), ALU.mult)
                    Vtt(y[:], y[:], RPv("lnw"), ALU.mult)
                    Vtt(y[:], y[:], RPv("lnb"), ALU.add)
                    v3 = v_tm[:].rearrange("p (h d) -> p h d", h=8)
                    Vtt(Z[:].rearrange("p (h d) -> p h d", h=8), v3, bon[:, 0:8].unsqueeze(2).to_broadcast([128, 8, 64]), ALU.mult) if False else None
                    bz = bpT
                    Vtt(bz[:].rearrange("p (h d) -> p h d", h=8), v3, bon[:, 0:8].unsqueeze(2).to_broadcast([128, 8, 64]), ALU.mult)
                    Vtt(y[:], y[:], bz[:], ALU.add)
                    Vtt(y[:], y[:], zs[c][:], ALU.mult)
                    import os
                    dsel = os.environ.get("RWDBG", "")
                    if dsel == "py":
                        Acp(y[:], py[:, 0:512])
                    elif dsel == "z":
                        Vcp(y[:], Z[:])
                    elif dsel == "v":
                        Vcp(y[:], v_tm[:])
                    elif dsel == "lw":
                        Vcp(y[:], lw[:])
                    elif dsel == "kpT":
                        Vcp(y[:], kpT[:])
                    elif dsel == "pv":
                        Vcp(y[:], pvs[:])
                    elif dsel == "bon":
                        Vcp(y[:], bz[:])
                    tm_to_fm_bf16(y, 0, c)
                scopes.pop()

        def merge_out(l, mt, xsrc, xdst, is_last):
            S_.barrier()
            with contextlib.ExitStack() as sc:
                scopes.append(sc)
                macc = T("mg_acc", [128, 8, NT])
                mT = T("mg_mT", [128, 8, NT], BF16)
                sgs = [T(f"mg_sg{i}", [128, NT]) for i in range(2)]
                tts = [T(f"mg_t{i}", [128, NT]) for i in range(2)]
                for br in range(4):
                    Wb = W(("br", l, br, 0))
                    Wg = [None, None]
                    for half in range(2):
                        Wg[half] = W(("in", l, O_GATE + br * 1024 + 512 * half, 512), live=1 + half)
                        for d4 in range(4):
                            dmb = half * 4 + d4
                            sg, tt_ = sgs[d4 % 2], tts[d4 % 2]
                            pg = PS()
                            proj_fm(Wg[half], d4 * 128, 128, pg)
                            A(sg[:], pg[:, 0:NT], AF.Sigmoid)
                            pb_ = PS()
                            for kc in range(4):
                                mm(pb_[:, 0:NT], Wb[:, kc, dmb * 128:(dmb + 1) * 128], u_all[:, br * 4 + kc, :], start=(kc == 0), stop=(kc == 3))
                            if br == 0:
                                Vtt(macc[:, dmb, :], sg[:], pb_[:, 0:NT], ALU.mult)
                            elif br < 3:
                                Vtt(tt_[:], sg[:], pb_[:, 0:NT], ALU.mult)
                                Vtt(macc[:, dmb, :], macc[:, dmb, :], tt_[:], ALU.add)
                            else:
                                Vtt(tt_[:], sg[:], pb_[:, 0:NT], ALU.mult)
                                Vtt(mT[:, dmb, :], macc[:, dmb, :], tt_[:], ALU.add)
                for half in range(2):
                    Wo = W(("out", l, 512 * half, 512))
                    for c in range(NCH):
                        ps = PS()
                        for kc in range(8):
                            mm(ps[:, 0:512], mT[:, kc, c * C:(c + 1) * C], Wo[:, kc, :], start=(kc == 0), stop=(kc == 7))
                        Vtt(xt[:, c, half * 512:(half + 1) * 512], xt[:, c, half * 512:(half + 1) * 512], ps[:, 0:512], ALU.add)
                if is_last and final_norm:
                    junk = T("fn_junk", [128, D])
                    ssf = T("fn_ss", [128, NCH])
                    for c in range(NCH):
                        A(junk[:], xt[:, c, :], AF.Square, accum=ssf[:, c:c + 1])
                    rsqrt_(ssf[:], ssf[:], 1.0 / D, 1e-6)
                    for c in range(NCH):
                        Vstt(xt[:, c, :], xt[:, c, :], ssf[:, c:c + 1], finw[:], ALU.mult, ALU.mult)
                rows = slice(mt * NT, (mt + 1) * NT)
                dma("sp", xdst[rows, :].rearrange("(c p) d -> p c d", p=128), xt[:], sem_o, ins=[xt], outs=[("xd", id(xdst), mt)])
                scopes.pop()

        nl = len(layers)
        for li, l in enumerate(layers):
            load_params(l)
            xsrc = dr["x"] if li == 0 else scr[(li - 1) % 2]
            is_last = li == nl - 1
            xdst = out if is_last else scr[li % 2]
            for mt in range(NMT):
                rows = slice(mt * NT, (mt + 1) * NT)
                dma("sp", xt[:], xsrc[rows, :].rearrange("(c p) d -> p c d", p=128), sem_x,
                    ins=[("xd", id(xsrc), mt)], outs=[xt])
                dma("sp", ropet[:], dr["rope"][:, :, rows], sem_r, outs=[ropet])
                with contextlib.ExitStack() as sc:
                    scopes.append(sc)
                    S_.barrier()
                    junk = T("n_junk", [128, D])
                    ss = T("n_ss", [128, NCH])
                    hb = T("n_hb", [128, D], BF16)
                    for c in range(NCH):
                        A(junk[:], xt[:, c, :], AF.Square, accum=ss[:, c:c + 1])
                    rsqrt_(ss[:], ss[:], 1.0 / D, 1e-6)
                    for c in range(NCH):
                        Vstt(hb[:], xt[:, c, :], ss[:, c:c + 1], normw[:], ALU.mult, ALU.mult)
                        for kc in range(8):
                            tp(pbt[:, kc * 128:(kc + 1) * 128], hb[:, kc * 128:(kc + 1) * 128], identb[:])
                        CP(hT[:, :, c * C:(c + 1) * C], pbt[:, 0:1024].rearrange("p (k t) -> p k t", k=8))
                    scopes.pop()
                if "rwkv" in mixers:
                    mixer_rwkv(l, mt)
                if "ret" in mixers:
                    mixer_ret(l, mt)
                if "ssd" in mixers:
                    mixer_ssd(l, mt)
                if "gdn" in mixers:
                    mixer_gdn(l, mt)
                if dbg and is_last:
                    dma("sp", dbg_u[:, rows].rearrange("(b p) t -> p b t", p=128), u_all[:], sem_d, ins=[u_all], outs=[("dbg", mt)])
                merge_out(l, mt, xsrc, xdst, is_last)
        fin_ins = [("xd", id(out), mt) for mt in range(NMT)]
        if dbg:
            fin_ins += [("dbg", mt) for mt in range(NMT)]
        add("sp", lambda e: e.nop(), ins=fin_ins)
        assert jstate["used"] == len(jobs)
        S_.emit()
        build.stats = S_.stats
    return nc


NT_DEFAULT = 256


def kernel(**inputs):
    x = np.ascontiguousarray(np.asarray(inputs["x"], dtype=np.float32))
    B, S, _ = x.shape
    cst, rope = make_consts(S)
    params = {n: np.ascontiguousarray(np.asarray(inputs[n], dtype=np.float32)) for n in PARAM_NAMES}
    nc = build(S, list(range(DEPTH)), NT_DEFAULT, True)
    in_maps = []
    for b in range(B):
        m = {"x": x[b], "cst": cst, "rope": rope}
        m.update(params)
        in_maps.append(m)
    res = run_bass_kernel_spmd(nc, in_maps, core_ids=list(range(B)))
    return np.stack([np.asarray(r["out"], dtype=np.float32) for r in res.results], axis=0)
```

```python
import contextlib
import numpy as np
import concourse.bass as bass
import concourse.mybir as mybir
from concourse.bass_utils import run_bass_kernel_spmd

F32 = mybir.dt.float32
F32R = mybir.dt.float32r
BF16 = mybir.dt.bfloat16
ALU = mybir.AluOpType
AF = mybir.ActivationFunctionType
AX = mybir.AxisListType

D = 1024
NIN = 11408
DEPTH = 4
SEQ = 4096
BATCH = 8
C = 128
O_RW, O_RET, O_SSD, O_GDN, O_GATE = 0, 2176, 3712, 5256, 7312


class Op:
    __slots__ = ("eng", "fn", "deps", "needed", "dma_sem", "token", "group", "is_dma")


class Group:
    def __init__(self):
        self.n = 0
        self.token = None


class Sched:
    ROT = 3500
    DROT = 3488

    def __init__(self, nc, stack, same_sync=("act", "dve", "pool")):
        self.nc = nc
        self.stack = stack
        self.ops = []
        self.w = {}
        self.r = {}
        self.same_sync = set(same_sync)
        self.nsem = 0

    def new_sem(self, name):
        self.nsem += 1
        return self.stack.enter_context(self.nc.semaphore(f"{name}_{self.nsem}"))

    @staticmethod
    def key(a):
        if isinstance(a, (tuple, str)):
            return a
        return a.name

    def add(self, eng, fn, ins=(), outs=(), dma_sem=None, group=None):
        op = Op()
        op.eng = eng
        op.fn = fn
        op.needed = False
        op.dma_sem = dma_sem
        op.is_dma = dma_sem is not None
        op.group = group
        op.token = None
        if group is not None:
            group.n += 1
        ikeys = [self.key(a) for a in ins if a is not None]
        okeys = [self.key(a) for a in outs if a is not None]
        ikeys.append("EPOCH")
        deps = []
        for k in ikeys:
            deps += self.w.get(k, [])
        for k in okeys:
            deps += self.w.get(k, [])
            rd = self.r.get(k)
            if rd:
                deps += list(rd[0].values())
                deps += rd[1]
        fdeps = []
        seen = set()
        for d in deps:
            if id(d) in seen or d is op:
                continue
            seen.add(id(d))
            if group is not None and d.group is group:
                continue
            if (not d.is_dma) and d.eng == eng and eng not in self.same_sync:
                continue
            d.needed = True
            fdeps.append(d)
        op.deps = fdeps
        for k in ikeys:
            rd = self.r.setdefault(k, [{}, []])
            if op.is_dma:
                rd[1].append(op)
            else:
                rd[0][eng] = op
        for k in okeys:
            cur = self.w.get(k, [])
            if group is not None and cur and all(c.group is group for c in cur):
                cur.append(op)
                self.w[k] = cur
            else:
                self.w[k] = [op]
            self.r[k] = [{}, []]
        self.ops.append(op)
        return op

    def barrier(self):
        self.add("dve", lambda e: e.engine_nop(), outs=["EPOCH"])

    def emit(self):
        nc = self.nc
        cnt, cursem, dcount, waited, dsem = {}, {}, {}, {}, {}
        per_eng = {e: [] for e in ("pe", "act", "dve", "pool", "sp")}
        nwaits = 0
        for op in self.ops:
            waits = {}
            for d in op.deps:
                sem, val = d.token
                k = id(sem)
                if k not in waits or waits[k][1] < val:
                    waits[k] = (sem, val)
            wl = []
            for k, (sem, val) in waits.items():
                wk = (op.eng, k)
                if waited.get(wk, 0) >= val:
                    continue
                waited[wk] = val
                wl.append((sem, val))
            nwaits += len(wl)
            inc = None
            if op.is_dma:
                fam = id(op.dma_sem)
                g = op.group
                need = 16 * (g.n if (g is not None and g.token is None) else 1)
                if fam not in dsem:
                    dsem[fam] = op.dma_sem
                    dcount[fam] = 0
                if (g is None or g.token is None) and dcount[fam] + need > self.DROT:
                    dsem[fam] = self.new_sem("dr")
                    dcount[fam] = 0
                sem = dsem[fam]
                if g is not None:
                    if g.token is None:
                        g.token = (sem, dcount[fam] + 16 * g.n)
                    sem = g.token[0]
                    dcount[fam] += 16
                    op.token = g.token
                else:
                    dcount[fam] += 16
                    op.token = (sem, dcount[fam])
                inc = (sem, 16)
            elif op.needed:
                e = op.eng
                if e not in cursem or cnt[e] >= self.ROT:
                    cursem[e] = self.new_sem("e" + e)
                    cnt[e] = 0
                cnt[e] += 1
                op.token = (cursem[e], cnt[e])
                inc = (cursem[e], 1)
            per_eng[op.eng].append((wl, op.fn, inc))
            op.deps = None
        self.stats = dict(n_ops=len(self.ops), n_waits=nwaits, n_sems=self.nsem,
                          per_eng={e: len(v) for e, v in per_eng.items()})

        def run(e, lst):
            for wl, fn, inc in lst:
                for sem, val in wl:
                    e.wait_ge(sem, val)
                ins = fn(e)
                if inc is not None:
                    ins.then_inc(inc[0], inc[1])

        with nc.Block() as block:
            @block.tensor
            def _(e):
                run(e, per_eng["pe"])

            @block.scalar
            def _(e):
                run(e, per_eng["act"])

            @block.vector
            def _(e):
                run(e, per_eng["dve"])

            @block.gpsimd
            def _(e):
                run(e, per_eng["pool"])

            @block.sync
            def _(e):
                run(e, per_eng["sp"])


CST = {}


def _cst_layout():
    off = 0
    for name, n in [("ident", 128), ("maskA", 512), ("iu", 128), ("niu", 128), ("negt", 128),
                    ("negs", 128), ("sl", 128), ("tmat", 132), ("ones", 128), ("bones", 128),
                    ("sel2", 8), ("swapp", 128), ("dm2", 1152), ("qdec", 384), ("ktbl", 8),
                    ("tblc", 768), ("mask01", 256)]:
        CST[name] = (off, n)
        off += n
    return off


NCST = _cst_layout()


def make_consts(S):
    c = np.zeros((128, NCST), np.float32)

    def put(name, arr):
        o, n = CST[name]
        c[:, o:o + arr.shape[1]] = arr

    i = np.arange(128)
    su = (i[:, None] < i[None, :]).astype(np.float32)
    iu = (i[:, None] <= i[None, :]).astype(np.float32)
    put("ident", np.eye(128, dtype=np.float32))
    put("maskA", np.concatenate([su, iu, su, iu], axis=1))
    put("iu", iu)
    put("niu", -iu)
    put("negt", np.where(i[:, None] <= i[None, :], 0.0, -1e30).astype(np.float32))
    put("negs", np.where(i[None, :] < i[:, None], 0.0, -1e30).astype(np.float32))
    put("sl", su.T.copy())
    tm = np.zeros((128, 132), np.float32)
    m = 63
    tm[:, :128] = iu - (i[:, None] <= m).astype(np.float32)
    tm[:, 128] = -(i <= m).astype(np.float32)
    put("tmat", tm)
    put("ones", np.ones((128, 128), np.float32))
    bo = np.zeros((128, 128), np.float32)
    bo[:64, :64] = 1
    bo[64:, 64:] = 1
    put("bones", bo)
    s2 = np.zeros((128, 8), np.float32)
    s2[:64, 0] = 1
    s2[64:, 1] = 1
    put("sel2", s2)
    sp = np.zeros((128, 128), np.float32)
    sp[i, i ^ 1] = 1
    put("swapp", sp)
    gam = 1.0 - np.exp2(-5.0 - np.arange(8, dtype=np.float64))
    lg = np.log(gam)
    scale = 32 ** -0.5
    dm2 = np.zeros((128, 3, 3, 128), np.float64)
    for h in range(8):
        r, bq = h % 3, h // 3
        dm2[:, r, bq, :] = np.where(i[:, None] <= i[None, :], np.exp(lg[h] * (i[None, :] - i[:, None])), 0.0) * scale
    put("dm2", dm2.reshape(128, 1152).astype(np.float32))
    qd = np.ones((128, 3, 128), np.float64)
    for bq in range(3):
        for p in range(96):
            h = bq * 3 + p // 32
            if h < 8:
                qd[p, bq, :] = np.exp(lg[h] * (i + 1))
    put("qdec", qd.reshape(128, 384).astype(np.float32))
    kt = np.zeros((128, 8), np.float64)
    for h in range(8):
        kt[:, h] = np.exp(lg[h] * (127 - i)) * scale
    put("ktbl", kt.astype(np.float32))
    tc_ = np.zeros((128, 3, 256), np.float64)
    m01 = np.zeros((128, 256), np.float32)
    for hh in range(4):
        m01[hh * 32:(hh + 1) * 32, hh * 64:(hh + 1) * 64] = 1
    for bq in range(3):
        for hh in range(3):
            h = bq * 3 + hh
            if h < 8:
                tc_[hh * 32:(hh + 1) * 32, bq, hh * 64:(hh + 1) * 64] = np.exp(lg[h] * 128)
    put("tblc", tc_.reshape(128, 768).astype(np.float32))
    put("mask01", m01)
    half = 16
    angle = 1.0 / (10000.0 ** np.linspace(0.0, 1.0, half, dtype=np.float32)).astype(np.float32)
    theta = np.arange(S, dtype=np.float32)[:, None] * angle[None, :]
    cos = np.cos(theta).astype(np.float32)
    sin = np.sin(theta).astype(np.float32)
    rope = np.zeros((128, 2, S), np.float32)
    for p in range(128):
        ii = (p % 32) // 2
        rope[p, 0, :] = cos[:, ii]
        rope[p, 1, :] = (-sin[:, ii]) if (p % 2 == 0) else sin[:, ii]
    return c, rope


PARAM_NAMES = ["norm_w", "w_in", "rwkv_mu_rkv", "rwkv_mu_wa", "rwkv_w_up", "rwkv_w0", "rwkv_a_up", "rwkv_a0",
               "rwkv_k_k", "rwkv_k_a", "rwkv_r_k", "rwkv_ln_w", "rwkv_ln_b", "ret_norm_w", "ssd_conv_w",
               "ssd_conv_b", "ssd_dt_bias", "ssd_A_log", "ssd_D", "ssd_norm_w", "gdn_conv_w", "gdn_dt_bias",
               "gdn_A_log", "gdn_norm_w", "w_branch", "w_out", "final_norm_w"]
PARAM_SHAPES = {
    "norm_w": [4, 1024], "w_in": [4, 1024, NIN], "rwkv_mu_rkv": [4, 3, 512], "rwkv_mu_wa": [4, 2, 64],
    "rwkv_w_up": [4, 64, 512], "rwkv_w0": [4, 512], "rwkv_a_up": [4, 64, 512], "rwkv_a0": [4, 512],
    "rwkv_k_k": [4, 512], "rwkv_k_a": [4, 512], "rwkv_r_k": [4, 8, 64], "rwkv_ln_w": [4, 512],
    "rwkv_ln_b": [4, 512], "ret_norm_w": [4, 512], "ssd_conv_w": [4, 4, 1024], "ssd_conv_b": [4, 1024],
    "ssd_dt_bias": [4, 8], "ssd_A_log": [4, 8], "ssd_D": [4, 8], "ssd_norm_w": [4, 512],
    "gdn_conv_w": [4, 4, 1536], "gdn_dt_bias": [4, 4], "gdn_A_log": [4, 4], "gdn_norm_w": [4, 128],
    "w_branch": [4, 4, 512, 1024], "w_out": [4, 1024, 1024], "final_norm_w": [1024],
}

RP = {}


def _rp_layout():
    off = 0
    for name, n in [("w0", 512), ("lnw", 512), ("lnb", 512), ("retnw", 512), ("ssdnw", 512), ("gdnnw", 128),
                    ("ssdD", 8), ("ssddtb", 8), ("ssdA", 8), ("gdndtb", 4), ("gdnA", 4)]:
        RP[name] = (off, n)
        off += n
    return off


NRP = _rp_layout()
PP = {"mu": 0, "kk": 13, "ka": 17, "rk": 21, "a0": 25, "scw": 29, "scb": 61, "gcw": 69}
NPP = 128


def build(S, layers, NT, final_norm, mixers=("rwkv", "ret", "ssd", "gdn"), dbg=False, same_sync=True,
          nslot=3, n_param_layers=DEPTH, lmap=None):
    lmap = lmap or {l: l for l in range(DEPTH)}
    NCH = NT // C
    NMT = S // NT
    nc = bass.Bass("TRN2", target_bir_lowering=False)
    dr = {}
    dr["x"] = nc.dram_tensor("x", [S, D], F32, kind="ExternalInput").ap()
    for n in PARAM_NAMES:
        shp = list(PARAM_SHAPES[n])
        if n != "final_norm_w":
            shp[0] = n_param_layers
        dr[n] = nc.dram_tensor(n, shp, F32, kind="ExternalInput").ap()
    dr["cst"] = nc.dram_tensor("cst", [128, NCST], F32, kind="ExternalInput").ap()
    dr["rope"] = nc.dram_tensor("rope", [128, 2, S], F32, kind="ExternalInput").ap()
    out = nc.dram_tensor("out", [S, D], F32, kind="ExternalOutput").ap()
    scr = [nc.dram_tensor(f"scr{i}", [S, D], F32, kind="Internal").ap() for i in range(2)] if len(layers) > 1 else []
    if dbg:
        dbg_u = nc.dram_tensor("dbg_u", [16 * 128, S], BF16, kind="ExternalOutput").ap()

    with contextlib.ExitStack() as st:
        S_ = Sched(nc, st, same_sync=("act", "dve", "pool") if same_sync else ())
        add = S_.add
        scopes = [st]
        tcount = [0]
        tcache = {}

        def T(name, shape, dt=F32):
            ck = (id(scopes[-1]), name)
            if ck in tcache:
                return tcache[ck]
            tcount[0] += 1
            t_ = scopes[-1].enter_context(nc.sbuf_tensor(f"s{tcount[0]}_{name}", shape, dt))
            tcache[ck] = t_
            return t_

        pbs = [st.enter_context(nc.psum_tensor(f"pb{i}", [128, 512], F32)) for i in range(7)]
        pbt = st.enter_context(nc.psum_tensor("pbt", [128, 1024], BF16))
        pstate = [0]

        def PS():
            p = pbs[pstate[0] % 7]
            pstate[0] += 1
            return p

        def mm(out_, lhsT, rhs, start=True, stop=True):
            add("pe", lambda e: e.matmul(out_, lhsT, rhs, start=start, stop=stop), ins=[lhsT, rhs], outs=[out_])

        def tp(out_, in_, ident):
            add("pe", lambda e: e.transpose(out_, in_, ident), ins=[in_, ident], outs=[out_])

        def A(out_, in_, func, bias=None, scale=None, accum=None):
            kw = {}
            ins = [in_]
            if bias is not None:
                kw["bias"] = bias
                if not isinstance(bias, float):
                    ins.append(bias)
            if scale is not None:
                kw["scale"] = scale
                if not isinstance(scale, float):
                    ins.append(scale)
            outs = [out_]
            if accum is not None:
                kw["accum_out"] = accum
                outs.append(accum)
            add("act", lambda e: e.activation(out_, in_, func, **kw), ins=ins, outs=outs)

        def Acp(out_, in_):
            add("act", lambda e: e.copy(out_, in_), ins=[in_], outs=[out_])

        def Vcp(out_, in_):
            add("dve", lambda e: e.tensor_copy(out_, in_), ins=[in_], outs=[out_])

        def Vtt(out_, a, b, op):
            add("dve", lambda e: e.tensor_tensor(out_, a, b, op), ins=[a, b], outs=[out_])

        def Vts(out_, a, s1, op0, s2=None, op1=None):
            ins = [a] + [s for s in (s1, s2) if s is not None and not isinstance(s, float)]
            if op1 is None:
                add("dve", lambda e: e.tensor_scalar(out_, a, s1, None, op0), ins=ins, outs=[out_])
            else:
                add("dve", lambda e: e.tensor_scalar(out_, a, s1, s2, op0, op1), ins=ins, outs=[out_])

        def Vstt(out_, in0, scalar, in1, op0, op1):
            ins = [in0, in1] + ([] if isinstance(scalar, float) else [scalar])
            add("dve", lambda e: e.scalar_tensor_tensor(out_, in0, scalar, in1, op0, op1), ins=ins, outs=[out_])

        def Vred(out_, in_):
            add("dve", lambda e: e.reduce_sum(out_, in_, AX.X), ins=[in_], outs=[out_])

        def Vrec(out_, in_):
            add("dve", lambda e: e.reciprocal(out_, in_), ins=[in_], outs=[out_])

        def Vset(out_, val):
            add("dve", lambda e: e.memset(out_, val), outs=[out_])

        def rsqrt_(out_, in_, mult, eps):
            Vts(out_, in_, mult, ALU.mult, eps, ALU.add)
            Vrec(out_, out_)
            A(out_, out_, AF.Sqrt)

        cpflip = [0]

        def CP(out_, in_):
            cpflip[0] ^= 1
            (Acp if cpflip[0] else Vcp)(out_, in_)

        def dma(eng, out_, in_, sem, ins=(), outs=(), group=None, slow=False):
            if slow:
                add(eng, lambda e: e.dma_start(out=out_, in_=in_, allow_slow_non_contiguous=True), ins=ins, outs=outs, dma_sem=sem, group=group)
            else:
                add(eng, lambda e: e.dma_start(out=out_, in_=in_), ins=ins, outs=outs, dma_sem=sem, group=group)

        cst = T("cst", [128, NCST])
        sem_c = S_.new_sem("cst")
        g0 = Group()
        dma("sp", cst[:, 0:NCST // 2], dr["cst"][:, 0:NCST // 2], sem_c, outs=[cst], group=g0)
        dma("sp", cst[:, NCST // 2:NCST], dr["cst"][:, NCST // 2:NCST], sem_c, outs=[cst], group=g0)

        def K(name, a=0, b=None):
            o, n = CST[name]
            if b is None:
                b = n
            return cst[:, o + a:o + b]

        ident = K("ident")
        identb = T("identb", [128, 128], BF16)
        Vcp(identb[:], ident)
        finw = T("finw", [128, D])
        sem_f = S_.new_sem("finw")
        if final_norm:
            dma("sp", finw[:], dr["final_norm_w"].partition_broadcast(128), sem_f, outs=[finw])

        xt = T("xt", [128, NCH, D])
        hT = T("hT", [128, 8, NT], BF16)
        u_all = T("u_all", [128, 16, NT], BF16)
        normw = T("normw", [128, D])
        pp = T("pp", [128, NPP])
        rp = T("rp", [128, NRP])
        wau = T("wau", [128, 512])
        ropet = T("ropet", [128, 2, NT])
        slots = [T(f"wslot{i}", [128, 4096], BF16) for i in range(nslot)]
        slot_sem = [S_.new_sem(f"ws{i}") for i in range(nslot)]
        sem_x = S_.new_sem("x")
        sem_o = S_.new_sem("o")
        sem_p = S_.new_sem("p")
        sem_r = S_.new_sem("rope")
        sem_d = S_.new_sem("dbg")
        carry_rw = T("carry_rw", [128, 16])
        carry_sd = T("carry_sd", [128, 8, 3])
        carry_gd = T("carry_gd", [128, 12, 3])
        st_rw = [T(f"st_rw{b}", [128, 128]) for b in range(4)]
        st_ret = [T(f"st_ret{b}", [128, 256]) for b in range(3)]
        st_sd = [T(f"st_sd{g}", [128, 256]) for g in range(2)]
        st_gd = [T(f"st_gd{h}", [128, 128]) for h in range(4)]

        if len(mixers) < 4:
            Vset(u_all[:], 0.0)

        def RPv(name, a=0, b=None):
            o, n = RP[name]
            if b is None:
                b = n
            return rp[:, o + a:o + b]

        jobs = []
        for l in layers:
            for mt in range(NMT):
                if "rwkv" in mixers:
                    jobs += [("in", l, O_RW + 0, 512), ("in", l, O_RW + 512, 512), ("in", l, O_RW + 1024, 512),
                             ("in", l, O_RW + 1536, 128), ("in", l, O_RW + 1664, 512)]
                if "ret" in mixers:
                    jobs += [("in", l, O_RET, 512), ("in", l, O_RET + 512, 512), ("in", l, O_RET + 1024, 512)]
                if "ssd" in mixers:
                    jobs += [("in", l, O_SSD, 512), ("in", l, O_SSD + 512, 512), ("in", l, O_SSD + 1024, 512),
                             ("in", l, O_SSD + 1536, 8)]
                if "gdn" in mixers:
                    jobs += [("in", l, O_GDN, 512), ("in", l, O_GDN + 512, 512), ("in", l, O_GDN + 1024, 512),
                             ("in", l, O_GDN + 1536, 512), ("in", l, O_GDN + 2048, 8)]
                for br in range(4):
                    jobs += [("br", l, br, 0), ("in", l, O_GATE + br * 1024, 512), ("in", l, O_GATE + br * 1024 + 512, 512)]
                jobs += [("out", l, 0, 512), ("out", l, 512, 512)]
        jstate = {"issued": 0, "used": 0}
        stg = [T(f"wstg{i}", [128, 2048]) for i in range(2)]
        stg_sem = [S_.new_sem(f"wstg{i}") for i in range(2)]

        def job_src(j, hh):
            kind, l, a, n = jobs[j]
            l = lmap[l]
            if kind == "in":
                src = dr["w_in"][l][:, a:a + n].rearrange("(k p) c -> p k c", p=128)
                return src[:, hh * 4:(hh + 1) * 4, :], 4, n
            if kind == "br":
                src = dr["w_branch"][l][a].rearrange("(k p) c -> p k c", p=128)
                return src[:, hh * 2:(hh + 1) * 2, :], 2, 1024
            src = dr["w_out"][l][:, a:a + n].rearrange("(k p) c -> p k c", p=128)
            return src[:, hh * 4:(hh + 1) * 4, :], 4, n

        def issue_dma(j):
            for hh in range(2):
                src, nk, n = job_src(j, hh)
                sv = stg[hh][:, 0:nk * n].rearrange("p (k c) -> p k c", k=nk)
                dma("sp", sv, src, stg_sem[hh], outs=[stg[hh]])

        def issue_cast(j):
            sl = slots[j % nslot]
            for hh in range(2):
                src, nk, n = job_src(j, hh)
                w_ = nk * n
                (Acp if hh == 0 else Vcp)(sl[:, hh * w_:(hh + 1) * w_], stg[hh][:, 0:w_])

        def W(desc, live=0):
            j = jstate["used"]
            assert jobs[j] == desc, (jobs[j], desc)
            assert live < nslot - 0
            if j == 0:
                issue_dma(0)
            issue_cast(j)
            if j + 1 < len(jobs):
                issue_dma(j + 1)
            jstate["used"] += 1
            kind, l, a, n = desc
            sl = slots[j % nslot]
            if kind == "br":
                return sl[:, 0:4096].rearrange("p (k c) -> p k c", k=4)
            return sl[:, 0:8 * n].rearrange("p (k c) -> p k c", k=8)

        def proj_fm(Wv, col0, nrows, ps, t0=0, nt=None):
            nt = NT if nt is None else nt
            for kc in range(8):
                mm(ps[:nrows, :nt], Wv[:, kc, col0:col0 + nrows], hT[:, kc, t0:t0 + nt], start=(kc == 0), stop=(kc == 7))

        def proj_tm(Wv, col0, ncols, c, ps):
            for kc in range(8):
                mm(ps[:, :ncols], hT[:, kc, c * C:(c + 1) * C], Wv[:, kc, col0:col0 + ncols], start=(kc == 0), stop=(kc == 7))

        def u_store(u_tm, blk0):
            raise NotImplementedError

        def tm_to_fm_bf16(src_tm, blk0, c):
            ps = PS()
            for b in range(4):
                tp(ps[:, b * 128:(b + 1) * 128], src_tm[:, b * 128:(b + 1) * 128], ident)
            CP(u_all[:, blk0:blk0 + 4, c * C:(c + 1) * C], ps[:, 0:512].rearrange("p (b t) -> p b t", b=4))

        def head_rms(y_ap3, nh, hd, eps, tagscope, sq=None):
            if sq is None:
                sq = T(f"sq_{tagscope}", [128, nh * hd])
            Vtt(sq[:].rearrange("p (h d) -> p h d", h=nh), y_ap3, y_ap3, ALU.mult)
            ss = T(f"ss_{tagscope}", [128, nh])
            Vred(ss[:], sq[:].rearrange("p (h d) -> p h d", h=nh))
            rsqrt_(ss[:], ss[:], 1.0 / hd, eps)
            return ss

        def load_params(l):
            l = lmap[l]
            g = Group()
            dma("sp", normw[:], dr["norm_w"][l].partition_broadcast(128), sem_p, outs=[normw], group=g)
            for name, src in [("w0", dr["rwkv_w0"][l]), ("lnw", dr["rwkv_ln_w"][l]), ("lnb", dr["rwkv_ln_b"][l]),
                              ("retnw", dr["ret_norm_w"][l]), ("ssdnw", dr["ssd_norm_w"][l]),
                              ("gdnnw", dr["gdn_norm_w"][l]), ("ssdD", dr["ssd_D"][l]),
                              ("ssddtb", dr["ssd_dt_bias"][l]), ("ssdA", dr["ssd_A_log"][l]),
                              ("gdndtb", dr["gdn_dt_bias"][l]), ("gdnA", dr["gdn_A_log"][l])]:
                dma("sp", RPv(name), src.partition_broadcast(128), sem_p, outs=[rp], group=g)
            dma("sp", wau[0:64, :], dr["rwkv_w_up"][l], sem_p, outs=[wau], group=g)
            dma("sp", wau[64:128, :], dr["rwkv_a_up"][l], sem_p, outs=[wau], group=g)

            def ppl(col, src, nb):
                dma("sp", pp[:, col:col + nb], src.rearrange("(b p) -> p b", p=128), sem_p, outs=[pp], group=g, slow=True)
            for j in range(3):
                ppl(PP["mu"] + 4 * j, dr["rwkv_mu_rkv"][l][j], 4)
            ppl(PP["mu"] + 12, dr["rwkv_mu_wa"][l].rearrange("a b -> (a b)"), 1)
            ppl(PP["kk"], dr["rwkv_k_k"][l], 4)
            ppl(PP["ka"], dr["rwkv_k_a"][l], 4)
            ppl(PP["rk"], dr["rwkv_r_k"][l].rearrange("a b -> (a b)"), 4)
            ppl(PP["a0"], dr["rwkv_a0"][l], 4)
            for j in range(4):
                ppl(PP["scw"] + 8 * j, dr["ssd_conv_w"][l][j], 8)
            ppl(PP["scb"], dr["ssd_conv_b"][l], 8)
            for j in range(4):
                ppl(PP["gcw"] + 12 * j, dr["gdn_conv_w"][l][j], 12)
            A(RPv("ssdA"), RPv("ssdA"), AF.Exp)
            Vts(RPv("ssdA"), RPv("ssdA"), -1.0, ALU.mult)
            A(RPv("gdnA"), RPv("gdnA"), AF.Exp)
            Vts(RPv("gdnA"), RPv("gdnA"), -1.0, ALU.mult)
            for t_ in [carry_rw, carry_sd, carry_gd] + st_rw + st_ret + st_sd + st_gd:
                Vset(t_[:], 0.0)

        def conv_block(ps, carry, bi, wcol, nblk, bias, out_ap, rbuf, acc):
            Acp(rbuf[:, 0:3], carry[:, bi, :])
            Acp(rbuf[:, 3:3 + NT], ps[:, 0:NT])
            Vts(acc[:], rbuf[:, 0:NT], pp[:, wcol + bi:wcol + bi + 1], ALU.mult)
            for j in range(1, 4):
                c0 = wcol + nblk * j + bi
                Vstt(acc[:], rbuf[:, j:j + NT], pp[:, c0:c0 + 1], acc[:], ALU.mult, ALU.add)
            Acp(carry[:, bi, :], rbuf[:, NT:NT + 3])
            if bias is None:
                A(out_ap, acc[:], AF.Silu)
            else:
                A(out_ap, acc[:], AF.Silu, bias=bias)

        def mixer_ret(l, mt, then_ssd=False):
            S_.barrier()
            HB = [(0, 3), (3, 3), (6, 2)]
            with contextlib.ExitStack() as sc:
                scopes.append(sc)
                Wqk = W(("in", l, O_RET, 512))
                qk_raw = T("rt_qkraw", [128, NT])
                qk = T("rt_qk", [128, 6, NT])
                t1 = T("rt_t1", [128, NT])
                for b in range(6):
                    h0, nh = HB[b % 3]
                    nr = 32 * nh
                    col0 = (0 if b < 3 else 256) + 32 * h0
                    ps = PS()
                    proj_fm(Wqk, col0, nr, ps)
                    Acp(qk_raw[0:nr, :], ps[0:nr, 0:NT])
                    ps2 = PS()
                    mm(ps2[0:nr, 0:NT], K("swapp")[0:nr, 0:nr], qk_raw[0:nr, :])
                    Vtt(t1[0:nr, :], qk_raw[0:nr, :], ropet[0:nr, 0, :], ALU.mult)
                    Vtt(qk[0:nr, b, :], ps2[0:nr, 0:NT], ropet[0:nr, 1, :], ALU.mult)
                    Vtt(qk[0:nr, b, :], qk[0:nr, b, :], t1[0:nr, :], ALU.add)
                Wv = W(("in", l, O_RET + 512, 512))
                v_tm = [T(f"rt_v{c}", [128, 512]) for c in range(NCH)]
                for c in range(NCH):
                    ps = PS()
                    proj_tm(Wv, 0, 512, c, ps)
                    Acp(v_tm[c][:], ps[:, 0:512])
                Wz = W(("in", l, O_RET + 1024, 512))
                zs = [T(f"rt_z{c}", [128, 512]) for c in range(NCH)]
                for c in range(NCH):
                    ps = PS()
                    proj_tm(Wz, 0, 512, c, ps)
                    A(zs[c][:], ps[:, 0:512], AF.Silu)
                ktail = T("rt_ktail", [128, 256])
                P = T("rt_P", [128, 3, 384])
                qd = T("rt_qd", [128, 3, 128])
                y = T("rt_y", [128, 512])
                tmp = T("rt_tmp", [128, 256])
                for c in range(NCH):
                    cs = slice(c * C, (c + 1) * C)
                    ps = PS()
                    for bq in range(3):
                        h0, nh = HB[bq]
                        nr = 32 * nh
                        tp(ps[:, 32 * h0:32 * h0 + nr], qk[0:nr, 3 + bq, cs], ident[0:nr, 0:nr])
                    Vtt(ktail[:].rearrange("p (h d) -> p h d", h=8), ps[:, 0:256].rearrange("p (h d) -> p h d", h=8),
                        K("ktbl")[:, 0:8].unsqueeze(2).to_broadcast([128, 8, 32]), ALU.mult)
                    pr = [PS() for _ in range(3)]
                    for r in range(3):
                        for bq in range(3):
                            if bq * 3 + r >= 8:
                                continue
                            mm(pr[r][:, bq * 128:(bq + 1) * 128], qk[32 * r:32 * r + 32, 3 + bq, cs], qk[32 * r:32 * r + 32, bq, cs])
                    for r in range(3):
                        w_ = 384 if r < 2 else 256
                        Vtt(P[:, r, 0:w_], pr[r][:, 0:w_], K("dm2")[:, r * 384:r * 384 + w_], ALU.mult)
                    for bq in range(3):
                        nr = 32 * HB[bq][1]
                        Vtt(qd[0:nr, bq, :], qk[0:nr, bq, cs], K("qdec")[0:nr, bq * 128:(bq + 1) * 128], ALU.mult)
                    py = PS()
                    for bq in range(3):
                        h0, nh = HB[bq]
                        nr = 32 * nh
                        mm(py[:, 64 * h0:64 * (h0 + nh)], qd[0:nr, bq, :], st_ret[bq][0:nr, 0:64 * nh], start=True, stop=False)
                        for r in range(nh):
                            h = h0 + r
                            mm(py[:, h * 64:(h + 1) * 64], P[:, r, bq * 128:(bq + 1) * 128], v_tm[c][:, h * 64:(h + 1) * 64],
                               start=False, stop=(r == nh - 1))
                    Acp(y[:], py[:, 0:512])
                    y3 = y[:].rearrange("p (h d) -> p h d", h=8)
                    rstd = head_rms(y3, 8, 64, 1e-6, "rt")
                    Vtt(y3, y3, rstd[:, 0:8].unsqueeze(2).to_broadcast([128, 8, 64]), ALU.mult)
                    Vtt(y[:], y[:], RPv("retnw"), ALU.mult)
                    Vtt(y[:], y[:], zs[c][:], ALU.mult)
                    tm_to_fm_bf16(y, 4, c)
                    for bq in range(3):
                        h0, nh = HB[bq]
                        nr, nv = 32 * nh, 64 * nh
                        ps = PS()
                        mm(ps[0:nr, 0:nv], ktail[:, 32 * h0:32 * h0 + nr], v_tm[c][:, 64 * h0:64 * h0 + nv])
                        Vtt(tmp[0:nr, 0:nv], ps[0:nr, 0:nv], K("mask01")[0:nr, 0:nv], ALU.mult)
                        Vtt(st_ret[bq][0:nr, 0:nv], st_ret[bq][0:nr, 0:nv], K("tblc")[0:nr, bq * 256:bq * 256 + nv], ALU.mult)
                        Vtt(st_ret[bq][0:nr, 0:nv], st_ret[bq][0:nr, 0:nv], tmp[0:nr, 0:nv], ALU.add)
                if then_ssd:
                    mixer_ssd(l, mt, do_barrier=False)
                scopes.pop()

        def decay_prep(ps_raw, nh, dtb, Aneg, tag):
            dt = T(f"dp_dt_{tag}", [128, nh])
            Vtt(dt[:], ps_raw, dtb, ALU.add)
            A(dt[:], dt[:], AF.Exp)
            A(dt[:], dt[:], AF.Ln, bias=1.0)
            la = T(f"dp_la_{tag}", [128, nh])
            Vtt(la[:], dt[:], Aneg, ALU.mult)
            ps = PS()
            mm(ps[:, 0:nh], K("iu"), la[:])
            g = T(f"dp_g_{tag}", [128, nh])
            Acp(g[:], ps[:, 0:nh])
            ng = T(f"dp_ng_{tag}", [128, nh])
            Vts(ng[:], g[:], -1.0, ALU.mult)
            eg = T(f"dp_eg_{tag}", [128, nh])
            A(eg[:], g[:], AF.Exp)
            return dict(dt=dt, la=la, g=g, ng=ng, eg=eg)

        def mixer_ssd(l, mt, do_barrier=True):
            if do_barrier:
                S_.barrier()
            with contextlib.ExitStack() as sc:
                scopes.append(sc)
                xbc = T("sd_xbc", [128, 8, NT])
                rbuf = T("sd_rbuf", [128, NT + 3])
                acc = T("sd_acc", [128, NT])
                for half in range(2):
                    Wx = W(("in", l, O_SSD + 512 * half, 512))
                    for b4 in range(4):
                        bi = half * 4 + b4
                        ps = PS()
                        proj_fm(Wx, b4 * 128, 128, ps)
                        conv_block(ps, carry_sd, bi, PP["scw"], 8, pp[:, PP["scb"] + bi:PP["scb"] + bi + 1], xbc[:, bi, :], rbuf, acc)
                Wz = W(("in", l, O_SSD + 1024, 512))
                zs = [T(f"sd_z{c}", [128, 512]) for c in range(NCH)]
                for c in range(NCH):
                    ps = PS()
                    proj_tm(Wz, 0, 512, c, ps)
                    A(zs[c][:], ps[:, 0:512], AF.Silu)
                Wdt = W(("in", l, O_SSD + 1536, 8))
                x_tm = T("sd_x", [128, 512])
                b_tm = T("sd_b", [128, 256])
                xdt = T("sd_xdt", [128, 512])
                xdt2 = T("sd_xdt2", [128, 512])
                sc_ = T("sd_sc", [128, 256])
                LAb = [T(f"sd_lab{i}", [128, 128]) for i in range(2)]
                DT = [T(f"sd_dt{i}", [128, 128]) for i in range(2)]
                P = T("sd_P", [128, 8, 128])
                yi = T("sd_yi", [128, 512])
                y = T("sd_y", [128, 512])
                egl = T("sd_egl", [128, 8])
                for c in range(NCH):
                    cs = slice(c * C, (c + 1) * C)
                    ps = PS()
                    proj_tm(Wdt, 0, 8, c, ps)
                    dp = decay_prep(ps[:, 0:8], 8, RPv("ssddtb"), RPv("ssdA"), "sd")
                    ps = PS()
                    mm(ps[:, 0:8], K("ones"), dp["la"][:])
                    A(egl[:], ps[:, 0:8], AF.Exp)
                    ps = PS()
                    for b in range(4):
                        tp(ps[:, b * 128:(b + 1) * 128], xbc[:, b, cs], ident)
                    Acp(x_tm[:], ps[:, 0:512])
                    ps = PS()
                    for g in range(2):
                        tp(ps[:, g * 128:(g + 1) * 128], xbc[:, 4 + g, cs], ident)
                    Vcp(b_tm[:], ps[:, 0:256])
                    Vtt(xdt[:].rearrange("p (h d) -> p h d", h=8), x_tm[:].rearrange("p (h d) -> p h d", h=8),
                        dp["dt"][:, 0:8].unsqueeze(2).to_broadcast([128, 8, 64]), ALU.mult)
                    ps = PS()
                    for g in range(2):
                        mm(ps[:, g * 128:(g + 1) * 128], xbc[:, 4 + g, cs], xbc[:, 6 + g, cs])
                    Acp(sc_[:], ps[:, 0:256])
                    for h in range(8):
                        g = h // 4
                        lab, dtm = LAb[h % 2], DT[h % 2]
                        Vcp(lab[:], dp["la"][:, h:h + 1].to_broadcast([128, 128]))
                        pd = PS()
                        mm(pd[:, 0:128], lab[:], K("iu"), start=True, stop=False)
                        mm(pd[:, 0:128], ident, K("negt"), start=False, stop=True)
                        A(dtm[:], pd[:, 0:128], AF.Exp, bias=dp["ng"][:, h:h + 1])
                        Vtt(P[:, h, :], sc_[:, g * 128:(g + 1) * 128], dtm[:], ALU.mult)
                        Vts(xdt2[:, h * 64:(h + 1) * 64], xdt[:, h * 64:(h + 1) * 64], dtm[:, 127:128], ALU.mult)
                    pyi = PS()
                    for g in range(2):
                        mm(pyi[:, g * 256:(g + 1) * 256], xbc[:, 6 + g, cs], st_sd[g][:])
                    Vtt(yi[:].rearrange("p (h d) -> p h d", h=8), pyi[:, 0:512].rearrange("p (h d) -> p h d", h=8),
                        dp["eg"][:, 0:8].unsqueeze(2).to_broadcast([128, 8, 64]), ALU.mult)
                    py = PS()
                    for h in range(8):
                        mm(py[:, h * 64:(h + 1) * 64], P[:, h, :], xdt[:, h * 64:(h + 1) * 64])
                    Vtt(y[:], py[:, 0:512], yi[:], ALU.add)
                    Vtt(yi[:].rearrange("p (h d) -> p h d", h=8), x_tm[:].rearrange("p (h d) -> p h d", h=8),
                        RPv("ssdD")[:, 0:8].unsqueeze(2).to_broadcast([128, 8, 64]), ALU.mult)
                    Vtt(y[:], y[:], yi[:], ALU.add)
                    Vtt(y[:], y[:], zs[c][:], ALU.mult)
                    y3 = y[:].rearrange("p (h d) -> p h d", h=2)
                    rstd = head_rms(y3, 2, 256, 1e-6, "sd")
                    Vtt(y3, y3, rstd[:, 0:2].unsqueeze(2).to_broadcast([128, 2, 256]), ALU.mult)
                    Vtt(y[:], y[:], RPv("ssdnw"), ALU.mult)
                    tm_to_fm_bf16(y, 8, c)
                    for g in range(2):
                        ps = PS()
                        mm(ps[:, 0:256], b_tm[:, g * 128:(g + 1) * 128], xdt2[:, g * 256:(g + 1) * 256])
                        Vtt(st_sd[g][:].rearrange("p (h d) -> p h d", h=4), st_sd[g][:].rearrange("p (h d) -> p h d", h=4),
                            egl[:, 4 * g:4 * g + 4].unsqueeze(2).to_broadcast([128, 4, 64]), ALU.mult)
                        Vtt(st_sd[g][:], st_sd[g][:], ps[:, 0:256], ALU.add)
                scopes.pop()

        def neumann_apply(Nm, NTm, Z, nh, width, tag, levels=7):
            N2 = [T(f"nm_n2_{tag}{h}", [128, 128], F32R) for h in range(nh)]
            NT2 = [T(f"nm_nt2_{tag}{h}", [128, 128], F32R) for h in range(nh)]
            cur, curT, nxt, nxtT = Nm, NTm, N2, NT2
            for lev in range(levels):
                ps = PS()
                for h in range(nh):
                    mm(ps[:, h * width:(h + 1) * width], curT[h][:], Z[:, h * width:(h + 1) * width])
                dZ = T(f"nm_dz_{tag}", [128, nh * width], F32R)
                Acp(dZ[:], ps[:, 0:nh * width])
                if lev < levels - 1:
                    for h in range(nh):
                        pq = PS()
                        mm(pq[:, 0:128], curT[h][:], cur[h][:])
                        mm(pq[:, 128:256], cur[h][:], curT[h][:])
                        cpe = Acp if (h % 2 == 0) else Vcp
                        cpe(nxt[h][:], pq[:, 0:128])
                        cpe(nxtT[h][:], pq[:, 128:256])
                Vtt(Z[:, 0:nh * width], Z[:, 0:nh * width], dZ[:], ALU.add)
                cur, curT, nxt, nxtT = nxt, nxtT, cur, curT

        def mixer_gdn(l, mt):
            S_.barrier()
            with contextlib.ExitStack() as sc:
                scopes.append(sc)
                qkv = T("gd_qkv", [128, 12, NT])
                rbuf = T("gd_rbuf", [128, NT + 3])
                acc = T("gd_acc", [128, NT])
                sq = T("gd_sq", [128, NT])
                for part in range(3):
                    Wx = W(("in", l, O_GDN + 512 * part, 512))
                    for b4 in range(4):
                        bi = part * 4 + b4
                        ps = PS()
                        proj_fm(Wx, b4 * 128, 128, ps)
                        conv_block(ps, carry_gd, bi, PP["gcw"], 12, None, qkv[:, bi, :], rbuf, acc)
                        if part < 2:
                            Vtt(sq[:], qkv[:, bi, :], qkv[:, bi, :], ALU.mult)
                            ps2 = PS()
                            mm(ps2[:, 0:NT], K("ones"), sq[:])
                            rsqrt_(sq[:], ps2[:, 0:NT], 1.0, 1e-6)
                            if part == 0:
                                Vstt(qkv[:, bi, :], qkv[:, bi, :], float(128 ** -0.5), sq[:], ALU.mult, ALU.mult)
                            else:
                                Vtt(qkv[:, bi, :], qkv[:, bi, :], sq[:], ALU.mult)
                import os
                GDSTOP = int(os.environ.get("GDSTOP", "9"))
                Wz = W(("in", l, O_GDN + 1536, 512))
                zs = [T(f"gd_z{c}", [128, 512]) for c in range(NCH)]
                for c in range(NCH):
                    ps = PS()
                    proj_tm(Wz, 0, 512, c, ps)
                    A(zs[c][:], ps[:, 0:512], AF.Silu)
                Wba = W(("in", l, O_GDN + 2048, 8))
                v_tm = T("gd_v", [128, 512])
                ktail = T("gd_ktail", [128, 512])
                beta = T("gd_beta", [128, 4])
                lnb = T("gd_lnb", [128, 4])
                gb = T("gd_gb", [128, 4])
                neg = T("gd_neg", [128, 4])
                egl = T("gd_egl", [128, 4])
                LAb = [T(f"gd_lab{h}", [128, 128]) for h in range(4)]
                DT = [T(f"gd_dt{h}", [128, 128]) for h in range(4)]
                DB = [T(f"gd_db{h}", [128, 128]) for h in range(4)]
                Nm = [T(f"gd_n{h}", [128, 128], F32R) for h in range(4)]
                NTm = [T(f"gd_nt{h}", [128, 128], F32R) for h in range(4)]
                attnT = [T(f"gd_at{h}", [128, 128]) for h in range(4)]
                Z = T("gd_Z", [128, 512], F32R)
                o1 = T("gd_o1", [128, 512])
                o = T("gd_o", [128, 512])
                for c in range(NCH):
                    cs = slice(c * C, (c + 1) * C)
                    if GDSTOP <= 1:
                        continue
                    ps = PS()
                    proj_tm(Wba, 0, 8, c, ps)
                    ba = T("gd_ba", [128, 8])
                    Acp(ba[:], ps[:, 0:8])
                    A(beta[:], ba[:, 0:4], AF.Sigmoid)
                    A(lnb[:], beta[:], AF.Ln)
                    dp = decay_prep(ba[:, 4:8], 4, RPv("gdndtb"), RPv("gdnA"), "gd")
                    Vtt(gb[:], dp["g"][:], lnb[:], ALU.add)
                    Vts(neg[:], dp["eg"][:], -1.0, ALU.mult)
                    ps = PS()
                    for h in range(4):
                        tp(ps[:, h * 128:(h + 1) * 128], qkv[:, 8 + h, cs], ident)
                    Acp(v_tm[:], ps[:, 0:512])
                    pk_ = PS()
                    for h in range(4):
                        tp(pk_[:, h * 128:(h + 1) * 128], qkv[:, 4 + h, cs], ident)
                    pk = T("gd_ktm", [128, 512])
                    Vcp(pk[:], pk_[:, 0:512])
                    for h in range(4):
                        kT = qkv[:, 4 + h, cs]
                        qT = qkv[:, h, cs]
                        Vcp(LAb[h][:], dp["la"][:, h:h + 1].to_broadcast([128, 128]))
                        pd = PS()
                        mm(pd[:, 0:128], LAb[h][:], K("iu"), start=True, stop=False)
                        mm(pd[:, 0:128], ident, K("negt"), start=False, stop=True)
                        mm(pd[:, 128:256], LAb[h][:], K("niu"), start=True, stop=False)
                        mm(pd[:, 128:256], ident, K("negs"), start=False, stop=True)
                        A(DT[h][:], pd[:, 0:128], AF.Exp, bias=dp["ng"][:, h:h + 1])
                        A(egl[:, h:h + 1], pd[:, 127:128], AF.Exp)
                        A(DB[h][:], pd[:, 128:256], AF.Exp, bias=gb[:, h:h + 1])
                        Vts(ktail[:, h * 128:(h + 1) * 128], pk[:, h * 128:(h + 1) * 128], DT[h][:, 127:128], ALU.mult)
                        pq = PS()
                        mm(pq[:, 0:128], kT, kT)
                        mm(pq[:, 128:256], kT, qT)
                        mm(pq[:, 256:384], kT, st_gd[h][:])
                        Vstt(Nm[h][:], pq[:, 0:128], -1.0, DB[h][:], ALU.mult, ALU.mult)
                        Vtt(attnT[h][:], pq[:, 128:256], DT[h][:], ALU.mult)
                        pt = PS()
                        tp(pt[:, 0:128], Nm[h][:].bitcast(F32), ident)
                        Acp(NTm[h][:], pt[:, 0:128])
                        Vstt(Z[:, h * 128:(h + 1) * 128], pq[:, 256:384], neg[:, h:h + 1], v_tm[:, h * 128:(h + 1) * 128], ALU.mult, ALU.add)
                        Vts(Z[:, h * 128:(h + 1) * 128], Z[:, h * 128:(h + 1) * 128], beta[:, h:h + 1], ALU.mult)
                    if GDSTOP <= 2:
                        continue
                    neumann_apply(Nm, NTm, Z, 4, 128, "gd")
                    if GDSTOP <= 3:
                        continue
                    po1 = PS()
                    po2 = PS()
                    for h in range(4):
                        mm(po1[:, h * 128:(h + 1) * 128], qkv[:, h, cs], st_gd[h][:])
                        mm(po2[:, h * 128:(h + 1) * 128], attnT[h][:], Z[:, h * 128:(h + 1) * 128].bitcast(F32))
                    Vtt(o1[:].rearrange("p (h d) -> p h d", h=4), po1[:, 0:512].rearrange("p (h d) -> p h d", h=4),
                        dp["eg"][:, 0:4].unsqueeze(2).to_broadcast([128, 4, 128]), ALU.mult)
                    Vtt(o[:], o1[:], po2[:, 0:512], ALU.add)
                    o3 = o[:].rearrange("p (h d) -> p h d", h=4)
                    rstd = head_rms(o3, 4, 128, 1e-6, "gd")
                    Vtt(o3, o3, rstd[:, 0:4].unsqueeze(2).to_broadcast([128, 4, 128]), ALU.mult)
                    Vtt(o3, o3, RPv("gdnnw").unsqueeze(1).to_broadcast([128, 4, 128]), ALU.mult)
                    Vtt(o[:], o[:], zs[c][:], ALU.mult)
                    tm_to_fm_bf16(o, 12, c)
                    for h in range(4):
                        ps = PS()
                        mm(ps[:, 0:128], ktail[:, h * 128:(h + 1) * 128], Z[:, h * 128:(h + 1) * 128].bitcast(F32))
                        Vstt(st_gd[h][:], st_gd[h][:], egl[:, h:h + 1], ps[:, 0:128], ALU.mult, ALU.add)
                scopes.pop()

        def mixer_rwkv(l, mt):
            S_.barrier()
            with contextlib.ExitStack() as sc:
                scopes.append(sc)
                rkv = T("rw_rkv", [128, 12, NT])
                wam = T("rw_wam", [128, NT])
                rbuf = T("rw_rbuf", [128, NT + 1])
                dtl = T("rw_d", [128, NT])

                def shift_block(ps, idx, out_ap):
                    Acp(rbuf[:, 0:1], carry_rw[:, idx:idx + 1])
                    Acp(rbuf[:, 1:NT + 1], ps[:, 0:NT])
                    Vtt(dtl[:], rbuf[:, 0:NT], rbuf[:, 1:NT + 1], ALU.subtract)
                    Vstt(out_ap, dtl[:], pp[:, PP["mu"] + idx:PP["mu"] + idx + 1], rbuf[:, 1:NT + 1], ALU.mult, ALU.add)
                    Acp(carry_rw[:, idx:idx + 1], rbuf[:, NT:NT + 1])
                for j in range(3):
                    Wj = W(("in", l, O_RW + 512 * j, 512))
                    for b in range(4):
                        ps = PS()
                        proj_fm(Wj, b * 128, 128, ps)
                        shift_block(ps, 4 * j + b, rkv[:, 4 * j + b, :])
                Wwa = W(("in", l, O_RW + 1536, 128))
                ps = PS()
                proj_fm(Wwa, 0, 128, ps)
                shift_block(ps, 12, wam[:])
                A(wam[0:64, :], wam[0:64, :], AF.Tanh)
                Wz = W(("in", l, O_RW + 1664, 512))
                zs = [T(f"rw_z{c}", [128, 512]) for c in range(NCH)]
                for c in range(NCH):
                    ps = PS()
                    proj_tm(Wz, 0, 512, c, ps)
                    A(zs[c][:], ps[:, 0:512], AF.Silu)
                iclr = T("rw_iclr", [128, 4, NT])
                kk = T("rw_kk", [128, 4, NT])
                sq = T("rw_sq", [128, NT])
                rkr = T("rw_rkr", [128, 4, NT])
                for b in range(4):
                    ps = PS()
                    mm(ps[:, 0:NT], wau[64:128, b * 128:(b + 1) * 128], wam[64:128, :])
                    A(iclr[:, b, :], ps[:, 0:NT], AF.Sigmoid, bias=pp[:, PP["a0"] + b:PP["a0"] + b + 1])
                    km = rkv[:, 4 + b, :]
                    Vts(kk[:, b, :], km, pp[:, PP["kk"] + b:PP["kk"] + b + 1], ALU.mult)
                    Vtt(sq[:], kk[:, b, :], kk[:, b, :], ALU.mult)
                    ps2 = PS()
                    mm(ps2[:, 0:NT], K("bones"), sq[:])
                    rsqrt_(sq[:], ps2[:, 0:NT], 1.0, 1e-6)
                    Vtt(kk[:, b, :], kk[:, b, :], sq[:], ALU.mult)
                    Vts(sq[:], iclr[:, b, :], -1.0, ALU.add, pp[:, PP["ka"] + b:PP["ka"] + b + 1], ALU.mult)
                    Vstt(km, sq[:], 1.0, km, ALU.add, ALU.mult)
                    Vtt(iclr[:, b, :], iclr[:, b, :], kk[:, b, :], ALU.mult)
                    Vstt(rkr[:, b, :], rkv[:, b, :], pp[:, PP["rk"] + b:PP["rk"] + b + 1], km, ALU.mult, ALU.mult)
                bm = iclr
                lw = T("rw_lw", [128, 512])
                Gx = T("rw_G", [128, 4, 132])
                eG = T("rw_eG", [128, 4, 128])
                enG = T("rw_enG", [128, 4, 128])
                lwf = T("rw_lwf", [128, 4, 128])
                AR = T("rw_AR", [128, 4, 256])
                kp = T("rw_kp", [128, 4, 128])
                bp = T("rw_bp", [128, 4, 128])
                sc1 = T("rw_sc1", [128, 4])
                sc2 = T("rw_sc2", [128, 4])
                sc3 = T("rw_sc3", [128, 4])
                A0s = [T(f"rw_a0s{b}", [128, 128]) for b in range(4)]
                kpT = T("rw_kpT", [128, 512])
                bpT = T("rw_bpT", [128, 512])
                v_tm = T("rw_v", [128, 512])
                SC = [T(f"rw_SC{h}", [128, 512]) for h in range(8)]
                Nm = [T(f"rw_N{h}", [128, 128], F32R) for h in range(8)]
                NTm = [T(f"rw_NT{h}", [128, 128], F32R) for h in range(8)]
                Z = T("rw_Z", [128, 512], F32R)
                y = T("rw_y", [128, 512])
                bon = T("rw_bon", [128, 8])
                mv = T("rw_mv", [128, 8])
                tmpb = T("rw_tmpb", [128, 128])
                for c in range(NCH):
                    cs = slice(c * C, (c + 1) * C)
                    ps = PS()
                    mm(ps[:, 0:512], wam[0:64, cs], wau[0:64, :])
                    Vtt(lw[:], ps[:, 0:512], RPv("w0"), ALU.add)
                    A(lw[:], lw[:], AF.Sigmoid)
                    Vts(lw[:], lw[:], float(-np.exp(-0.5)), ALU.mult)
                    for b in range(4):
                        ps = PS()
                        mm(ps[:, 0:132], lw[:, b * 128:(b + 1) * 128], K("tmat"))
                        Acp(Gx[:, b, :], ps[:, 0:132])
                        ps2 = PS()
                        tp(ps2[:, 0:128], lw[:, b * 128:(b + 1) * 128], ident)
                        Vcp(lwf[:, b, :], ps2[:, 0:128])
                    A(eG[:], Gx[:, :, 0:128], AF.Exp)
                    A(enG[:], Gx[:, :, 0:128], AF.Exp, scale=-1.0)
                    A(sc1[:], Gx[:, :, 128], AF.Exp, scale=-1.0)
                    A(sc2[:], Gx[:, :, 127], AF.Exp)
                    Vtt(sc3[:], sc1[:], sc2[:], ALU.mult)
                    Vtt(AR[:, :, 128:256], rkv[:, 0:4, cs], eG[:], ALU.mult)
                    Vtt(kp[:], rkv[:, 4:8, cs], enG[:], ALU.mult)
                    Vtt(bp[:], bm[:, :, cs], enG[:], ALU.mult)
                    A(lwf[:], lwf[:], AF.Exp, scale=-1.0)
                    Vtt(lwf[:], lwf[:], eG[:], ALU.mult)
                    Vstt(AR[:, :, 0:128], kk[:, :, cs], -1.0, lwf[:], ALU.mult, ALU.mult)
                    for b in range(4):
                        Vts(A0s[b][:], st_rw[b][:], sc1[:, b:b + 1], ALU.mult)
                    ps = PS()
                    ps2 = PS()
                    ps3 = PS()
                    for b in range(4):
                        tp(ps[:, b * 128:(b + 1) * 128], kp[:, b, :], ident)
                        tp(ps2[:, b * 128:(b + 1) * 128], bp[:, b, :], ident)
                        tp(ps3[:, b * 128:(b + 1) * 128], rkv[:, 8 + b, cs], ident)
                    Acp(kpT[:], ps[:, 0:512])
                    Vcp(bpT[:], ps2[:, 0:512])
                    Acp(v_tm[:], ps3[:, 0:512])
                    psb = PS()
                    for b in range(4):
                        mm(psb[:, 2 * b:2 * b + 2], rkr[:, b, cs], K("sel2")[:, 0:2])
                    Acp(bon[:], psb[:, 0:8])
                    for h in range(8):
                        b, r0 = h // 2, 64 * (h % 2)
                        rows = slice(r0, r0 + 64)
                        pa = PS()
                        mm(pa[:, 0:256], bp[rows, b, :], AR[rows, b, :])
                        mm(pa[:, 256:512], kp[rows, b, :], AR[rows, b, :])
                        Vtt(SC[h][:], pa[:, 0:512], K("maskA"), ALU.mult)
                        pn = PS()
                        mm(pn[:, 0:128], AR[rows, b, 0:128], bp[rows, b, :])
                        Vtt(Nm[h][:], pn[:, 0:128], K("sl"), ALU.mult)
                        Acp(NTm[h][:], SC[h][:, 0:128])
                    pz = PS()
                    for b in range(4):
                        mm(pz[:, b * 128:(b + 1) * 128], AR[:, b, 0:128], A0s[b][:], start=True, stop=False)
                        for hh in range(2):
                            h = 2 * b + hh
                            mm(pz[:, h * 64:(h + 1) * 64], SC[h][:, 256:384], v_tm[:, h * 64:(h + 1) * 64], start=False, stop=(hh == 1))
                    Acp(Z[:], pz[:, 0:512])
                    import os
                    if os.environ.get("RWDBG", "") == "pv":
                        pvs = T("rw_pvs", [128, 512])
                        Vcp(pvs[:], Z[:])
                    neumann_apply(Nm, NTm, Z, 8, 64, "rw")
                    py = PS()
                    for b in range(4):
                        mm(py[:, b * 128:(b + 1) * 128], AR[:, b, 128:256], A0s[b][:], start=True, stop=False)
                        for hh in range(2):
                            h = 2 * b + hh
                            mm(py[:, h * 64:(h + 1) * 64], SC[h][:, 384:512], v_tm[:, h * 64:(h + 1) * 64], start=False, stop=False)
                            mm(py[:, h * 64:(h + 1) * 64], SC[h][:, 128:256], Z[:, h * 64:(h + 1) * 64].bitcast(F32), start=False, stop=(hh == 1))
                    Acp(y[:], py[:, 0:512])
                    for b in range(4):
                        ps = PS()
                        mm(ps[:, 0:128], kpT[:, b * 128:(b + 1) * 128], v_tm[:, b * 128:(b + 1) * 128], start=True, stop=False)
                        mm(ps[:, 0:128], bpT[:, b * 128:(b + 1) * 128], Z[:, b * 128:(b + 1) * 128].bitcast(F32), start=False, stop=True)
                        Vstt(tmpb[:], ps[:, 0:128], sc2[:, b:b + 1], K("bones"), ALU.mult, ALU.mult)
                        Vstt(st_rw[b][:], st_rw[b][:], sc3[:, b:b + 1], tmpb[:], ALU.mult, ALU.add)
                    y3 = y[:].rearrange("p (h d) -> p h d", h=8)
                    Vred(mv[:], y3)
                    Vts(mv[:], mv[:], float(-1.0 / 64), ALU.mult)
                    Vtt(y3, y3, mv[:, 0:8].unsqueeze(2).to_broadcast([128, 8, 64]), ALU.add)
                    rstd = head_rms(y3, 8, 64, 64e-5, "rw", sq=kpT)
                    Vtt(y3, y3, rstd[:, 0:8].unsqueeze(2).to_broadcast([128, 8, 64]), ALU.mult)
                    Vtt(y[:], y[:], RPv("lnw"), ALU.mult)
                    Vtt(y[:], y[:], RPv("lnb"), ALU.add)
                    v3 = v_tm[:].rearrange("p (h d) -> p h d", h=8)
                    Vtt(Z[:].rearrange("p (h d) -> p h d", h=8), v3, bon[:, 0:8].unsqueeze(2).to_broadcast([128, 8, 64]), ALU.mult) if False else None
                    bz = bpT
                    Vtt(bz[:].rearrange("p (h d) -> p h d", h=8), v3, bon[:, 0:8].unsqueeze(2).to_broadcast([128, 8, 64]), ALU.mult)
                    Vtt(y[:], y[:], bz[:], ALU.add)
                    Vtt(y[:], y[:], zs[c][:], ALU.mult)
                    import os
                    dsel = os.environ.get("RWDBG", "")
                    if dsel == "py":
                        Acp(y[:], py[:, 0:512])
                    elif dsel == "z":
                        Vcp(y[:], Z[:])
                    elif dsel == "v":
                        Vcp(y[:], v_tm[:])
                    elif dsel == "lw":
                        Vcp(y[:], lw[:])
                    elif dsel == "kpT":
                        Vcp(y[:], kpT[:])
                    elif dsel == "pv":
                        Vcp(y[:], pvs[:])
                    elif dsel == "bon":
                        Vcp(y[:], bz[:])
                    tm_to_fm_bf16(y, 0, c)
                scopes.pop()

        def merge_out(l, mt, xsrc, xdst, is_last):
            S_.barrier()
            with contextlib.ExitStack() as sc:
                scopes.append(sc)
                macc = T("mg_acc", [128, 8, NT])
                mT = T("mg_mT", [128, 8, NT], BF16)
                sgs = [T(f"mg_sg{i}", [128, NT]) for i in range(2)]
                tts = [T(f"mg_t{i}", [128, NT]) for i in range(2)]
                for br in range(4):
                    Wb = W(("br", l, br, 0))
                    Wg = [None, None]
                    for half in range(2):
                        Wg[half] = W(("in", l, O_GATE + br * 1024 + 512 * half, 512), live=1 + half)
                        for d4 in range(4):
                            dmb = half * 4 + d4
                            sg, tt_ = sgs[d4 % 2], tts[d4 % 2]
                            pg = PS()
                            proj_fm(Wg[half], d4 * 128, 128, pg)
                            A(sg[:], pg[:, 0:NT], AF.Sigmoid)
                            pb_ = PS()
                            for kc in range(4):
                                mm(pb_[:, 0:NT], Wb[:, kc, dmb * 128:(dmb + 1) * 128], u_all[:, br * 4 + kc, :], start=(kc == 0), stop=(kc == 3))
                            if br == 0:
                                Vtt(macc[:, dmb, :], sg[:], pb_[:, 0:NT], ALU.mult)
                            elif br < 3:
                                Vtt(tt_[:], sg[:], pb_[:, 0:NT], ALU.mult)
                                Vtt(macc[:, dmb, :], macc[:, dmb, :], tt_[:], ALU.add)
                            else:
                                Vtt(tt_[:], sg[:], pb_[:, 0:NT], ALU.mult)
                                Vtt(mT[:, dmb, :], macc[:, dmb, :], tt_[:], ALU.add)
                for half in range(2):
                    Wo = W(("out", l, 512 * half, 512))
                    for c in range(NCH):
                        ps = PS()
                        for kc in range(8):
                            mm(ps[:, 0:512], mT[:, kc, c * C:(c + 1) * C], Wo[:, kc, :], start=(kc == 0), stop=(kc == 7))
                        Vtt(xt[:, c, half * 512:(half + 1) * 512], xt[:, c, half * 512:(half + 1) * 512], ps[:, 0:512], ALU.add)
                if is_last and final_norm:
                    junk = T("fn_junk", [128, D])
                    ssf = T("fn_ss", [128, NCH])
                    for c in range(NCH):
                        A(junk[:], xt[:, c, :], AF.Square, accum=ssf[:, c:c + 1])
                    rsqrt_(ssf[:], ssf[:], 1.0 / D, 1e-6)
                    for c in range(NCH):
                        Vstt(xt[:, c, :], xt[:, c, :], ssf[:, c:c + 1], finw[:], ALU.mult, ALU.mult)
                rows = slice(mt * NT, (mt + 1) * NT)
                dma("sp", xdst[rows, :].rearrange("(c p) d -> p c d", p=128), xt[:], sem_o, ins=[xt], outs=[("xd", id(xdst), mt)])
                scopes.pop()

        nl = len(layers)
        for li, l in enumerate(layers):
            load_params(l)
            xsrc = dr["x"] if li == 0 else scr[(li - 1) % 2]
            is_last = li == nl - 1
            xdst = out if is_last else scr[li % 2]
            for mt in range(NMT):
                rows = slice(mt * NT, (mt + 1) * NT)
                dma("sp", xt[:], xsrc[rows, :].rearrange("(c p) d -> p c d", p=128), sem_x,
                    ins=[("xd", id(xsrc), mt)], outs=[xt])
                dma("sp", ropet[:], dr["rope"][:, :, rows], sem_r, outs=[ropet])
                with contextlib.ExitStack() as sc:
                    scopes.append(sc)
                    S_.barrier()
                    junk = T("n_junk", [128, D])
                    ss = T("n_ss", [128, NCH])
                    hb = T("n_hb", [128, D], BF16)
                    for c in range(NCH):
                        A(junk[:], xt[:, c, :], AF.Square, accum=ss[:, c:c + 1])
                    rsqrt_(ss[:], ss[:], 1.0 / D, 1e-6)
                    for c in range(NCH):
                        Vstt(hb[:], xt[:, c, :], ss[:, c:c + 1], normw[:], ALU.mult, ALU.mult)
                        for kc in range(8):
                            tp(pbt[:, kc * 128:(kc + 1) * 128], hb[:, kc * 128:(kc + 1) * 128], identb[:])
                        CP(hT[:, :, c * C:(c + 1) * C], pbt[:, 0:1024].rearrange("p (k t) -> p k t", k=8))
                    scopes.pop()
                if "rwkv" in mixers:
                    mixer_rwkv(l, mt)
                fuse_rs = ("ret" in mixers) and ("ssd" in mixers)
                if "ret" in mixers:
                    mixer_ret(l, mt, then_ssd=fuse_rs)
                if "ssd" in mixers and not fuse_rs:
                    mixer_ssd(l, mt)
                if "gdn" in mixers:
                    mixer_gdn(l, mt)
                if dbg and is_last:
                    dma("sp", dbg_u[:, rows].rearrange("(b p) t -> p b t", p=128), u_all[:], sem_d, ins=[u_all], outs=[("dbg", mt)])
                merge_out(l, mt, xsrc, xdst, is_last)
        fin_ins = [("xd", id(out), mt) for mt in range(NMT)]
        if dbg:
            fin_ins += [("dbg", mt) for mt in range(NMT)]
        add("sp", lambda e: e.nop(), ins=fin_ins)
        assert jstate["used"] == len(jobs)
        S_.emit()
        build.stats = S_.stats
    return nc


NT_DEFAULT = 256


def kernel(**inputs):
    x = np.ascontiguousarray(np.asarray(inputs["x"], dtype=np.float32))
    B, S, _ = x.shape
    cst, rope = make_consts(S)
    params = {n: np.ascontiguousarray(np.asarray(inputs[n], dtype=np.float32)) for n in PARAM_NAMES}
    nc = build(S, list(range(DEPTH)), NT_DEFAULT, True)
    in_maps = []
    for b in range(B):
        m = {"x": x[b], "cst": cst, "rope": rope}
        m.update(params)
        in_maps.append(m)
    res = run_bass_kernel_spmd(nc, in_maps, core_ids=list(range(B)))
    return np.stack([np.asarray(r["out"], dtype=np.float32) for r in res.results], axis=0)
```
